# Optimizing a Trainium2 kernel written in Bass

```python
import math
import jax, jax.numpy as jnp
from jax import lax
import numpy as np

D_MODEL = 1024
BATCH = 16
SEQ = 2048
DEPTH = 4

N_MIXERS = 4
D_FF = 2816
NORM_EPS = 1e-6
GN_EPS = 1e-5
ROPE_THETA = 10000.0
Q_BLOCK = 128
NEG_INF = -1e30

A_HEADS = 16
A_HEAD_DIM = 64
A_PATTERNS = ((128, 1), (512, 4), (2048, 16))
A_GROUPS = len(A_PATTERNS)
A_WIDTH = A_GROUPS * A_HEADS * A_HEAD_DIM

B_HEADS = 16
B_NOPE = 64
B_ROPE = 32
B_QK = B_NOPE + B_ROPE
B_VDIM = 64
B_Q_RANK = 384
B_KV_RANK = 256

C_HEADS = 8
C_KDIM = 128
C_VDIM = 256
C_CHUNK = 128

D_WIDTH = 1024
D_BLOCKS = 4
D_BLOCK = D_WIDTH // D_BLOCKS
D_CONV = 4
LRU_C = 8.0

kernel_name = 'hybrid_interleaved_dilated_mla_retention_rglru'

f32 = jnp.float32


def rmsnorm(x, g):
    xf = x.astype(f32)
    y = xf * lax.rsqrt(jnp.mean(xf * xf, axis=-1, keepdims=True) + NORM_EPS)
    return (y * g.astype(f32)).astype(x.dtype)


def rope(x, pos):
    d = x.shape[-1]
    half = d // 2
    inv = ROPE_THETA ** (-jnp.arange(half, dtype=f32) * 2.0 / d)
    ang = pos.astype(f32)[:, :, None, None] * inv
    cos, sin = jnp.cos(ang), jnp.sin(ang)
    xf = x.astype(f32)
    x1, x2 = xf[..., :half], xf[..., half:]
    return jnp.concatenate([x1 * cos - x2 * sin, x2 * cos + x1 * sin], axis=-1).astype(x.dtype)


def swiglu(x, w_in, w_out):
    gate, up = jnp.split(x @ w_in, 2, axis=-1)
    return (jax.nn.silu(gate) * up) @ w_out


def _dilated_band(q, k, v, window, dilation):
    B, S, H, dh = q.shape
    W = window // dilation
    L = S // dilation
    nb = -(-L // W)
    Lp = nb * W

    def by_residue(t):
        t = t.reshape(B, L, dilation, H, t.shape[-1]).transpose(0, 2, 3, 1, 4)
        return jnp.pad(t, ((0, 0), (0, 0), (0, 0), (0, Lp - L), (0, 0)))

    def band(t):
        t = jnp.pad(t, ((0, 0), (0, 0), (0, 0), (W, 0), (0, 0)))
        t = t.reshape(B, dilation, H, nb + 1, W, t.shape[-1])
        return jnp.concatenate([t[:, :, :, :-1], t[:, :, :, 1:]], axis=4)

    qb = by_residue(q).reshape(B, dilation, H, nb, W, dh)
    kb = band(by_residue(k))
    vb = band(by_residue(v))
    s = jnp.einsum('brhnqd,brhnkd->brhnqk', qb, kb).astype(f32) * (dh ** -0.5)
    u = jnp.arange(W)[None, :, None]
    c = jnp.arange(2 * W)[None, None, :]
    blk = jnp.arange(nb)[:, None, None]
    valid = (c >= u) & (c <= u + W) & (blk * W - W + c >= 0)
    s = jnp.where(valid, s, NEG_INF)
    lse = jax.nn.logsumexp(s, axis=-1)
    p = jnp.exp(s - lse[..., None]).astype(v.dtype)
    o = jnp.einsum('brhnqk,brhnkd->brhnqd', p, vb)
    o = o.reshape(B, dilation, H, Lp, dh)[:, :, :, :L].transpose(0, 3, 1, 2, 4).reshape(B, S, H, dh)
    lse = lse.reshape(B, dilation, H, Lp)[..., :L].transpose(0, 3, 1, 2).reshape(B, S, H)
    return o, lse


def dilated_window_attention(x, pos, w_in, q_gain, k_gain, w_out):
    B, S, _ = x.shape
    q, k, v = jnp.split(x @ w_in, 3, axis=-1)
    shp = (B, S, A_GROUPS * A_HEADS, A_HEAD_DIM)
    q = rope(rmsnorm(q.reshape(shp), q_gain), pos)
    k = rope(rmsnorm(k.reshape(shp), k_gain), pos)
    v = v.reshape(shp)
    outs, lses = [], []
    for g, (window, dilation) in enumerate(A_PATTERNS):
        sl = slice(g * A_HEADS, (g + 1) * A_HEADS)
        o, lse = _dilated_band(q[:, :, sl], k[:, :, sl], v[:, :, sl], window, dilation)
        outs.append(o)
        lses.append(lse)
    wts = jax.nn.softmax(jnp.stack(lses, axis=0), axis=0).astype(x.dtype)
    o = jnp.einsum('gbsh,gbshd->bshd', wts, jnp.stack(outs, axis=0))
    return o.reshape(B, S, A_HEADS * A_HEAD_DIM) @ w_out


def _causal_attention(q, k, v):
    B, S, H, dq = q.shape
    nb = S // Q_BLOCK
    scale = dq ** -0.5
    qb = q.reshape(B, nb, Q_BLOCK, H, dq).transpose(1, 0, 2, 3, 4)
    kpos = jnp.arange(S)

    def block(args):
        qblk, b = args
        s = jnp.einsum('bqhd,bkhd->bhqk', qblk, k).astype(f32) * scale
        qpos = b * Q_BLOCK + jnp.arange(Q_BLOCK)
        s = jnp.where(kpos[None, :] <= qpos[:, None], s, NEG_INF)
        p = jax.nn.softmax(s, axis=-1).astype(v.dtype)
        return jnp.einsum('bhqk,bkhd->bqhd', p, v)

    o = lax.map(block, (qb, jnp.arange(nb)))
    return o.transpose(1, 0, 2, 3, 4).reshape(B, S, H, v.shape[-1])


def latent_attention(x, pos, w_in, q_a_gain, w_q_up, kv_a_gain, w_kv_up, q_gain, k_gain, w_out):
    B, S, _ = x.shape
    h = x @ w_in
    c_q = h[..., :B_Q_RANK]
    c_kv = h[..., B_Q_RANK:B_Q_RANK + B_KV_RANK]
    k_rope = h[..., B_Q_RANK + B_KV_RANK:]
    q = (rmsnorm(c_q, q_a_gain) @ w_q_up).reshape(B, S, B_HEADS, B_QK)
    kv = (rmsnorm(c_kv, kv_a_gain) @ w_kv_up).reshape(B, S, B_HEADS, B_NOPE + B_VDIM)
    k_nope, v = kv[..., :B_NOPE], kv[..., B_NOPE:]
    k = jnp.concatenate([k_nope, jnp.broadcast_to(k_rope[:, :, None, :], (B, S, B_HEADS, B_ROPE))], axis=-1)
    q = rmsnorm(q, q_gain)
    k = rmsnorm(k, k_gain)
    q = jnp.concatenate([q[..., :B_NOPE], rope(q[..., B_NOPE:], pos)], axis=-1)
    k = jnp.concatenate([k[..., :B_NOPE], rope(k[..., B_NOPE:], pos)], axis=-1)
    o = _causal_attention(q, k, v)
    return o.reshape(B, S, B_HEADS * B_VDIM) @ w_out


def retention(x, pos, w_in, w_out):
    B, S, _ = x.shape
    h = x @ w_in
    qk_w = C_HEADS * C_KDIM
    v_w = C_HEADS * C_VDIM
    q = h[..., :qk_w].reshape(B, S, C_HEADS, C_KDIM)
    k = h[..., qk_w:2 * qk_w].reshape(B, S, C_HEADS, C_KDIM)
    v = h[..., 2 * qk_w:2 * qk_w + v_w].reshape(B, S, C_HEADS, C_VDIM)
    g = h[..., 2 * qk_w + v_w:]
    q = rope(q, pos).astype(f32)
    k = rope(k, pos).astype(f32) * (C_KDIM ** -0.5)
    v = v.astype(f32)
    log_g = jnp.log(1.0 - 2.0 ** (-5.0 - jnp.arange(C_HEADS, dtype=f32)))
    idx = jnp.arange(C_CHUNK, dtype=f32)
    diff = idx[:, None] - idx[None, :]
    causal = diff >= 0
    decay = jnp.where(causal[None], jnp.exp(jnp.where(causal, diff, 0.0)[None] * log_g[:, None, None]), 0.0)
    xi = jnp.exp((idx + 1.0)[None, :] * log_g[:, None])
    zeta = jnp.exp((C_CHUNK - 1.0 - idx)[None, :] * log_g[:, None])
    chunk_decay = jnp.exp(C_CHUNK * log_g)
    nc = S // C_CHUNK

    def to_chunks(t):
        return t.reshape(B, nc, C_CHUNK, C_HEADS, t.shape[-1]).transpose(1, 0, 3, 2, 4)

    def step(R, inp):
        qc, kc, vc = inp
        s = jnp.einsum('bhid,bhjd->bhij', qc, kc) * decay[None]
        inner = jnp.einsum('bhij,bhjv->bhiv', s, vc)
        cross = jnp.einsum('bhid,bhdv->bhiv', qc, R) * xi[None, :, :, None]
        R = chunk_decay[None, :, None, None] * R + jnp.einsum('bhjd,bhjv->bhdv', kc * zeta[None, :, :, None], vc)
        return R, inner + cross

    R0 = jnp.zeros((B, C_HEADS, C_KDIM, C_VDIM), f32)
    _, y = lax.scan(step, R0, (to_chunks(q), to_chunks(k), to_chunks(v)))
    y = y.transpose(1, 0, 3, 2, 4).reshape(B, S, C_HEADS, C_VDIM)
    mu = jnp.mean(y, axis=-1, keepdims=True)
    var = jnp.mean(jnp.square(y - mu), axis=-1, keepdims=True)
    y = ((y - mu) * lax.rsqrt(var + GN_EPS)).astype(x.dtype).reshape(B, S, v_w)
    return (jax.nn.silu(g) * y) @ w_out


def _linear_combine(c1, c2):
    a1, b1 = c1
    a2, b2 = c2
    return a1 * a2, a2 * b1 + b2


def recurrent_block(x, w_in, conv_w, conv_b, w_rg, b_rg, w_ig, b_ig, lru_param, w_out):
    B, S, _ = x.shape
    gate, u = jnp.split(x @ w_in, 2, axis=-1)
    u = lax.conv_general_dilated(u, conv_w[:, None, :], window_strides=(1,),
                                 padding=[(D_CONV - 1, 0)],
                                 dimension_numbers=('NWC', 'WIO', 'NWC'),
                                 feature_group_count=D_WIDTH) + conv_b
    ub = u.reshape(B, S, D_BLOCKS, D_BLOCK)
    r = jax.nn.sigmoid(jnp.einsum('bsnc,ncd->bsnd', ub, w_rg).reshape(B, S, D_WIDTH) + b_rg)
    ig = jax.nn.sigmoid(jnp.einsum('bsnc,ncd->bsnd', ub, w_ig).reshape(B, S, D_WIDTH) + b_ig)
    log_a = -LRU_C * r.astype(f32) * jax.nn.softplus(-lru_param.astype(f32))
    a = jnp.exp(log_a)
    b = jnp.sqrt(jnp.maximum(-jnp.expm1(2.0 * log_a), 0.0)) * (ig * u).astype(f32)
    _, hs = lax.associative_scan(_linear_combine, (a, b), axis=1)
    return (jax.nn.gelu(gate) * hs.astype(x.dtype)) @ w_out


def _layers_of(m):
    return len(range(m, DEPTH, N_MIXERS))


def setup_inputs(seed: int = 0) -> dict:
    key = jax.random.key(seed)
    kit = iter(jax.random.split(key, 40))

    def w(shape, fan_in):
        return jax.random.normal(next(kit), shape, f32) * (fan_in ** -0.5)

    def gain(shape):
        return 1.0 + 0.05 * jax.random.normal(next(kit), shape, f32)

    def bias(shape):
        return 0.01 * jax.random.normal(next(kit), shape, f32)

    nA, nB, nC, nD = (_layers_of(m) for m in range(N_MIXERS))
    x = jax.random.normal(next(kit), (BATCH, SEQ, D_MODEL), f32)
    offsets = jax.random.randint(next(kit), (BATCH, 1), 0, 1024)
    positions = (offsets + jnp.arange(SEQ)[None, :]).astype(jnp.int32)
    u = jax.random.uniform(next(kit), (nD, D_WIDTH), f32, minval=0.9, maxval=0.999)
    a0 = u ** (1.0 / LRU_C)
    lru_param = jnp.log(a0) - jnp.log1p(-a0)
    return {
        'x': x,
        'positions': positions,
        'norm_gains': gain((DEPTH, 3, D_MODEL)),
        'ffn_w_in': w((DEPTH, 2, D_MODEL, 2 * D_FF), D_MODEL),
        'ffn_w_out': w((DEPTH, 2, D_FF, D_MODEL), D_FF),
        'a_w_in': w((nA, D_MODEL, 3 * A_WIDTH), D_MODEL),
        'a_q_gain': gain((nA, A_HEAD_DIM)),
        'a_k_gain': gain((nA, A_HEAD_DIM)),
        'a_w_out': w((nA, A_HEADS * A_HEAD_DIM, D_MODEL), A_HEADS * A_HEAD_DIM),
        'b_w_in': w((nB, D_MODEL, B_Q_RANK + B_KV_RANK + B_ROPE), D_MODEL),
        'b_q_a_gain': gain((nB, B_Q_RANK)),
        'b_w_q_up': w((nB, B_Q_RANK, B_HEADS * B_QK), B_Q_RANK),
        'b_kv_a_gain': gain((nB, B_KV_RANK)),
        'b_w_kv_up': w((nB, B_KV_RANK, B_HEADS * (B_NOPE + B_VDIM)), B_KV_RANK),
        'b_q_gain': gain((nB, B_QK)),
        'b_k_gain': gain((nB, B_QK)),
        'b_w_out': w((nB, B_HEADS * B_VDIM, D_MODEL), B_HEADS * B_VDIM),
        'c_w_in': w((nC, D_MODEL, 2 * C_HEADS * C_KDIM + 2 * C_HEADS * C_VDIM), D_MODEL),
        'c_w_out': w((nC, C_HEADS * C_VDIM, D_MODEL), C_HEADS * C_VDIM),
        'd_w_in': w((nD, D_MODEL, 2 * D_WIDTH), D_MODEL),
        'd_conv_w': w((nD, D_CONV, D_WIDTH), D_CONV),
        'd_conv_b': bias((nD, D_WIDTH)),
        'd_w_rg': w((nD, D_BLOCKS, D_BLOCK, D_BLOCK), D_BLOCK),
        'd_b_rg': bias((nD, D_WIDTH)),
        'd_w_ig': w((nD, D_BLOCKS, D_BLOCK, D_BLOCK), D_BLOCK),
        'd_b_ig': bias((nD, D_WIDTH)),
        'd_lru_param': lru_param,
        'd_w_out': w((nD, D_WIDTH, D_MODEL), D_WIDTH),
    }


def reference(x, positions, norm_gains, ffn_w_in, ffn_w_out,
              a_w_in, a_q_gain, a_k_gain, a_w_out,
              b_w_in, b_q_a_gain, b_w_q_up, b_kv_a_gain, b_w_kv_up, b_q_gain, b_k_gain, b_w_out,
              c_w_in, c_w_out,
              d_w_in, d_conv_w, d_conv_b, d_w_rg, d_b_rg, d_w_ig, d_b_ig, d_lru_param, d_w_out):
    for i in range(DEPTH):
        m, j = i % N_MIXERS, i // N_MIXERS
        x = x + 0.5 * swiglu(rmsnorm(x, norm_gains[i, 0]), ffn_w_in[i, 0], ffn_w_out[i, 0])
        h = rmsnorm(x, norm_gains[i, 1])
        if m == 0:
            y = dilated_window_attention(h, positions, a_w_in[j], a_q_gain[j], a_k_gain[j], a_w_out[j])
        elif m == 1:
            y = latent_attention(h, positions, b_w_in[j], b_q_a_gain[j], b_w_q_up[j], b_kv_a_gain[j],
                                 b_w_kv_up[j], b_q_gain[j], b_k_gain[j], b_w_out[j])
        elif m == 2:
            y = retention(h, positions, c_w_in[j], c_w_out[j])
        else:
            y = recurrent_block(h, d_w_in[j], d_conv_w[j], d_conv_b[j], d_w_rg[j], d_b_rg[j],
                                d_w_ig[j], d_b_ig[j], d_lru_param[j], d_w_out[j])
        x = x + y
        x = x + 0.5 * swiglu(rmsnorm(x, norm_gains[i, 2]), ffn_w_in[i, 1], ffn_w_out[i, 1])
    return x
```

```python
import numpy as np
import concourse.bass as bass
import concourse.mybir as mybir
from concourse.bass_utils import run_bass_kernel_spmd
from contextlib import ExitStack

F32 = mybir.dt.float32
BF16 = mybir.dt.bfloat16
I32 = mybir.dt.int32
AF = mybir.ActivationFunctionType
ALU = mybir.AluOpType
AX = mybir.AxisListType


class DSem:
    __slots__ = ("sem", "cnt")

    def __init__(self, sem):
        self.sem = sem
        self.cnt = 0


class Buf:
    __slots__ = ("t", "writers", "reads", "ds", "name")

    def __init__(self, t, name=""):
        self.t = t
        self.writers = {}
        self.reads = {}
        self.ds = None
        self.name = name

    def __getitem__(self, k):
        return self.t[k]


class Eng:
    def __init__(self, name, h, sem):
        self.name = name
        self.h = h
        self.sem = sem
        self.count = 0
        self.seen = {}


class Prog:
    def __init__(self):
        self.nc = bass.Bass("TRN2", target_bir_lowering=False)
        nc = self.nc
        self.root = ExitStack()
        self.E = {}
        for name, h in (("pe", nc.tensor), ("act", nc.scalar), ("dve", nc.vector),
                        ("pool", nc.gpsimd), ("sp", nc.sync)):
            sem = self.root.enter_context(nc.semaphore("es_" + name))
            self.E[name] = Eng(name, h, sem)
        self.free_ds = []
        self.n_ds = 0
        self.stage = None
        self.group = None
        self.stage_bufs = []
        self.uid = 0
        self.gbufs = []

    def _scope(self):
        if self.stage is not None:
            return self.stage
        if self.group is not None:
            return self.group
        return self.root

    def begin_group(self):
        assert self.group is None and self.stage is None
        self.group = ExitStack()
        self.group_bufs = []

    def end_group(self):
        assert self.stage is None
        for b in self.group_bufs:
            if b.ds is not None:
                self.free_ds.append(b.ds)
                b.ds = None
        self.group.close()
        self.group = None
        self.group_bufs = []

    def sb(self, shape, dtype, name=None):
        self.uid += 1
        name = (name or "t") + "_%d" % self.uid
        t = self._scope().enter_context(self.nc.sbuf_tensor(name, list(shape), dtype))
        b = Buf(t, name)
        if self.stage is not None:
            self.stage_bufs.append(b)
        elif self.group is not None:
            self.group_bufs.append(b)
        return b

    def ps(self, shape=(128, 512), dtype=F32, name=None):
        self.uid += 1
        name = (name or "p") + "_%d" % self.uid
        t = self._scope().enter_context(self.nc.psum_tensor(name, list(shape), dtype))
        b = Buf(t, name)
        self.stage_bufs.append(b)
        return b

    def dram(self, name, shape, dtype, kind="Internal"):
        t = self.nc.dram_tensor(name, list(shape), dtype, kind=kind)
        return t

    def dbuf(self, ap, name=""):
        b = Buf(ap, name)
        self.gbufs.append(b)
        return b

    def _get_ds(self):
        if self.free_ds:
            return self.free_ds.pop()
        self.n_ds += 1
        sem = self.root.enter_context(self.nc.semaphore("ds_%d" % self.n_ds))
        return DSem(sem)

    def _deps(self, eng, reads, writes, is_dma=False, partial=False):
        need = {}

        def add(tok):
            if tok is None:
                return
            sem, val = tok
            k = id(sem)
            if k not in need or need[k][1] < val:
                need[k] = tok

        for b in reads:
            for tok in b.writers.values():
                add(tok)
        for b in writes:
            if not partial:
                for tok in b.writers.values():
                    if is_dma and b.ds is not None and tok[0] is b.ds.sem:
                        continue
                    add(tok)
            for tok in b.reads.values():
                add(tok)
        for k, (sem, val) in need.items():
            if sem is eng.sem:
                if eng.name == "pe":
                    continue
                if eng.count - val >= 2:
                    continue
            if eng.seen.get(k, 0) >= val:
                continue
            eng.h.wait_ge(sem, val)
            eng.seen[k] = val

    def _record(self, tok, reads, writes, partial=False):
        k = id(tok[0])
        for b in reads:
            old = b.reads.get(k)
            if old is None or old[1] < tok[1]:
                b.reads[k] = tok
        for b in writes:
            if partial:
                b.writers[k] = tok
            else:
                b.writers = {k: tok}
                b.reads = {}

    def op(self, en, fn, reads=(), writes=(), signal=True, partial=False):
        eng = self.E[en]
        self._deps(eng, reads, writes, partial=partial)
        ins = fn(eng.h)
        if signal:
            eng.count += 1
            ins.then_inc(eng.sem, 1)
            tok = (eng.sem, eng.count)
        else:
            tok = (eng.sem, eng.count + 1)
        self._record(tok, reads, writes, partial)
        return ins

    def dma(self, q, out_ap, in_ap, reads=(), writes=(), partial=False, **kw):
        eng = self.E[q]
        self._deps(eng, reads, writes, is_dma=True, partial=partial)
        dst = writes[0]
        if dst.ds is None:
            dst.ds = self._get_ds()
        ins = eng.h.dma_start(out=out_ap, in_=in_ap, **kw)
        dst.ds.cnt += 16
        ins.then_inc(dst.ds.sem, 16)
        tok = (dst.ds.sem, dst.ds.cnt)
        self._record(tok, reads, writes, partial)
        return ins

    def begin_stage(self):
        assert self.stage is None
        self.stage = ExitStack()
        self.stage_bufs = []

    def barrier(self, extra_bufs=()):
        toks = {}
        for e in self.E.values():
            if e.count > 0:
                toks[id(e.sem)] = (e.sem, e.count)
        for b in list(self.stage_bufs) + list(getattr(self, 'group_bufs', [])) + list(self.gbufs) + list(extra_bufs):
            if b.ds is not None and b.ds.cnt > 0:
                toks[id(b.ds.sem)] = (b.ds.sem, b.ds.cnt)
            for k, tok in b.reads.items():
                if k not in toks or toks[k][1] < tok[1]:
                    toks[k] = tok
            for k, tok in b.writers.items():
                if k not in toks or toks[k][1] < tok[1]:
                    toks[k] = tok
        for e in self.E.values():
            for k, (sem, val) in toks.items():
                if sem is e.sem:
                    continue
                if e.seen.get(k, 0) >= val:
                    continue
                e.h.wait_ge(sem, val)
                e.seen[k] = val

    def end_stage(self, extra_bufs=()):
        self.barrier(extra_bufs)
        for b in self.stage_bufs:
            if b.ds is not None:
                self.free_ds.append(b.ds)
                b.ds = None
        self.stage.close()
        self.stage = None
        self.stage_bufs = []

    def finish(self, out_bufs):
        self.barrier(out_bufs)


A_DIL = (1, 4, 16)


def a_tile_start(g, ti):
    dil = A_DIL[g]
    nb = 16 // dil
    r, n = ti // nb, ti % nb
    return n * 128 * dil + r, dil, nb


NTOK = 4096
SEQ = 2048
DM = 1024
EPS = 1e-6


class Stream:
    def __init__(self, P, ap, nb=4):
        self.ap = ap
        self.view = ap.rearrange("(k p) t -> p k t", p=128)
        self.nb = nb
        self.bsz = NTOK // nb
        self.bufs = [P.dbuf(ap, "strm") for _ in range(nb)]

    def buf(self, t0):
        return self.bufs[t0 // self.bsz]


class NormUnit:
    def __init__(self, P, TT, g_ap):
        self.P = P
        self.TT = TT
        self.gcol = P.sb([128, 8], F32, "gcol")
        P.dma("sp", self.gcol[:], g_ap, writes=[self.gcol])
        self.ones = P.sb([128, 128], BF16, "ones")
        P.op("dve", lambda e: e.memset(self.ones[:], 1.0), writes=[self.ones])
        self.epsc = P.sb([128, 1], F32, "epsc")
        P.op("dve", lambda e: e.memset(self.epsc[:], EPS), writes=[self.epsc])
        self.sq = [P.sb([128, 8, TT], BF16, "sq") for _ in range(2)]
        self.rs = [P.sb([128, TT], F32, "rs") for _ in range(2)]
        self.pss = P.ps(name="pss")
        self.i = 0

    def run(self, xt, xn):
        P, TT = self.P, self.TT
        sq = self.sq[self.i % 2]
        rs = self.rs[self.i % 2]
        self.i += 1
        pss, ones, gcol = self.pss, self.ones, self.gcol
        P.op("act", lambda e: e.activation(out=sq[:], in_=xt[:], func=AF.Square), reads=[xt], writes=[sq])
        for k in range(8):
            P.op("pe", lambda e, k=k: e.matmul(pss[:, :TT], lhsT=ones[:], rhs=sq[:, k, :], start=(k == 0), stop=(k == 7)),
                 reads=[ones, sq], writes=[pss], signal=(k == 7))
        P.op("act", lambda e: e.activation(out=rs[:], in_=pss[:, :TT], func=AF.Sqrt, scale=1.0 / DM, bias=self.epsc[:, 0:1]),
             reads=[pss, self.epsc], writes=[rs])
        P.op("dve", lambda e: e.reciprocal(out=rs[:], in_=rs[:]), reads=[rs], writes=[rs])
        for k in range(8):
            P.op("dve", lambda e, k=k: e.scalar_tensor_tensor(out=xn[:, k, :], in0=xt[:, k, :], scalar=gcol[:, k:k + 1], in1=rs[:],
                                                               op0=ALU.mult, op1=ALU.mult),
                 reads=[xt, gcol, rs], writes=[xn], partial=(k > 0))


def load_x(P, S, t0, TT, xt):
    P.dma("sp", xt[:], S.view[:, :, t0:t0 + TT], reads=[S.buf(t0)], writes=[xt])


def store_x(P, D, t0, TT, xt):
    P.dma("sp", D.view[:, :, t0:t0 + TT], xt[:], reads=[xt], writes=[D.buf(t0)], partial=True)


def stage_ffn(P, C, S, D, li, fi):
    TT = 256
    NT = NTOK // TT
    P.begin_stage()
    w_in = C["ffn_w_in"][li, fi].rearrange("(k p) n -> p k n", p=128)
    w_out = C["ffn_w_out"][li, fi].rearrange("(k p) n -> p k n", p=128)
    win = [P.sb([128, 8, 512], BF16, "win") for _ in range(11)]
    wout = [P.sb([128, 2, 1024], BF16, "wout") for _ in range(11)]
    N = NormUnit(P, TT, C["gains"][li, 0 if fi == 0 else 2])
    for g in [0, 5, 6, 1, 7, 2, 8, 3, 9, 4, 10]:
        P.dma("pool", win[g][:], w_in[:, :, g * 512:(g + 1) * 512], writes=[win[g]])
    for g in range(11):
        P.dma("pool", wout[g][:], w_out[:, 2 * g:2 * g + 2, :], writes=[wout[g]])
    xt = [P.sb([128, 8, TT], F32, "xt") for _ in range(2)]
    xn = [P.sb([128, 8, TT], BF16, "xn") for _ in range(2)]
    h = P.sb([128, 22, TT], BF16, "h")
    sg = [P.sb([128, TT], F32, "sg") for _ in range(2)]
    pg = [P.ps(name="pg") for _ in range(2)]
    pu = [P.ps(name="pu") for _ in range(2)]
    py = [P.ps(name="py") for _ in range(2)]

    load_x(P, S, 0, TT, xt[0])
    N.run(xt[0], xn[0])
    for t in range(NT):
        X, XN = xt[t % 2], xn[t % 2]
        if t + 1 < NT:
            load_x(P, S, (t + 1) * TT, TT, xt[(t + 1) % 2])
        for m in range(22):
            a, b, s = pg[m % 2], pu[m % 2], sg[m % 2]
            c0 = m * 128
            gi, go = c0 // 512, c0 % 512
            for k in range(8):
                P.op("pe", lambda e, k=k, gi=gi, go=go: e.matmul(a[:, :TT], lhsT=win[gi][:, k, go:go + 128], rhs=XN[:, k, :],
                                                                 start=(k == 0), stop=(k == 7)),
                     reads=[win[gi], XN], writes=[a], signal=(k == 7))
            c0 = 2816 + m * 128
            gi, go = c0 // 512, c0 % 512
            for k in range(8):
                P.op("pe", lambda e, k=k, gi=gi, go=go: e.matmul(b[:, :TT], lhsT=win[gi][:, k, go:go + 128], rhs=XN[:, k, :],
                                                                 start=(k == 0), stop=(k == 7)),
                     reads=[win[gi], XN], writes=[b], signal=(k == 7))
            P.op("act", lambda e: e.activation(out=s[:], in_=a[:, :TT], func=AF.Silu), reads=[a], writes=[s])
            P.op("dve", lambda e, m=m: e.tensor_tensor(out=h[:, m, :], in0=s[:], in1=b[:, :TT], op=ALU.mult),
                 reads=[s, b], writes=[h], partial=(m > 0))
        if t + 1 < NT:
            N.run(xt[(t + 1) % 2], xn[(t + 1) % 2])
        for mo in range(8):
            c = py[mo % 2]
            for kf in range(22):
                P.op("pe", lambda e, kf=kf, mo=mo: e.matmul(c[:, :TT], lhsT=wout[kf // 2][:, kf % 2, mo * 128:(mo + 1) * 128],
                                                            rhs=h[:, kf, :], start=(kf == 0), stop=(kf == 21)),
                     reads=[wout[kf // 2], h], writes=[c], signal=(kf == 21))
            P.op("dve", lambda e, mo=mo: e.scalar_tensor_tensor(out=X[:, mo, :], in0=c[:, :TT], scalar=0.5, in1=X[:, mo, :],
                                                                 op0=ALU.mult, op1=ALU.add),
                 reads=[c, X], writes=[X], partial=True)
        store_x(P, D, t * TT, TT, X)
    P.end_stage()


def small_specs():
    return [("d_small", [128, 8, 8]), ("ident", [128, 128]), ("c_zeta", [128, 8]), ("c_inv", [128, 64]),
            ("c_decT", [128, 8, 128]), ("c_xi", [128, 8, 128]),
            ("b_grow", [128, 832]), ("b_inv", [128, 16]), ("b_mask", [128, 4, 512]),
            ("a_grow", [128, 128]), ("a_inv", [128, 32]), ("a_mask", [128, 256])]


def int_specs():
    return [("pos_pm", [128, 32]), ("pos_a", [128, 96])]


def scratch_specs():
    return [("c_qkT", [16, 128, NTOK]), ("c_kz", [NTOK, 1024]), ("c_vg", [NTOK, 4096]), ("oT", [16, 128, NTOK]),
            ("b_qkT", [32, 96, NTOK]), ("b_v", [NTOK, 1024]),
            ("a_qkT", [6, 16, 128, SEQ]), ("a_v", [6, SEQ, 1024])]


def host_small(inputs):
    m = {}
    f = lambda a: np.asarray(a, dtype=np.float32)
    cols = [f(inputs["d_conv_w"])[0, j] for j in range(4)] + [f(inputs["d_conv_b"])[0], f(inputs["d_b_rg"])[0],
                                                            f(inputs["d_b_ig"])[0], f(inputs["d_lru_param"])[0]]
    ds = np.stack(cols, axis=-1)
    m["d_small"] = np.ascontiguousarray(ds.reshape(8, 128, 8).transpose(1, 0, 2))
    m["ident"] = np.eye(128, dtype=np.float32)
    lg = np.log(np.float32(1.0) - np.float32(2.0) ** (-5.0 - np.arange(8, dtype=np.float32))).astype(np.float32)
    idx = np.arange(128, dtype=np.float32)
    sc = np.float32(128.0 ** -0.5)
    m["c_zeta"] = np.ascontiguousarray(np.exp((127.0 - idx)[:, None] * lg[None, :]).astype(np.float32))
    m["c_inv"] = np.ascontiguousarray(np.broadcast_to((10000.0 ** (-np.arange(64, dtype=np.float32) * 2.0 / 128.0)).astype(np.float32), (128, 64)))
    diff = idx[None, :] - idx[:, None]
    dec = np.where((diff >= 0)[:, None, :], np.exp(np.where(diff >= 0, diff, 0.0)[:, None, :] * lg[None, :, None]), 0.0)
    m["c_decT"] = np.ascontiguousarray((dec * sc).astype(np.float32))
    xi = np.exp((idx + 1.0)[None, :] * lg[:, None]) * sc
    m["c_xi"] = np.ascontiguousarray(np.broadcast_to(xi[None].astype(np.float32), (128, 8, 128)))
    grow = np.concatenate([f(inputs["b_q_a_gain"])[0], f(inputs["b_kv_a_gain"])[0], f(inputs["b_q_gain"])[0], f(inputs["b_k_gain"])[0]])
    m["b_grow"] = np.ascontiguousarray(np.broadcast_to(grow[None, :], (128, 832)))
    m["b_inv"] = np.ascontiguousarray(np.broadcast_to((10000.0 ** (-np.arange(16, dtype=np.float32) * 2.0 / 32.0)).astype(np.float32), (128, 16)))
    key = np.arange(128)[:, None]
    col = np.arange(512)[None, :]
    m["b_mask"] = np.ascontiguousarray(np.stack([(col - i * 128 >= key) for i in range(4)], axis=1).astype(np.float32))
    agrow = np.concatenate([f(inputs["a_q_gain"])[0], f(inputs["a_k_gain"])[0]])
    m["a_grow"] = np.ascontiguousarray(np.broadcast_to(agrow[None, :], (128, 128)))
    m["a_inv"] = np.ascontiguousarray(np.broadcast_to((10000.0 ** (-np.arange(32, dtype=np.float32) * 2.0 / 64.0)).astype(np.float32), (128, 32)))
    q = np.arange(128)[None, :]
    m["a_mask"] = np.ascontiguousarray(np.concatenate([(key >= q), (key <= q)], axis=1).astype(np.float32))
    return m


def host_pos(pos):
    m = {}
    m["pos_pm"] = np.ascontiguousarray(pos.reshape(32, 128).T)
    cols = []
    for s_ in range(2):
        for g in range(3):
            for ti in range(16):
                st, dil, nb = a_tile_start(g, ti)
                cols.append(pos[s_, st:st + 127 * dil + 1:dil])
    m["pos_a"] = np.ascontiguousarray(np.stack(cols, axis=1).astype(np.int32))
    return m


MIXERS = {}


def stage_mix_d(P, C, S, D, li):
    TT = 512
    P.begin_stage()
    wi_d = C["d_w_in"][0].rearrange("(k p) n -> p k n", p=128)
    wo_d = C["d_w_out"][0].rearrange("(k p) n -> p k n", p=128)
    wrg_d = C["d_w_rg"][0].rearrange("n (cc p) d -> p (n cc) d", p=128)
    wig_d = C["d_w_ig"][0].rearrange("n (cc p) d -> p (n cc) d", p=128)
    N = NormUnit(P, TT, C["gains"][li, 1])
    wi = [P.sb([128, 8, 512], BF16, "wi") for _ in range(4)]
    for g in [2, 0, 3, 1]:
        P.dma("pool", wi[g][:], wi_d[:, :, g * 512:(g + 1) * 512], writes=[wi[g]])
    wrg = P.sb([128, 8, 256], BF16, "wrg")
    wig = P.sb([128, 8, 256], BF16, "wig")
    wo = P.sb([128, 8, 1024], BF16, "wo")
    P.dma("pool", wrg[:], wrg_d, writes=[wrg])
    P.dma("pool", wig[:], wig_d, writes=[wig])
    P.dma("pool", wo[:], wo_d, writes=[wo])
    sm = P.sb([128, 8, 8], F32, "dsm")
    P.dma("sp", sm[:], C["d_small"], writes=[sm])
    nsp = P.sb([128, 8], F32, "nsp")
    onec = P.sb([128, 1], F32, "onec")
    P.op("dve", lambda e: e.memset(onec[:], 1.0), writes=[onec])
    P.op("act", lambda e: e.activation(out=nsp[:], in_=sm[:, :, 7], func=AF.Exp, scale=-1.0), reads=[sm], writes=[nsp])
    P.op("act", lambda e: e.activation(out=nsp[:], in_=nsp[:], func=AF.Ln, bias=onec[:, 0:1]), reads=[nsp, onec], writes=[nsp])
    P.op("dve", lambda e: e.tensor_scalar(out=nsp[:], in0=nsp[:], scalar1=-8.0, scalar2=None, op0=ALU.mult), reads=[nsp], writes=[nsp])

    xt = [P.sb([128, 8, TT], F32, "xt") for _ in range(2)]
    xn = [P.sb([128, 8, TT], BF16, "xn") for _ in range(2)]
    og = P.sb([128, 8, TT], BF16, "og")
    ub = [P.sb([128, 2, TT + 3], F32, "ub") for _ in range(2)]
    uc = [P.sb([128, 2, TT], F32, "uc") for _ in range(2)]
    ucb = [P.sb([128, 2, TT], BF16, "ucb") for _ in range(2)]
    carry_u = P.sb([128, 8, 3], F32, "carry_u")
    carry_h = P.sb([128, 8], F32, "carry_h")
    tmp = {nm: [P.sb([128, TT], F32, nm) for _ in range(2)] for nm in ("r", "ig", "a", "s", "b", "hs", "gl")}
    pu = [P.ps(name="pu") for _ in range(2)]
    pr, pi, pgt = P.ps(name="pr"), P.ps(name="pi"), P.ps(name="pgt")
    py = [P.ps(name="py") for _ in range(2)]

    NT = NTOK // TT
    load_x(P, S, 0, TT, xt[0])
    N.run(xt[0], xn[0])
    it = 0
    for t in range(NT):
        X, XN = xt[t % 2], xn[t % 2]
        if t + 1 < NT:
            load_x(P, S, (t + 1) * TT, TT, xt[(t + 1) % 2])
        if t % (SEQ // TT) == 0:
            P.op("pool", lambda e: e.memset(carry_u[:], 0.0), writes=[carry_u])
            P.op("pool", lambda e: e.memset(carry_h[:], 0.0), writes=[carry_h])
        for n in range(4):
            UB, UC, UCB = ub[n % 2], uc[n % 2], ucb[n % 2]
            for cc in range(2):
                c = 2 * n + cc
                p_u = pu[cc]
                col = 1024 + c * 128
                gi, go = col // 512, col % 512
                for k in range(8):
                    P.op("pe", lambda e, k=k, gi=gi, go=go: e.matmul(p_u[:, :TT], lhsT=wi[gi][:, k, go:go + 128], rhs=XN[:, k, :],
                                                                     start=(k == 0), stop=(k == 7)),
                         reads=[wi[gi], XN], writes=[p_u], signal=(k == 7))
                P.op("pool", lambda e, cc=cc, c=c: e.tensor_copy(out=UB[:, cc, 0:3], in_=carry_u[:, c, :]), reads=[carry_u], writes=[UB], partial=(cc > 0))
                P.op("act", lambda e, cc=cc: e.copy(out=UB[:, cc, 3:3 + TT], in_=p_u[:, :TT]), reads=[p_u], writes=[UB], partial=True)
                P.op("pool", lambda e, cc=cc, c=c: e.tensor_copy(out=carry_u[:, c, :], in_=UB[:, cc, TT:TT + 3]), reads=[UB], writes=[carry_u], partial=True)
                P.op("dve", lambda e, cc=cc, c=c: e.tensor_scalar(out=UC[:, cc, :], in0=UB[:, cc, 3:3 + TT], scalar1=sm[:, c, 3:4], scalar2=sm[:, c, 4:5],
                                                                   op0=ALU.mult, op1=ALU.add), reads=[UB, sm], writes=[UC], partial=(cc > 0))
                for j in range(3):
                    P.op("dve", lambda e, cc=cc, c=c, j=j: e.scalar_tensor_tensor(out=UC[:, cc, :], in0=UB[:, cc, j:j + TT], scalar=sm[:, c, j:j + 1], in1=UC[:, cc, :],
                                                                                   op0=ALU.mult, op1=ALU.add), reads=[UB, sm, UC], writes=[UC], partial=True)
                P.op("act", lambda e, cc=cc: e.copy(out=UCB[:, cc, :], in_=UC[:, cc, :]), reads=[UC], writes=[UCB], partial=(cc > 0))
            for dc in range(2):
                c = 2 * n + dc
                i2 = it % 2
                it += 1
                r, ig, a, s, b, hs, gl = (tmp[nm][i2] for nm in ("r", "ig", "a", "s", "b", "hs", "gl"))
                for cc in range(2):
                    P.op("pe", lambda e, cc=cc, dc=dc, n=n: e.matmul(pr[:, :TT], lhsT=wrg[:, 2 * n + cc, dc * 128:(dc + 1) * 128], rhs=UCB[:, cc, :],
                                                                     start=(cc == 0), stop=(cc == 1)), reads=[wrg, UCB], writes=[pr], signal=(cc == 1))
                for cc in range(2):
                    P.op("pe", lambda e, cc=cc, dc=dc, n=n: e.matmul(pi[:, :TT], lhsT=wig[:, 2 * n + cc, dc * 128:(dc + 1) * 128], rhs=UCB[:, cc, :],
                                                                     start=(cc == 0), stop=(cc == 1)), reads=[wig, UCB], writes=[pi], signal=(cc == 1))
                gi, go = (c * 128) // 512, (c * 128) % 512
                for k in range(8):
                    P.op("pe", lambda e, k=k, gi=gi, go=go: e.matmul(pgt[:, :TT], lhsT=wi[gi][:, k, go:go + 128], rhs=XN[:, k, :],
                                                                     start=(k == 0), stop=(k == 7)),
                         reads=[wi[gi], XN], writes=[pgt], signal=(k == 7))
                P.op("act", lambda e, c=c: e.activation(out=r[:], in_=pr[:, :TT], func=AF.Sigmoid, bias=sm[:, c, 5:6]), reads=[pr, sm], writes=[r])
                P.op("act", lambda e, c=c: e.activation(out=ig[:], in_=pi[:, :TT], func=AF.Sigmoid, bias=sm[:, c, 6:7]), reads=[pi, sm], writes=[ig])
                P.op("act", lambda e, c=c: e.activation(out=a[:], in_=r[:], func=AF.Exp, scale=nsp[:, c:c + 1]), reads=[r, nsp], writes=[a])
                P.op("act", lambda e: e.activation(out=s[:], in_=a[:], func=AF.Square), reads=[a], writes=[s])
                P.op("act", lambda e: e.activation(out=s[:], in_=s[:], func=AF.Sqrt, scale=-1.0, bias=onec[:, 0:1]), reads=[s, onec], writes=[s])
                P.op("dve", lambda e, dc=dc: e.tensor_tensor(out=b[:], in0=ig[:], in1=UC[:, dc, :], op=ALU.mult), reads=[ig, UC], writes=[b])
                P.op("dve", lambda e: e.tensor_tensor(out=b[:], in0=b[:], in1=s[:], op=ALU.mult), reads=[b, s], writes=[b])
                P.op("dve", lambda e, c=c: e.tensor_tensor_scan(out=hs[:], data0=a[:], data1=b[:], initial=carry_h[:, c:c + 1], op0=ALU.mult, op1=ALU.add),
                     reads=[a, b, carry_h], writes=[hs])
                P.op("pool", lambda e, c=c: e.tensor_copy(out=carry_h[:, c:c + 1], in_=hs[:, TT - 1:TT]), reads=[hs], writes=[carry_h], partial=True)
                P.op("act", lambda e: e.activation(out=gl[:], in_=pgt[:, :TT], func=AF.Gelu_apprx_tanh), reads=[pgt], writes=[gl])
                P.op("dve", lambda e, c=c: e.tensor_tensor(out=og[:, c, :], in0=gl[:], in1=hs[:], op=ALU.mult), reads=[gl, hs], writes=[og], partial=(c > 0))
        if t + 1 < NT:
            N.run(xt[(t + 1) % 2], xn[(t + 1) % 2])
        for mo in range(8):
            cps = py[mo % 2]
            for k in range(8):
                P.op("pe", lambda e, k=k, mo=mo: e.matmul(cps[:, :TT], lhsT=wo[:, k, mo * 128:(mo + 1) * 128], rhs=og[:, k, :],
                                                          start=(k == 0), stop=(k == 7)), reads=[wo, og], writes=[cps], signal=(k == 7))
            P.op("dve", lambda e, mo=mo: e.tensor_tensor(out=X[:, mo, :], in0=cps[:, :TT], in1=X[:, mo, :], op=ALU.add),
                 reads=[cps, X], writes=[X], partial=True)
        store_x(P, D, t * TT, TT, X)
    P.end_stage()


TWO_PI = 6.283185307179586
C1_2PI = 6.28125
C2_2PI = TWO_PI - C1_2PI


def mm_group(P, ps_ap, ps_buf, pairs, reads_extra=()):
    n = len(pairs)
    for i, (l, r, bufs) in enumerate(pairs):
        P.op("pe", lambda e, l=l, r=r, i=i: e.matmul(ps_ap, lhsT=l, rhs=r, start=(i == 0), stop=(i == n - 1)),
             reads=list(bufs) + list(reads_extra), writes=[ps_buf], signal=(i == n - 1))


def rope_tables(P, posi, inv, nt, half, cos, sin):
    posf = P.sb([128, nt], F32, "posf")
    ang = P.sb([128, nt, half], F32, "ang")
    a2 = P.sb([128, nt, half], F32, "a2")
    ni = P.sb([128, nt, half], I32, "ni")
    nf = P.sb([128, nt, half], F32, "nf")
    P.op("dve", lambda e: e.tensor_copy(out=posf[:], in_=posi[:]), reads=[posi], writes=[posf])
    P.op("dve", lambda e: e.tensor_tensor(out=ang[:], in0=posf[:, :].unsqueeze(2).to_broadcast([128, nt, half]),
                                          in1=inv[:, :].unsqueeze(1).to_broadcast([128, nt, half]), op=ALU.mult),
         reads=[posf, inv], writes=[ang])
    for dst, shift in ((sin, 0.0), (cos, 1.5707963267948966)):
        P.op("dve", lambda e, shift=shift: e.tensor_scalar(out=a2[:], in0=ang[:], scalar1=shift, scalar2=None, op0=ALU.add), reads=[ang], writes=[a2])
        P.op("dve", lambda e: e.tensor_scalar(out=ni[:], in0=a2[:], scalar1=1.0 / TWO_PI, scalar2=None, op0=ALU.mult), reads=[a2], writes=[ni])
        P.op("dve", lambda e: e.tensor_copy(out=nf[:], in_=ni[:]), reads=[ni], writes=[nf])
        P.op("dve", lambda e: e.scalar_tensor_tensor(out=a2[:], in0=nf[:], scalar=-C1_2PI, in1=a2[:], op0=ALU.mult, op1=ALU.add), reads=[nf, a2], writes=[a2])
        P.op("dve", lambda e: e.scalar_tensor_tensor(out=a2[:], in0=nf[:], scalar=-C2_2PI, in1=a2[:], op0=ALU.mult, op1=ALU.add), reads=[nf, a2], writes=[a2])
        P.op("dve", lambda e: e.tensor_scalar(out=a2[:], in0=a2[:], scalar1=3.1415925, scalar2=-3.1415925, op0=ALU.min, op1=ALU.max), reads=[a2], writes=[a2])
        P.op("act", lambda e, dst=dst: e.activation(out=dst[:], in_=a2[:], func=AF.Sin), reads=[a2], writes=[dst])


def rope_apply(P, ps3, psbuf, out3, outbuf, cosb, sinb, tabs, tmp, shape, partial):
    H, half = shape
    x1, x2 = ps3[:, :, 0:half], ps3[:, :, half:2 * half]
    v = lambda b: b[:, :].rearrange("p (h d) -> p h d", d=half)
    t1, t2, t3, t4 = tmp
    P.op("dve", lambda e: e.tensor_tensor(out=v(t1), in0=x1, in1=cosb, op=ALU.mult), reads=[psbuf] + tabs, writes=[t1])
    P.op("dve", lambda e: e.tensor_tensor(out=v(t2), in0=x2, in1=sinb, op=ALU.mult), reads=[psbuf] + tabs, writes=[t2])
    P.op("dve", lambda e: e.tensor_tensor(out=v(t3), in0=x2, in1=cosb, op=ALU.mult), reads=[psbuf] + tabs, writes=[t3])
    P.op("dve", lambda e: e.tensor_tensor(out=v(t4), in0=x1, in1=sinb, op=ALU.mult), reads=[psbuf] + tabs, writes=[t4])
    P.op("pool", lambda e: e.tensor_tensor(out=out3[:, :, 0:half], in0=v(t1), in1=v(t2), op=ALU.subtract), reads=[t1, t2], writes=[outbuf], partial=partial)
    P.op("pool", lambda e: e.tensor_tensor(out=out3[:, :, half:2 * half], in0=v(t3), in1=v(t4), op=ALU.add), reads=[t3, t4], writes=[outbuf], partial=True)


def stage_tables(P, C, pos_key, inv_key, nt, half, cos, sin):
    P.begin_stage()
    inv = P.sb([128, half], F32, "inv")
    P.dma("sp", inv[:], C[inv_key], writes=[inv])
    posi = P.sb([128, nt], I32, "posi")
    P.dma("sp", posi[:], C[pos_key], writes=[posi])
    rope_tables(P, posi, inv, nt, half, cos, sin)
    P.end_stage()


def stage_c1a(P, C, S, li, cos, sin):
    TT = 512
    P.begin_stage()
    w_d = C["c_w_in"][0].rearrange("(k p) n -> p k n", p=128)
    wc = [P.sb([128, 8, 512], BF16, "wc") for _ in range(4)]
    for g in range(4):
        P.dma("pool", wc[g][:], w_d[:, :, g * 512:(g + 1) * 512], writes=[wc[g]])
    N = NormUnit(P, TT, C["gains"][li, 1])
    ident = P.sb([128, 128], BF16, "ident")
    P.dma("pool", ident[:], C["ident"], writes=[ident])
    zeta = P.sb([128, 8], F32, "zeta")
    P.dma("sp", zeta[:], C["c_zeta"], writes=[zeta])
    xt = P.sb([128, 8, TT], F32, "xt")
    xn = [P.sb([128, 8, TT], BF16, "xn") for _ in range(2)]
    qk = [P.sb([128, 2048], BF16, "qk") for _ in range(2)]
    kz = [P.sb([128, 1024], BF16, "kz") for _ in range(2)]
    tmp = [P.sb([128, 256], F32, "rt") for _ in range(8)]
    qTs = [P.sb([128, 16, TT], BF16, "qTs") for _ in range(2)]
    pp = [P.ps(name="pp") for _ in range(3)]
    ptr = [P.ps(name="ptr") for _ in range(2)]
    NT = NTOK // TT
    qkT_v = C["c_qkT"].rearrange("h d t -> d h t")
    ipp = 0
    itr = 0
    load_x(P, S, 0, TT, xt)
    N.run(xt, xn[0])
    for t in range(NT):
        XN = xn[t % 2]
        QT = qTs[t % 2]
        if t + 1 < NT:
            load_x(P, S, (t + 1) * TT, TT, xt)
        for sub in range(4):
            j = t * 4 + sub
            QK = qk[j % 2]
            KZ = kz[j % 2]
            for cg in range(4):
                ps = pp[ipp % 3]
                ipp += 1
                mm_group(P, ps[:, :], ps, [(XN[:, k, sub * 128:(sub + 1) * 128], wc[cg][:, k, :], [XN, wc[cg]]) for k in range(8)])
                cb = cos[:, j, :].unsqueeze(1).to_broadcast([128, 4, 64])
                sb_ = sin[:, j, :].unsqueeze(1).to_broadcast([128, 4, 64])
                ps3 = ps[:, :].rearrange("p (h d) -> p h d", d=128)
                out3 = QK[:, cg * 512:(cg + 1) * 512].rearrange("p (h d) -> p h d", d=128)
                rope_apply(P, ps3, ps, out3, QK, cb, sb_, [cos, sin], tmp[4 * (cg % 2):4 * (cg % 2) + 4], (4, 64), partial=(cg > 0))
            P.op("dve", lambda e: e.tensor_tensor(out=KZ[:, :].rearrange("p (h d) -> p h d", d=128),
                                                  in0=QK[:, 1024:2048].rearrange("p (h d) -> p h d", d=128),
                                                  in1=zeta[:, :].unsqueeze(2).to_broadcast([128, 8, 128]), op=ALU.mult),
                 reads=[QK, zeta], writes=[KZ])
            P.dma("sp", C["c_kz"][j * 128:(j + 1) * 128, :], KZ[:], reads=[KZ], writes=[C["c_kz_buf"]], partial=True)
            for b4 in range(4):
                pt = ptr[itr % 2]
                itr += 1
                for i in range(4):
                    hh = b4 * 4 + i
                    P.op("pe", lambda e, hh=hh, i=i: e.matmul(pt[:, i * 128:(i + 1) * 128], lhsT=QK[:, hh * 128:(hh + 1) * 128], rhs=ident[:],
                                                              start=True, stop=True), reads=[QK, ident], writes=[pt], signal=(i == 3))
                P.op("act", lambda e, b4=b4: e.copy(out=QT[:, b4 * 4:(b4 + 1) * 4, sub * 128:(sub + 1) * 128],
                                                    in_=pt[:, :].rearrange("p (h t) -> p h t", t=128)),
                     reads=[pt], writes=[QT], partial=not (sub == 0 and b4 == 0))
        P.dma("sp", qkT_v[:, :, t * TT:(t + 1) * TT], QT[:], reads=[QT], writes=[C["c_qkT_buf"]], partial=True)
        if t + 1 < NT:
            N.run(xt, xn[(t + 1) % 2])
    P.end_stage()


def stage_c1b(P, C, S, li):
    TT = 512
    P.begin_stage()
    w_d = C["c_w_in"][0].rearrange("(k p) n -> p k n", p=128)
    wc = [P.sb([128, 8, 512], BF16, "wc") for _ in range(8)]
    for g in range(8):
        P.dma("pool", wc[g][:], w_d[:, :, (4 + g) * 512:(5 + g) * 512], writes=[wc[g]])
    N = NormUnit(P, TT, C["gains"][li, 1])
    xt = P.sb([128, 8, TT], F32, "xt")
    xn = [P.sb([128, 8, TT], BF16, "xn") for _ in range(2)]
    vg = [P.sb([128, 4096], BF16, "vg") for _ in range(2)]
    pp = [P.ps(name="pp") for _ in range(4)]
    NT = NTOK // TT
    ipp = 0
    load_x(P, S, 0, TT, xt)
    N.run(xt, xn[0])
    for t in range(NT):
        XN = xn[t % 2]
        if t + 1 < NT:
            load_x(P, S, (t + 1) * TT, TT, xt)
        for sub in range(4):
            j = t * 4 + sub
            VG = vg[j % 2]
            for cg in range(8):
                ps = pp[ipp % 4]
                ipp += 1
                mm_group(P, ps[:, :], ps, [(XN[:, k, sub * 128:(sub + 1) * 128], wc[cg][:, k, :], [XN, wc[cg]]) for k in range(8)])
                if cg < 4:
                    P.op("dve", lambda e, cg=cg: e.tensor_copy(out=VG[:, cg * 512:(cg + 1) * 512], in_=ps[:, :]), reads=[ps], writes=[VG], partial=(cg > 0))
                else:
                    P.op("act", lambda e, cg=cg: e.activation(out=VG[:, cg * 512:(cg + 1) * 512], in_=ps[:, :], func=AF.Silu), reads=[ps], writes=[VG], partial=True)
            P.dma("sp", C["c_vg"][j * 128:(j + 1) * 128, :], VG[:], reads=[VG], writes=[C["c_vg_buf"]], partial=True)
        if t + 1 < NT:
            N.run(xt, xn[(t + 1) % 2])
    P.end_stage()


def stage_c2(P, C):
    P.begin_stage()
    ident = P.sb([128, 128], BF16, "ident")
    P.dma("pool", ident[:], C["ident"], writes=[ident])
    decT = P.sb([128, 8, 128], F32, "decT")
    P.dma("sp", decT[:], C["c_decT"], writes=[decT])
    xib = P.sb([128, 8, 128], F32, "xib")
    P.dma("sp", xib[:], C["c_xi"], writes=[xib])
    eps5 = P.sb([128, 1], F32, "eps5")
    P.op("dve", lambda e: e.memset(eps5[:], 1e-5), writes=[eps5])
    lg = np.log(np.float32(1.0) - np.float32(2.0) ** (-5.0 - np.arange(8, dtype=np.float32))).astype(np.float32)
    cd = [float(np.exp(np.float32(128.0) * lg[h])) for h in range(8)]
    qkc = [P.sb([128, 16, 128], BF16, "qkc") for _ in range(2)]
    kzc = [P.sb([128, 1024], BF16, "kzc") for _ in range(2)]
    vgc = [P.sb([128, 4096], BF16, "vgc") for _ in range(2)]
    qx = [P.sb([128, 8, 128], BF16, "qx") for _ in range(2)]
    R = P.sb([128, 8, 256], F32, "R")
    Rb = P.sb([128, 8, 256], BF16, "Rb")
    AT = [P.sb([128, 128], BF16, "AT") for _ in range(2)]
    st = [P.sb([128, 6], F32, "st") for _ in range(2)]
    mv = [P.sb([128, 2], F32, "mv") for _ in range(2)]
    rstd = [P.sb([128, 1], F32, "rstd") for _ in range(2)]
    yn = [P.sb([128, 256], F32, "yn") for _ in range(2)]
    ob = [P.sb([128, 2048], BF16, "ob") for _ in range(2)]
    oTs = [P.sb([128, 16, 512], BF16, "oTs") for _ in range(2)]
    ps_s = [P.ps(name="ps_s") for _ in range(2)]
    ps_y = [P.ps(name="ps_y") for _ in range(2)]
    ps_u = [P.ps(name="ps_u") for _ in range(2)]
    ptr = [P.ps(name="ptr") for _ in range(2)]
    qkT_v = C["c_qkT"].rearrange("h d t -> d h t")
    oT_v = C["oT"].rearrange("k f t -> f k t")
    it = 0
    itr = 0

    def load_chunk(g):
        i = g % 2
        P.dma("sp", qkc[i][:], qkT_v[:, :, g * 128:(g + 1) * 128], reads=[C["c_qkT_buf"]], writes=[qkc[i]])
        P.dma("sp", kzc[i][:], C["c_kz"][g * 128:(g + 1) * 128, :], reads=[C["c_kz_buf"]], writes=[kzc[i]])
        P.dma("sp", vgc[i][:], C["c_vg"][g * 128:(g + 1) * 128, :], reads=[C["c_vg_buf"]], writes=[vgc[i]])

    load_chunk(0)
    for g in range(32):
        c = g % 16
        QKC, KZC, VGC, QX, OB = qkc[g % 2], kzc[g % 2], vgc[g % 2], qx[g % 2], ob[g % 2]
        OT = oTs[(g // 4) % 2]
        if g + 1 < 32:
            load_chunk(g + 1)
        if c > 0:
            P.op("pool", lambda e: e.tensor_tensor(out=QX[:], in0=QKC[:, 0:8, :], in1=xib[:], op=ALU.mult), reads=[QKC, xib], writes=[QX])
        for h in range(8):
            i2 = it % 2
            it += 1
            pss, psy, psu = ps_s[i2], ps_y[i2], ps_u[i2]
            vh = VGC[:, h * 256:(h + 1) * 256]
            P.op("pe", lambda e, h=h: e.matmul(pss[:, 0:128], lhsT=QKC[:, 8 + h, :], rhs=QKC[:, h, :], start=True, stop=True), reads=[QKC], writes=[pss])
            P.op("dve", lambda e, h=h: e.tensor_tensor(out=AT[i2][:], in0=pss[:, 0:128], in1=decT[:, h, :], op=ALU.mult), reads=[pss, decT], writes=[AT[i2]])
            if c > 0:
                mm_group(P, psy[:, 0:256], psy, [(AT[i2][:], vh, [AT[i2], VGC]), (QX[:, h, :], Rb[:, h, :], [QX, Rb])])
            else:
                mm_group(P, psy[:, 0:256], psy, [(AT[i2][:], vh, [AT[i2], VGC])])
            if c < 15:
                P.op("pe", lambda e, h=h: e.matmul(psu[:, 0:256], lhsT=KZC[:, h * 128:(h + 1) * 128], rhs=VGC[:, h * 256:(h + 1) * 256], start=True, stop=True),
                     reads=[KZC, VGC], writes=[psu])
                if c == 0:
                    P.op("dve", lambda e, h=h: e.tensor_copy(out=R[:, h, :], in_=psu[:, 0:256]), reads=[psu, Rb], writes=[R], partial=True)
                else:
                    P.op("dve", lambda e, h=h: e.scalar_tensor_tensor(out=R[:, h, :], in0=R[:, h, :], scalar=cd[h], in1=psu[:, 0:256], op0=ALU.mult, op1=ALU.add),
                         reads=[psu, R], writes=[R], partial=True)
                P.op("act", lambda e, h=h: e.copy(out=Rb[:, h, :], in_=R[:, h, :]), reads=[R], writes=[Rb], partial=True)
            P.op("dve", lambda e: e.bn_stats(out=st[i2][:], in_=psy[:, 0:256]), reads=[psy], writes=[st[i2]])
            P.op("dve", lambda e: e.bn_aggr(out=mv[i2][:], in_=st[i2][:]), reads=[st[i2]], writes=[mv[i2]])
            P.op("act", lambda e: e.activation(out=rstd[i2][:], in_=mv[i2][:, 1:2], func=AF.Sqrt, bias=eps5[:, 0:1]), reads=[mv[i2], eps5], writes=[rstd[i2]])
            P.op("dve", lambda e: e.reciprocal(out=rstd[i2][:], in_=rstd[i2][:]), reads=[rstd[i2]], writes=[rstd[i2]])
            P.op("dve", lambda e: e.tensor_scalar(out=yn[i2][:], in0=psy[:, 0:256], scalar1=mv[i2][:, 0:1], scalar2=rstd[i2][:, 0:1], op0=ALU.subtract, op1=ALU.mult),
                 reads=[psy, mv[i2], rstd[i2]], writes=[yn[i2]])
            P.op("pool", lambda e, h=h: e.tensor_tensor(out=OB[:, h * 256:(h + 1) * 256], in0=yn[i2][:], in1=VGC[:, 2048 + h * 256:2048 + (h + 1) * 256], op=ALU.mult),
                 reads=[yn[i2], VGC], writes=[OB], partial=(h > 0))
        for b4 in range(4):
            pt = ptr[itr % 2]
            itr += 1
            for i in range(4):
                kk = b4 * 4 + i
                P.op("pe", lambda e, kk=kk, i=i: e.matmul(pt[:, i * 128:(i + 1) * 128], lhsT=OB[:, kk * 128:(kk + 1) * 128], rhs=ident[:], start=True, stop=True),
                     reads=[OB, ident], writes=[pt], signal=(i == 3))
            P.op("act", lambda e, b4=b4: e.copy(out=OT[:, b4 * 4:(b4 + 1) * 4, (g % 4) * 128:(g % 4 + 1) * 128], in_=pt[:, :].rearrange("p (k t) -> p k t", t=128)),
                 reads=[pt], writes=[OT], partial=not (g % 4 == 0 and b4 == 0))
        if g % 4 == 3:
            t0 = (g // 4) * 512
            P.dma("sp", oT_v[:, 0:16, t0:t0 + 512], OT[:], reads=[OT], writes=[C["oT_buf"]], partial=True)
    P.end_stage()


def stage_outproj(P, C, S, D, w_ap, nk):
    TT = 512
    P.begin_stage()
    w_d = w_ap.rearrange("(k p) n -> p k n", p=128)
    wo = [P.sb([128, 4, 1024], BF16, "wo") for _ in range(nk // 4)]
    for g in range(nk // 4):
        P.dma("pool", wo[g][:], w_d[:, 4 * g:4 * g + 4, :], writes=[wo[g]])
    oT_v = C["oT"].rearrange("k f t -> f k t")
    xt = [P.sb([128, 8, TT], F32, "xt") for _ in range(2)]
    ot = [P.sb([128, nk, TT], BF16, "ot") for _ in range(2)]
    py = [P.ps(name="py") for _ in range(2)]
    NT = NTOK // TT

    def ld(t):
        load_x(P, S, t * TT, TT, xt[t % 2])
        P.dma("sp", ot[t % 2][:], oT_v[:, 0:nk, t * TT:(t + 1) * TT], reads=[C["oT_buf"]], writes=[ot[t % 2]])

    ld(0)
    for t in range(NT):
        X, OTT = xt[t % 2], ot[t % 2]
        if t + 1 < NT:
            ld(t + 1)
        for mo in range(8):
            cps = py[mo % 2]
            mm_group(P, cps[:, :], cps, [(wo[k // 4][:, k % 4, mo * 128:(mo + 1) * 128], OTT[:, k, :], [wo[k // 4], OTT]) for k in range(nk)])
            P.op("dve", lambda e, mo=mo: e.tensor_tensor(out=X[:, mo, :], in0=cps[:, :], in1=X[:, mo, :], op=ALU.add), reads=[cps, X], writes=[X], partial=True)
        store_x(P, D, t * TT, TT, X)
    P.end_stage()


def stage_mix_c(P, C, S, D, li):
    P.begin_group()
    cos = P.sb([128, 32, 64], F32, "cos")
    sin = P.sb([128, 32, 64], F32, "sin")
    stage_tables(P, C, "pos_pm", "c_inv", 32, 64, cos, sin)
    stage_c1a(P, C, S, li, cos, sin)
    stage_c1b(P, C, S, li)
    stage_c2(P, C)
    stage_outproj(P, C, S, D, C["c_w_out"][0], 16)
    P.end_group()


def v3(ap, d):
    return ap.rearrange("p (h d) -> p h d", d=d)


def stage_b1(P, C, S, li, cos, sin, rk_all):
    TT = 512
    P.begin_stage()
    wbi = P.sb([128, 8, 672], BF16, "wbi")
    P.dma("pool", wbi[:], C["b_w_in"][0].rearrange("(k p) n -> p k n", p=128), writes=[wbi])
    wqu = P.sb([128, 3, 1536], BF16, "wqu")
    P.dma("pool", wqu[:], C["b_w_q_up"][0].rearrange("(k p) n -> p k n", p=128), writes=[wqu])
    wkvu = P.sb([128, 2, 2048], BF16, "wkvu")
    P.dma("pool", wkvu[:], C["b_w_kv_up"][0].rearrange("(k p) n -> p k n", p=128), writes=[wkvu])
    N = NormUnit(P, TT, C["gains"][li, 1])
    ident = P.sb([128, 128], BF16, "ident")
    P.dma("pool", ident[:], C["ident"], writes=[ident])
    grow = P.sb([128, 384 + 256 + 96 + 96], F32, "grow")
    P.dma("sp", grow[:], C["b_grow"], writes=[grow])
    gqa, gkva = grow[:, 0:384], grow[:, 384:640]
    GQ0, GK0 = 640, 736
    epsq = P.sb([128, 3], F32, "epsq")
    P.op("dve", lambda e: e.memset(epsq[:, 0:1], 1e-6), writes=[epsq])
    P.op("dve", lambda e: e.memset(epsq[:, 1:2], 96e-6), writes=[epsq], partial=True)
    xt = P.sb([128, 8, TT], F32, "xt")
    xn = [P.sb([128, 8, TT], BF16, "xn") for _ in range(2)]
    sq = [P.sb([128, 512], F32, "sq") for _ in range(2)]
    ssa = [P.sb([128, 4], F32, "ssa") for _ in range(2)]
    cn = [P.sb([128, 640], BF16, "cn") for _ in range(2)]
    cT = [P.sb([128, 5, 128], BF16, "cT") for _ in range(2)]
    qf = [P.sb([128, 1536], F32, "qf") for _ in range(2)]
    kvf = [P.sb([128, 2048], F32, "kvf") for _ in range(2)]
    krf = [P.sb([128, 32], F32, "krf") for _ in range(2)]
    ss16 = [P.sb([128, 32], F32, "ss16") for _ in range(2)]
    tq = [P.sb([128, 1536], F32, "tq") for _ in range(2)]
    gt = [P.sb([128, 8, 16], F32, "gt") for _ in range(2)]
    rt = [P.sb([128, 16 * 16], F32, "rt") for _ in range(4)]
    krr = [P.sb([128, 32], F32, "krr") for _ in range(2)]
    qb = [P.sb([128, 1536], BF16, "qb") for _ in range(2)]
    kb = [P.sb([128, 1536], BF16, "kb") for _ in range(2)]
    vb = [P.sb([128, 1024], BF16, "vb") for _ in range(2)]
    qTs = [P.sb([128, 32, 128], BF16, "qTs") for _ in range(2)]
    psA, psB = P.ps(name="psA"), P.ps(name="psB")
    pq = [P.ps(name="pq") for _ in range(3)]
    ptr = [P.ps(name="ptr") for _ in range(2)]
    qkT_v = C["b_qkT"].rearrange("h d t -> d h t")
    NT = NTOK // TT
    ipq = 0
    itr = 0
    import os
    B1CUT = int(os.environ.get("B1CUT", "99"))
    load_x(P, S, 0, TT, xt)
    N.run(xt, xn[0])
    for t in range(NT):
        XN = xn[t % 2]
        if t + 1 < NT:
            load_x(P, S, (t + 1) * TT, TT, xt)
        for sub in range(4):
            j = t * 4 + sub
            b = j % 2
            SQ, SSA, CN, CT, QF, KVF, KRF, SS16, TQ, GT, KRR, QB, KB, VB, QT = (sq[b], ssa[b], cn[b], cT[b], qf[b], kvf[b], krf[b], ss16[b], tq[b], gt[b],
                                                                                  krr[b], qb[b], kb[b], vb[b], qTs[b])
            xs_ = lambda k: XN[:, k, sub * 128:(sub + 1) * 128]
            mm_group(P, psA[:, 0:384], psA, [(xs_(k), wbi[:, k, 0:384], [XN, wbi]) for k in range(8)])
            mm_group(P, psB[:, 0:288], psB, [(xs_(k), wbi[:, k, 384:672], [XN, wbi]) for k in range(8)])
            P.op("act", lambda e: e.activation(out=SQ[:, 0:384], in_=psA[:, 0:384], func=AF.Square), reads=[psA], writes=[SQ])
            P.op("dve", lambda e: e.reduce_sum(out=SSA[:, 0:1], in_=SQ[:, 0:384], axis=AX.X), reads=[SQ], writes=[SSA])
            P.op("act", lambda e: e.activation(out=SQ[:, 0:288], in_=psB[:, 0:288], func=AF.Square), reads=[psB], writes=[SQ])
            P.op("dve", lambda e: e.reduce_sum(out=SSA[:, 1:2], in_=SQ[:, 0:256], axis=AX.X), reads=[SQ], writes=[SSA], partial=True)
            P.op("dve", lambda e: e.reduce_sum(out=SSA[:, 2:3], in_=SQ[:, 256:288], axis=AX.X), reads=[SQ], writes=[SSA], partial=True)
            P.op("act", lambda e: e.activation(out=SSA[:, 0:1], in_=SSA[:, 0:1], func=AF.Sqrt, scale=1.0 / 384, bias=epsq[:, 0:1]), reads=[SSA, epsq], writes=[SSA], partial=True)
            P.op("act", lambda e: e.activation(out=SSA[:, 1:2], in_=SSA[:, 1:2], func=AF.Sqrt, scale=1.0 / 256, bias=epsq[:, 0:1]), reads=[SSA, epsq], writes=[SSA], partial=True)
            P.op("dve", lambda e: e.reciprocal(out=SSA[:, 0:2], in_=SSA[:, 0:2]), reads=[SSA], writes=[SSA], partial=True)
            P.op("dve", lambda e: e.scalar_tensor_tensor(out=CN[:, 0:384], in0=psA[:, 0:384], scalar=SSA[:, 0:1], in1=gqa, op0=ALU.mult, op1=ALU.mult),
                 reads=[psA, SSA, grow], writes=[CN])
            P.op("dve", lambda e: e.scalar_tensor_tensor(out=CN[:, 384:640], in0=psB[:, 0:256], scalar=SSA[:, 1:2], in1=gkva, op0=ALU.mult, op1=ALU.mult),
                 reads=[psB, SSA, grow], writes=[CN], partial=True)
            P.op("act", lambda e: e.copy(out=KRF[:], in_=psB[:, 256:288]), reads=[psB], writes=[KRF])
            if B1CUT <= 1:
                continue
            pt = ptr[itr % 2]
            itr += 1
            for i in range(4):
                P.op("pe", lambda e, i=i: e.matmul(pt[:, i * 128:(i + 1) * 128], lhsT=CN[:, i * 128:(i + 1) * 128], rhs=ident[:], start=True, stop=True),
                     reads=[CN, ident], writes=[pt], signal=(i == 3))
            P.op("act", lambda e: e.copy(out=CT[:, 0:4, :], in_=v3(pt[:, :], 128)), reads=[pt], writes=[CT])
            pt = ptr[itr % 2]
            itr += 1
            P.op("pe", lambda e: e.matmul(pt[:, 0:128], lhsT=CN[:, 512:640], rhs=ident[:], start=True, stop=True), reads=[CN, ident], writes=[pt])
            P.op("act", lambda e: e.copy(out=CT[:, 4, :], in_=pt[:, 0:128]), reads=[pt], writes=[CT], partial=True)
            if B1CUT <= 2:
                continue
            cs, sn = cos[:, j, :], sin[:, j, :]
            for o, g0 in ((0, GQ0), (4, GK0)):
                ga, gb = grow[:, g0 + 64:g0 + 80], grow[:, g0 + 80:g0 + 96]
                for idx, (gg, tt) in enumerate(((ga, cs), (gb, sn), (gb, cs), (ga, sn))):
                    P.op("pool", lambda e, o=o, idx=idx, gg=gg, tt=tt: e.tensor_tensor(out=GT[:, o + idx, :], in0=gg, in1=tt, op=ALU.mult),
                         reads=[grow, cos, sin], writes=[GT], partial=not (o == 0 and idx == 0))
            if B1CUT <= 3:
                continue
            for hb in range(4):
                ps = pq[ipq % 3]
                ipq += 1
                mm_group(P, ps[:, 0:384], ps, [(CT[:, kk, :], wqu[:, kk, hb * 384:(hb + 1) * 384], [CT, wqu]) for kk in range(3)])
                P.op("act", lambda e, hb=hb: e.copy(out=QF[:, hb * 384:(hb + 1) * 384], in_=ps[:, 0:384]), reads=[ps], writes=[QF], partial=(hb > 0))
                P.op("act", lambda e: e.activation(out=SQ[:, 0:384], in_=ps[:, 0:384], func=AF.Square), reads=[ps], writes=[SQ])
                P.op("dve", lambda e, hb=hb: e.reduce_sum(out=SS16[:, hb * 4:(hb + 1) * 4], in_=v3(SQ[:, 0:384], 96), axis=AX.X), reads=[SQ], writes=[SS16],
                     partial=(hb > 0))
            for hb in range(4):
                ps = pq[ipq % 3]
                ipq += 1
                mm_group(P, ps[:, :], ps, [(CT[:, 3 + kk, :], wkvu[:, kk, hb * 512:(hb + 1) * 512], [CT, wkvu]) for kk in range(2)])
                P.op("act", lambda e, hb=hb: e.copy(out=KVF[:, hb * 512:(hb + 1) * 512], in_=ps[:, :]), reads=[ps], writes=[KVF], partial=(hb > 0))
                P.op("act", lambda e: e.activation(out=v3(SQ[:, 0:256], 64), in_=v3(ps[:, :], 128)[:, :, 0:64], func=AF.Square), reads=[ps], writes=[SQ])
                P.op("dve", lambda e, hb=hb: e.reduce_sum(out=SS16[:, 16 + hb * 4:16 + (hb + 1) * 4], in_=v3(SQ[:, 0:256], 64), axis=AX.X), reads=[SQ], writes=[SS16],
                     partial=True)
            if B1CUT <= 4:
                continue
            P.op("act", lambda e: e.activation(out=SS16[:, 0:16], in_=SS16[:, 0:16], func=AF.Sqrt, scale=1.0 / 96, bias=epsq[:, 0:1]), reads=[SS16, epsq], writes=[SS16], partial=True)
            P.op("dve", lambda e: e.tensor_scalar(out=SS16[:, 16:32], in0=SS16[:, 16:32], scalar1=SSA[:, 2:3], scalar2=None, op0=ALU.add), reads=[SS16, SSA], writes=[SS16], partial=True)
            P.op("act", lambda e: e.activation(out=SS16[:, 16:32], in_=SS16[:, 16:32], func=AF.Sqrt, bias=epsq[:, 1:2]), reads=[SS16, epsq], writes=[SS16], partial=True)
            P.op("dve", lambda e: e.reciprocal(out=SS16[:, 0:16], in_=SS16[:, 0:16]), reads=[SS16], writes=[SS16], partial=True)
            P.op("dve", lambda e, j=j: e.reciprocal(out=rk_all[:, j, :], in_=SS16[:, 16:32]), reads=[SS16], writes=[rk_all], partial=True)
            if B1CUT <= 5:
                continue
            QF3, TQ3 = v3(QF[:, :], 96), v3(TQ[:, :], 96)
            P.op("dve", lambda e: e.tensor_tensor(out=TQ3[:, :, 0:64], in0=QF3[:, :, 0:64], in1=grow[:, GQ0:GQ0 + 64].unsqueeze(1).to_broadcast([128, 16, 64]), op=ALU.mult),
                 reads=[QF, grow], writes=[TQ])
            x1, x2 = QF3[:, :, 64:80], QF3[:, :, 80:96]
            bc = lambda i: GT[:, i, :].unsqueeze(1).to_broadcast([128, 16, 16])
            r3 = [v3(r_[:, :], 16) for r_ in rt]
            P.op("dve", lambda e: e.tensor_tensor(out=r3[0], in0=x1, in1=bc(0), op=ALU.mult), reads=[QF, GT], writes=[rt[0]])
            P.op("dve", lambda e: e.tensor_tensor(out=r3[1], in0=x2, in1=bc(1), op=ALU.mult), reads=[QF, GT], writes=[rt[1]])
            P.op("dve", lambda e: e.tensor_tensor(out=r3[2], in0=x2, in1=bc(2), op=ALU.mult), reads=[QF, GT], writes=[rt[2]])
            P.op("dve", lambda e: e.tensor_tensor(out=r3[3], in0=x1, in1=bc(3), op=ALU.mult), reads=[QF, GT], writes=[rt[3]])
            P.op("pool", lambda e: e.tensor_tensor(out=TQ3[:, :, 64:80], in0=r3[0], in1=r3[1], op=ALU.subtract), reads=[rt[0], rt[1]], writes=[TQ], partial=True)
            P.op("pool", lambda e: e.tensor_tensor(out=TQ3[:, :, 80:96], in0=r3[2], in1=r3[3], op=ALU.add), reads=[rt[2], rt[3]], writes=[TQ], partial=True)
            P.op("dve", lambda e: e.tensor_tensor(out=v3(QB[:, :], 96), in0=TQ3, in1=SS16[:, 0:16].unsqueeze(2).to_broadcast([128, 16, 96]), op=ALU.mult),
                 reads=[TQ, SS16], writes=[QB])
            if B1CUT <= 6:
                continue
            KV3, KB3 = v3(KVF[:, :], 128), v3(KB[:, :], 96)
            P.op("pool", lambda e: e.tensor_tensor(out=KB3[:, :, 0:64], in0=KV3[:, :, 0:64], in1=grow[:, GK0:GK0 + 64].unsqueeze(1).to_broadcast([128, 16, 64]), op=ALU.mult),
                 reads=[KVF, grow], writes=[KB])
            k1, k2 = KRF[:, 0:16], KRF[:, 16:32]
            P.op("dve", lambda e: e.tensor_tensor(out=rt[0][:, 0:16], in0=k1, in1=GT[:, 4, :], op=ALU.mult), reads=[KRF, GT], writes=[rt[0]])
            P.op("dve", lambda e: e.tensor_tensor(out=rt[1][:, 0:16], in0=k2, in1=GT[:, 5, :], op=ALU.mult), reads=[KRF, GT], writes=[rt[1]])
            P.op("dve", lambda e: e.tensor_tensor(out=rt[2][:, 0:16], in0=k2, in1=GT[:, 6, :], op=ALU.mult), reads=[KRF, GT], writes=[rt[2]])
            P.op("dve", lambda e: e.tensor_tensor(out=rt[3][:, 0:16], in0=k1, in1=GT[:, 7, :], op=ALU.mult), reads=[KRF, GT], writes=[rt[3]])
            P.op("pool", lambda e: e.tensor_tensor(out=KRR[:, 0:16], in0=rt[0][:, 0:16], in1=rt[1][:, 0:16], op=ALU.subtract), reads=[rt[0], rt[1]], writes=[KRR])
            P.op("pool", lambda e: e.tensor_tensor(out=KRR[:, 16:32], in0=rt[2][:, 0:16], in1=rt[3][:, 0:16], op=ALU.add), reads=[rt[2], rt[3]], writes=[KRR], partial=True)
            P.op("pool", lambda e: e.tensor_copy(out=KB3[:, :, 64:96], in_=KRR[:, :].unsqueeze(1).to_broadcast([128, 16, 32])), reads=[KRR], writes=[KB], partial=True)
            P.op("act", lambda e: e.copy(out=v3(VB[:, :], 64), in_=KV3[:, :, 64:128]), reads=[KVF], writes=[VB])
            P.dma("sp", C["b_v"][j * 128:(j + 1) * 128, :], VB[:], reads=[VB], writes=[C["b_v_buf"]], partial=True)
            if B1CUT <= 7:
                continue
            for src, base in ((QB, 0), (KB, 16)):
                for b4 in range(4):
                    pt = ptr[itr % 2]
                    itr += 1
                    for i in range(4):
                        hh = b4 * 4 + i
                        P.op("pe", lambda e, hh=hh, i=i, src=src: e.matmul(pt[0:96, i * 128:(i + 1) * 128], lhsT=src[:, hh * 96:(hh + 1) * 96], rhs=ident[:], start=True, stop=True),
                             reads=[src, ident], writes=[pt], signal=(i == 3))
                    P.op("act", lambda e, b4=b4, base=base: e.copy(out=QT[0:96, base + b4 * 4:base + (b4 + 1) * 4, :], in_=v3(pt[0:96, :], 128)),
                         reads=[pt], writes=[QT], partial=not (base == 0 and b4 == 0))
            for q4 in range(4):
                P.dma("sp", qkT_v[:, q4 * 8:(q4 + 1) * 8, j * 128:(j + 1) * 128], QT[0:96, q4 * 8:(q4 + 1) * 8, :], reads=[QT], writes=[C["b_qkT_buf"]], partial=True)
        if t + 1 < NT:
            N.run(xt, xn[(t + 1) % 2])
    P.end_stage()


def stage_b2(P, C, rk_all):
    P.begin_stage()
    mask = P.sb([128, 4, 512], BF16, "mask")
    P.dma("pool", mask[:], C["b_mask"], writes=[mask])
    ones = P.sb([128, 64], BF16, "ones64")
    P.op("dve", lambda e: e.memset(ones[:], 1.0), writes=[ones])
    V = P.sb([128, 16, 1024], BF16, "V")
    qT = [P.sb([128, 2048], BF16, "qT") for _ in range(2)]
    kT = [P.sb([128, 2048], BF16, "kT") for _ in range(2)]
    PT = [P.sb([128, 512], BF16, "PT") for _ in range(3)]
    rden = [P.sb([128, 512], F32, "rden") for _ in range(2)]
    oTs = [P.sb([128, 2048], BF16, "oTs") for _ in range(2)]
    ps_s = [P.ps(name="ps_s") for _ in range(2)]
    psn = [P.ps(name="psn") for _ in range(2)]
    psd = [P.ps(name="psd") for _ in range(2)]
    ist = 0
    iq = 0

    def load_head(s, h, i):
        P.dma("sp", qT[i][0:96, :], C["b_qkT"][h, :, s * 2048:(s + 1) * 2048], reads=[C["b_qkT_buf"]], writes=[qT[i]])
        P.dma("sp", kT[i][0:96, :], C["b_qkT"][16 + h, :, s * 2048:(s + 1) * 2048], reads=[C["b_qkT_buf"]], writes=[kT[i]])

    for s in range(2):
        P.dma("sp", V[:], C["b_v"][s * 2048:(s + 1) * 2048, :].rearrange("(j p) n -> p j n", p=128), reads=[C["b_v_buf"]], writes=[V])
        load_head(s, 0, 0)
        for h in range(16):
            QT, KT = qT[h % 2], kT[h % 2]
            if h + 1 < 16:
                load_head(s, h + 1, (h + 1) % 2)
            e0 = (h % 2) * 64
            OT = oTs[(h // 2) % 2]
            for qb in range(4):
                pn, pd = psn[iq % 2], psd[iq % 2]
                RD = rden[iq % 2]
                iq += 1
                nj = 4 * qb + 4
                for j in range(nj):
                    pss = ps_s[ist % 2]
                    pt_ = PT[ist % 3]
                    ist += 1
                    P.op("pe", lambda e, j=j, qb=qb: e.matmul(pss[:, :], lhsT=KT[0:96, j * 128:(j + 1) * 128], rhs=QT[0:96, qb * 512:(qb + 1) * 512], start=True, stop=True),
                         reads=[KT, QT], writes=[pss])
                    P.op("act", lambda e, j=j, s=s, h=h: e.activation(out=pt_[:], in_=pss[:, :], func=AF.Exp, scale=rk_all[:, s * 16 + j, h:h + 1]),
                         reads=[pss, rk_all], writes=[pt_])
                    if j >= 4 * qb:
                        P.op("dve", lambda e, j=j, qb=qb: e.tensor_tensor(out=pt_[:], in0=pt_[:], in1=mask[:, j - 4 * qb, :], op=ALU.mult), reads=[pt_, mask], writes=[pt_])
                    P.op("pe", lambda e, j=j, h=h: e.matmul(pn[e0:e0 + 64, :], lhsT=V[:, j, h * 64:(h + 1) * 64], rhs=pt_[:], start=(j == 0), stop=(j == nj - 1)),
                         reads=[V, pt_], writes=[pn], signal=(j == nj - 1))
                    P.op("pe", lambda e, j=j: e.matmul(pd[e0:e0 + 64, :], lhsT=ones[:], rhs=pt_[:], start=(j == 0), stop=(j == nj - 1)),
                         reads=[ones, pt_], writes=[pd], signal=True)
                P.op("dve", lambda e: e.reciprocal(out=RD[e0:e0 + 64, :], in_=pd[e0:e0 + 64, :]), reads=[pd], writes=[RD])
                P.op("dve", lambda e, qb=qb: e.tensor_tensor(out=OT[e0:e0 + 64, qb * 512:(qb + 1) * 512], in0=pn[e0:e0 + 64, :], in1=RD[e0:e0 + 64, :], op=ALU.mult),
                     reads=[pn, RD], writes=[OT], partial=not (h % 2 == 0 and qb == 0))
            if h % 2 == 1:
                P.dma("sp", C["oT"][h // 2, :, s * 2048:(s + 1) * 2048], OT[:], reads=[OT], writes=[C["oT_buf"]], partial=True)
    P.end_stage()


def stage_mix_b(P, C, S, D, li):
    P.begin_group()
    cos = P.sb([128, 32, 16], F32, "cos")
    sin = P.sb([128, 32, 16], F32, "sin")
    rk_all = P.sb([128, 32, 16], F32, "rk_all")
    stage_tables(P, C, "pos_pm", "b_inv", 32, 16, cos, sin)
    import os
    dbg = os.environ.get("BDBG", "123")
    if "1" in dbg:
        stage_b1(P, C, S, li, cos, sin, rk_all)
    if "2" in dbg:
        stage_b2(P, C, rk_all)
    if "3" in dbg:
        stage_outproj(P, C, S, D, C["b_w_out"][0], 8)
    P.end_group()


def stage_a1(P, C, S, li, cos, sin):
    TT = 512
    P.begin_stage()
    w_d = C["a_w_in"][0].rearrange("(k p) n -> p k n", p=128)
    wa = [P.sb([128, 8, 512], BF16, "wa") for _ in range(6)]
    N = NormUnit(P, TT, C["gains"][li, 1])
    ident = P.sb([128, 128], BF16, "ident")
    P.dma("pool", ident[:], C["ident"], writes=[ident])
    grow = P.sb([128, 128], F32, "grow")
    P.dma("sp", grow[:], C["a_grow"], writes=[grow])
    epsq = P.sb([128, 2], F32, "epsq")
    P.op("dve", lambda e: e.memset(epsq[:, 0:1], 1e-6), writes=[epsq])
    P.op("dve", lambda e: e.memset(epsq[:, 1:2], 64e-6), writes=[epsq], partial=True)
    xt = P.sb([128, 8, TT], F32, "xt")
    xn = P.sb([128, 8, SEQ], BF16, "xnseq")
    xn_parts = [P.sb([128, 8, TT], BF16, "xnp") for _ in range(2)]
    sq = [P.sb([128, 512], F32, "sq") for _ in range(2)]
    SSb = [P.sb([128, 32], F32, "ss") for _ in range(2)]
    Tb = [P.sb([128, 2048], F32, "T") for _ in range(2)]
    GTb = [P.sb([128, 8, 32], F32, "gt") for _ in range(2)]
    rt = [P.sb([128, 256], F32, "rt") for _ in range(8)]
    qkb = [P.sb([128, 2048], BF16, "qkb") for _ in range(2)]
    vb = [P.sb([128, 1024], BF16, "vb") for _ in range(2)]
    qTs = [P.sb([128, 16, 128], BF16, "qTs") for _ in range(2)]
    pp = [P.ps(name="pp") for _ in range(4)]
    ptr = [P.ps(name="ptr") for _ in range(2)]
    ipp = 0
    itr = 0
    irt = 0
    jt = 0
    for s in range(2):
        for t4 in range(4):
            load_x(P, S, s * SEQ + t4 * TT, TT, xt)
            XP = xn_parts[t4 % 2]
            N.run(xt, XP)
            P.op("pool", lambda e, t4=t4, XP=XP: e.tensor_copy(out=xn[:, :, t4 * TT:(t4 + 1) * TT], in_=XP[:]), reads=[XP], writes=[xn], partial=(t4 > 0))
        for g in range(3):
            for c6 in range(6):
                col = (c6 // 2) * 3072 + g * 1024 + (c6 % 2) * 512
                P.dma("pool", wa[c6][:], w_d[:, :, col:col + 512], writes=[wa[c6]])
            qkT_v = C["a_qkT"][s * 3 + g].rearrange("h d t -> d h t")
            for ti in range(16):
                b = jt % 2
                jt += 1
                SS, T, GT, QKB, VB, QT = SSb[b], Tb[b], GTb[b], qkb[b], vb[b], qTs[b]
                tix = (s * 3 + g) * 16 + ti
                st, dil, nb = a_tile_start(g, ti)
                tok = slice(st, st + 127 * dil + 1, dil)
                cs, sn = cos[:, tix, :], sin[:, tix, :]
                for o, g0 in ((0, 0), (4, 64)):
                    ga, gb = grow[:, g0:g0 + 32], grow[:, g0 + 32:g0 + 64]
                    for idx, (gg, tt) in enumerate(((ga, cs), (gb, sn), (gb, cs), (ga, sn))):
                        P.op("pool", lambda e, o=o, idx=idx, gg=gg, tt=tt: e.tensor_tensor(out=GT[:, o + idx, :], in0=gg, in1=tt, op=ALU.mult),
                             reads=[grow, cos, sin], writes=[GT], partial=not (o == 0 and idx == 0))
                for c6 in range(6):
                    ps = pp[ipp % 4]
                    ipp += 1
                    mm_group(P, ps[:, :], ps, [(xn[:, k, tok], wa[c6][:, k, :], [xn, wa[c6]]) for k in range(8)])
                    if c6 >= 4:
                        P.op("act", lambda e, c6=c6: e.copy(out=VB[:, (c6 - 4) * 512:(c6 - 3) * 512], in_=ps[:, :]), reads=[ps], writes=[VB], partial=(c6 == 5))
                        continue
                    SQ = sq[c6 % 2]
                    P.op("act", lambda e: e.activation(out=SQ[:], in_=ps[:, :], func=AF.Square), reads=[ps], writes=[SQ])
                    P.op("dve", lambda e, c6=c6: e.reduce_sum(out=SS[:, c6 * 8:(c6 + 1) * 8], in_=SQ[:, :].rearrange("p (h d) -> p h d", d=64), axis=AX.X),
                         reads=[SQ], writes=[SS], partial=(c6 > 0))
                    o = 0 if c6 < 2 else 4
                    ps3 = ps[:, :].rearrange("p (h d) -> p h d", d=64)
                    x1, x2 = ps3[:, :, 0:32], ps3[:, :, 32:64]
                    bc = lambda i: GT[:, o + i, :].unsqueeze(1).to_broadcast([128, 8, 32])
                    r = [rt[(irt + i) % 8] for i in range(4)]
                    irt += 4
                    r3 = [r_[:, :].rearrange("p (h d) -> p h d", d=32) for r_ in r]
                    P.op("dve", lambda e: e.tensor_tensor(out=r3[0], in0=x1, in1=bc(0), op=ALU.mult), reads=[ps, GT], writes=[r[0]])
                    P.op("dve", lambda e: e.tensor_tensor(out=r3[1], in0=x2, in1=bc(1), op=ALU.mult), reads=[ps, GT], writes=[r[1]])
                    P.op("dve", lambda e: e.tensor_tensor(out=r3[2], in0=x2, in1=bc(2), op=ALU.mult), reads=[ps, GT], writes=[r[2]])
                    P.op("dve", lambda e: e.tensor_tensor(out=r3[3], in0=x1, in1=bc(3), op=ALU.mult), reads=[ps, GT], writes=[r[3]])
                    T3 = T[:, c6 * 512:(c6 + 1) * 512].rearrange("p (h d) -> p h d", d=64)
                    P.op("pool", lambda e: e.tensor_tensor(out=T3[:, :, 0:32], in0=r3[0], in1=r3[1], op=ALU.subtract), reads=[r[0], r[1]], writes=[T], partial=(c6 > 0))
                    P.op("pool", lambda e: e.tensor_tensor(out=T3[:, :, 32:64], in0=r3[2], in1=r3[3], op=ALU.add), reads=[r[2], r[3]], writes=[T], partial=True)
                P.op("act", lambda e: e.activation(out=SS[:, 0:16], in_=SS[:, 0:16], func=AF.Sqrt, scale=1.0 / 64, bias=epsq[:, 0:1]), reads=[SS, epsq], writes=[SS], partial=True)
                P.op("act", lambda e: e.activation(out=SS[:, 16:32], in_=SS[:, 16:32], func=AF.Sqrt, bias=epsq[:, 1:2]), reads=[SS, epsq], writes=[SS], partial=True)
                P.op("dve", lambda e: e.reciprocal(out=SS[:, :], in_=SS[:, :]), reads=[SS], writes=[SS], partial=True)
                v64 = lambda ap: ap.rearrange("p (h d) -> p h d", d=64)
                P.op("dve", lambda e: e.tensor_tensor(out=v64(QKB[:, 0:1024]), in0=v64(T[:, 0:1024]), in1=SS[:, 0:16].unsqueeze(2).to_broadcast([128, 16, 64]), op=ALU.mult),
                     reads=[T, SS], writes=[QKB])
                P.op("pool", lambda e: e.tensor_tensor(out=v64(QKB[:, 1024:2048]), in0=v64(T[:, 1024:2048]), in1=SS[:, 16:32].unsqueeze(2).to_broadcast([128, 16, 64]), op=ALU.mult),
                     reads=[T, SS], writes=[QKB], partial=True)
                P.dma("sp", C["a_v"][s * 3 + g, ti * 128:(ti + 1) * 128, :], VB[:], reads=[VB], writes=[C["a_v_buf"]], partial=True)
                for b4 in range(4):
                    pt = ptr[itr % 2]
                    itr += 1
                    for i in range(4):
                        hh = b4 * 4 + i
                        P.op("pe", lambda e, hh=hh, i=i: e.matmul(pt[:, i * 128:(i + 1) * 128], lhsT=QKB[:, hh * 128:(hh + 1) * 128], rhs=ident[:], start=True, stop=True),
                             reads=[QKB, ident], writes=[pt], signal=(i == 3))
                    P.op("act", lambda e, b4=b4: e.copy(out=QT[:, b4 * 4:(b4 + 1) * 4, :], in_=pt[:, :].rearrange("p (h t) -> p h t", t=128)),
                         reads=[pt], writes=[QT], partial=(b4 > 0))
                for q2 in range(2):
                    P.dma("sp", qkT_v[:, q2 * 8:(q2 + 1) * 8, ti * 128:(ti + 1) * 128], QT[:, q2 * 8:(q2 + 1) * 8, :], reads=[QT], writes=[C["a_qkT_buf"]], partial=True)
    P.end_stage()


def stage_a2(P, C):
    P.begin_stage()
    mask = P.sb([128, 256], BF16, "mask")
    P.dma("pool", mask[:], C["a_mask"], writes=[mask])
    ones = P.sb([128, 64], BF16, "ones64")
    P.op("dve", lambda e: e.memset(ones[:], 1.0), writes=[ones])
    qT = [[P.sb([64, 2048], BF16, "qT") for _ in range(2)] for _ in range(2)]
    kT = [[P.sb([64, 2048], BF16, "kT") for _ in range(2)] for _ in range(2)]
    Vp = [P.sb([128, 16, 128], BF16, "Vp") for _ in range(2)]
    PT = [P.sb([128, 256], BF16, "PT") for _ in range(3)]
    accn = [P.sb([128, 2048], F32, "accn") for _ in range(2)]
    accd = [P.sb([128, 2048], F32, "accd") for _ in range(2)]
    oTs = [P.sb([128, 2048], BF16, "oTs") for _ in range(2)]
    ps_s = [P.ps(name="ps_s") for _ in range(2)]
    psn = [P.ps(name="psn") for _ in range(2)]
    psd = [P.ps(name="psd") for _ in range(2)]
    ist = 0
    iq = 0
    units = [(s, hp, g) for s in range(2) for hp in range(8) for g in range(3)]

    def load_unit(u, i):
        s, hp, g = u
        sg = s * 3 + g
        for e_ in range(2):
            P.dma("sp", qT[i][e_][:], C["a_qkT"][sg, hp, e_ * 64:(e_ + 1) * 64, :], reads=[C["a_qkT_buf"]], writes=[qT[i][e_]])
            P.dma("sp", kT[i][e_][:], C["a_qkT"][sg, 8 + hp, e_ * 64:(e_ + 1) * 64, :], reads=[C["a_qkT_buf"]], writes=[kT[i][e_]])
        vsrc = C["a_v"][sg, :, hp * 128:(hp + 1) * 128].rearrange("(j p) n -> p j n", p=128)
        for h2 in range(2):
            P.dma("sp", Vp[i][:, h2 * 8:(h2 + 1) * 8, :], vsrc[:, h2 * 8:(h2 + 1) * 8, :], reads=[C["a_v_buf"]], writes=[Vp[i]], partial=(h2 > 0))

    load_unit(units[0], 0)
    for ui, (s, hp, g) in enumerate(units):
        i = ui % 2
        if ui + 1 < len(units):
            load_unit(units[ui + 1], (ui + 1) % 2)
        AN, AD = accn[(ui // 3) % 2], accd[(ui // 3) % 2]
        dil = A_DIL[g]
        nb = 16 // dil
        V = Vp[i]
        for e_ in range(2):
            e0 = e_ * 64
            QT, KT = qT[i][e_], kT[i][e_]
            for qb in range(4):
                pn, pd = psn[iq % 2], psd[iq % 2]
                iq += 1
                for blk in range(4):
                    tq = 4 * qb + blk
                    has_prev = (tq % nb) != 0
                    pss = ps_s[ist % 2]
                    pt_ = PT[ist % 3]
                    ist += 1
                    c0 = 0 if has_prev else 128
                    if has_prev:
                        P.op("pe", lambda e, tq=tq: e.matmul(pss[:, 0:128], lhsT=KT[:, (tq - 1) * 128:tq * 128], rhs=QT[:, tq * 128:(tq + 1) * 128], start=True, stop=True),
                             reads=[KT, QT], writes=[pss], signal=False)
                    P.op("pe", lambda e, tq=tq: e.matmul(pss[:, 128:256], lhsT=KT[:, tq * 128:(tq + 1) * 128], rhs=QT[:, tq * 128:(tq + 1) * 128], start=True, stop=True),
                         reads=[KT, QT], writes=[pss], partial=True)
                    P.op("act", lambda e, c0=c0: e.activation(out=pt_[:, c0:256], in_=pss[:, c0:256], func=AF.Exp), reads=[pss], writes=[pt_])
                    P.op("dve", lambda e, c0=c0: e.tensor_tensor(out=pt_[:, c0:256], in0=pt_[:, c0:256], in1=mask[:, c0:256], op=ALU.mult), reads=[pt_, mask], writes=[pt_])
                    osl = slice(blk * 128, (blk + 1) * 128)
                    if has_prev:
                        P.op("pe", lambda e, tq=tq, osl=osl: e.matmul(pn[e0:e0 + 64, osl], lhsT=V[:, tq - 1, e0:e0 + 64], rhs=pt_[:, 0:128], start=True, stop=False),
                             reads=[V, pt_], writes=[pn], signal=False, partial=True)
                    P.op("pe", lambda e, tq=tq, osl=osl, hp_=has_prev: e.matmul(pn[e0:e0 + 64, osl], lhsT=V[:, tq, e0:e0 + 64], rhs=pt_[:, 128:256], start=(not hp_), stop=True),
                         reads=[V, pt_], writes=[pn], signal=False, partial=True)
                    if has_prev:
                        P.op("pe", lambda e, osl=osl: e.matmul(pd[e0:e0 + 64, osl], lhsT=ones[:], rhs=pt_[:, 0:128], start=True, stop=False),
                             reads=[ones, pt_], writes=[pd], signal=False, partial=True)
                    P.op("pe", lambda e, osl=osl, hp_=has_prev: e.matmul(pd[e0:e0 + 64, osl], lhsT=ones[:], rhs=pt_[:, 128:256], start=(not hp_), stop=True),
                         reads=[ones, pt_], writes=[pd], signal=True, partial=True)
                for acc, src in ((AN, pn), (AD, pd)):
                    if g == 0:
                        dst = acc[e0:e0 + 64, qb * 512:(qb + 1) * 512]
                        sv = src[e0:e0 + 64, :]
                        P.op("act", lambda e, dst=dst, sv=sv: e.copy(out=dst, in_=sv), reads=[src], writes=[acc], partial=True)
                    else:
                        if g == 1:
                            dst = acc[e0:e0 + 64, :].rearrange("p (l r) -> p r l", r=4)[:, qb, :]
                            sv = src[e0:e0 + 64, :]
                        else:
                            dst = acc[e0:e0 + 64, :].rearrange("p (u r) -> p r u", r=16)[:, 4 * qb:4 * qb + 4, :]
                            sv = src[e0:e0 + 64, :].rearrange("p (b u) -> p b u", u=128)
                        P.op("dve", lambda e, dst=dst, sv=sv: e.tensor_tensor(out=dst, in0=dst, in1=sv, op=ALU.add), reads=[src, acc], writes=[acc], partial=True)
        if g == 2:
            OT = oTs[(ui // 3) % 2]
            P.op("dve", lambda e: e.reciprocal(out=AD[:], in_=AD[:]), reads=[AD], writes=[AD])
            P.op("dve", lambda e: e.tensor_tensor(out=OT[:], in0=AN[:], in1=AD[:], op=ALU.mult), reads=[AN, AD], writes=[OT])
            P.dma("sp", C["oT"][hp, :, s * 2048:(s + 1) * 2048], OT[:], reads=[OT, AN, AD], writes=[C["oT_buf"]], partial=True)
    P.end_stage()


def stage_mix_a(P, C, S, D, li):
    import os
    dbg = os.environ.get("ADBG", "123")
    P.begin_group()
    cos = P.sb([128, 96, 32], F32, "cos")
    sin = P.sb([128, 96, 32], F32, "sin")
    stage_tables(P, C, "pos_a", "a_inv", 96, 32, cos, sin)
    if "1" in dbg:
        stage_a1(P, C, S, li, cos, sin)
    if "2" in dbg:
        stage_a2(P, C)
    if "3" in dbg:
        stage_outproj(P, C, S, D, C["a_w_out"][0], 8)
    P.end_group()


MIXERS[3] = stage_mix_d
MIXERS[2] = stage_mix_c
MIXERS[1] = stage_mix_b
MIXERS[0] = stage_mix_a


FULL_STAGES = []
for _li in range(4):
    FULL_STAGES += [("ffn", _li, 0), ("mix", _li), ("ffn", _li, 1)]

WEIGHT_SPECS = [
    ("ffn_w_in", [4, 2, 1024, 5632]), ("ffn_w_out", [4, 2, 2816, 1024]),
    ("a_w_in", [1, 1024, 9216]), ("a_w_out", [1, 1024, 1024]),
    ("b_w_in", [1, 1024, 672]), ("b_w_q_up", [1, 384, 1536]), ("b_w_kv_up", [1, 256, 2048]), ("b_w_out", [1, 1024, 1024]),
    ("c_w_in", [1, 1024, 6144]), ("c_w_out", [1, 2048, 1024]),
    ("d_w_in", [1, 1024, 2048]), ("d_w_rg", [1, 4, 256, 256]), ("d_w_ig", [1, 4, 256, 256]), ("d_w_out", [1, 1024, 1024]),
]


def build_program(stages):
    P = Prog()
    nc = P.nc
    C = {}
    x_in = nc.dram_tensor("x", [DM, NTOK], F32, kind="ExternalInput").ap()
    out = nc.dram_tensor("out", [DM, NTOK], F32, kind="ExternalOutput").ap()
    xs = nc.dram_tensor("xs", [DM, NTOK], F32, kind="Internal").ap()
    C["positions"] = nc.dram_tensor("positions", [2, SEQ], I32, kind="ExternalInput").ap()
    C["gains"] = nc.dram_tensor("gains", [4, 3, 128, 8], F32, kind="ExternalInput").ap()
    for name, shape in WEIGHT_SPECS:
        C[name] = nc.dram_tensor(name, shape, F32, kind="ExternalInput").ap()
    for name, shape in small_specs():
        C[name] = nc.dram_tensor(name, shape, F32, kind="ExternalInput").ap()
    for name, shape in int_specs():
        C[name] = nc.dram_tensor(name, shape, I32, kind="ExternalInput").ap()
    for name, shape in scratch_specs():
        C[name] = nc.dram_tensor(name, shape, BF16, kind="Internal").ap()
        C[name + "_buf"] = P.dbuf(C[name], name)
    Sx, So, Ss = Stream(P, x_in), Stream(P, out), Stream(P, xs)
    n = len(stages)
    for i, st in enumerate(stages):
        S = Sx if i == 0 else Ss
        D = So if i == n - 1 else Ss
        if st[0] == "ffn":
            stage_ffn(P, C, S, D, st[1], st[2])
        else:
            MIXERS[st[1] % 4](P, C, S, D, st[1])
    P.finish(So.bufs)
    return P


def host_inputs(inputs, core):
    m = {}
    xc = np.asarray(inputs["x"][2 * core:2 * core + 2], dtype=np.float32).reshape(NTOK, DM)
    m["x"] = np.ascontiguousarray(xc.T)
    m["positions"] = np.ascontiguousarray(np.asarray(inputs["positions"][2 * core:2 * core + 2], dtype=np.int32))
    g = np.asarray(inputs["norm_gains"], dtype=np.float32).reshape(4, 3, 8, 128)
    m["gains"] = np.ascontiguousarray(g.transpose(0, 1, 3, 2))
    for name, shape in WEIGHT_SPECS:
        m[name] = np.ascontiguousarray(np.asarray(inputs[name], dtype=np.float32))
    m.update(host_small(inputs))
    m.update(host_pos(m["positions"]))
    return m


_PROG_CACHE = {}


def run_stages(inputs, stages, cores):
    key = tuple(stages)
    if key not in _PROG_CACHE:
        _PROG_CACHE[key] = build_program(stages)
    P = _PROG_CACHE[key]
    shared = None
    in_maps = []
    for c in cores:
        m = host_inputs(inputs, c) if shared is None else dict(shared)
        if shared is None:
            shared = m
        else:
            xc = np.asarray(inputs["x"][2 * c:2 * c + 2], dtype=np.float32).reshape(NTOK, DM)
            m["x"] = np.ascontiguousarray(xc.T)
            m["positions"] = np.ascontiguousarray(np.asarray(inputs["positions"][2 * c:2 * c + 2], dtype=np.int32))
            m.update(host_pos(m["positions"]))
        in_maps.append(m)
    res = run_bass_kernel_spmd(P.nc, in_maps, core_ids=list(range(len(cores))))
    outs = [np.asarray(r["out"]).T.reshape(2, SEQ, DM) for r in res.results]
    return outs


def kernel(**inputs):
    outs = run_stages(inputs, FULL_STAGES, list(range(8)))
    return np.concatenate(outs, axis=0).astype(np.float32)
```

```python
import numpy as np
import concourse.bass as bass
import concourse.mybir as mybir
from concourse.bass_utils import run_bass_kernel_spmd
from contextlib import ExitStack

F32 = mybir.dt.float32
BF16 = mybir.dt.bfloat16
I32 = mybir.dt.int32
AF = mybir.ActivationFunctionType
ALU = mybir.AluOpType
AX = mybir.AxisListType


class DSem:
    __slots__ = ("sem", "cnt")

    def __init__(self, sem):
        self.sem = sem
        self.cnt = 0


class Buf:
    __slots__ = ("t", "writers", "reads", "ds", "name")

    def __init__(self, t, name=""):
        self.t = t
        self.writers = {}
        self.reads = {}
        self.ds = None
        self.name = name

    def __getitem__(self, k):
        return self.t[k]


class Eng:
    def __init__(self, name, h, sem):
        self.name = name
        self.h = h
        self.sem = sem
        self.count = 0
        self.seen = {}


class Prog:
    def __init__(self):
        self.nc = bass.Bass("TRN2", target_bir_lowering=False)
        nc = self.nc
        self.root = ExitStack()
        self.E = {}
        for name, h in (("pe", nc.tensor), ("act", nc.scalar), ("dve", nc.vector),
                        ("pool", nc.gpsimd), ("sp", nc.sync)):
            sem = self.root.enter_context(nc.semaphore("es_" + name))
            self.E[name] = Eng(name, h, sem)
        self.free_ds = []
        self.n_ds = 0
        self.stage = None
        self.group = None
        self.stage_bufs = []
        self.uid = 0
        self.gbufs = []

    def _scope(self):
        if self.stage is not None:
            return self.stage
        if self.group is not None:
            return self.group
        return self.root

    def begin_group(self):
        assert self.group is None and self.stage is None
        self.group = ExitStack()
        self.group_bufs = []

    def end_group(self):
        assert self.stage is None
        for b in self.group_bufs:
            if b.ds is not None:
                self.free_ds.append(b.ds)
                b.ds = None
        self.group.close()
        self.group = None
        self.group_bufs = []

    def sb(self, shape, dtype, name=None):
        self.uid += 1
        name = (name or "t") + "_%d" % self.uid
        t = self._scope().enter_context(self.nc.sbuf_tensor(name, list(shape), dtype))
        b = Buf(t, name)
        if self.stage is not None:
            self.stage_bufs.append(b)
        elif self.group is not None:
            self.group_bufs.append(b)
        return b

    def ps(self, shape=(128, 512), dtype=F32, name=None):
        self.uid += 1
        name = (name or "p") + "_%d" % self.uid
        t = self._scope().enter_context(self.nc.psum_tensor(name, list(shape), dtype))
        b = Buf(t, name)
        self.stage_bufs.append(b)
        return b

    def dram(self, name, shape, dtype, kind="Internal"):
        t = self.nc.dram_tensor(name, list(shape), dtype, kind=kind)
        return t

    def dbuf(self, ap, name=""):
        b = Buf(ap, name)
        self.gbufs.append(b)
        return b

    def _get_ds(self):
        if self.free_ds:
            return self.free_ds.pop()
        self.n_ds += 1
        sem = self.root.enter_context(self.nc.semaphore("ds_%d" % self.n_ds))
        return DSem(sem)

    def _deps(self, eng, reads, writes, is_dma=False, partial=False):
        need = {}

        def add(tok):
            if tok is None:
                return
            sem, val = tok
            k = id(sem)
            if k not in need or need[k][1] < val:
                need[k] = tok

        for b in reads:
            for tok in b.writers.values():
                add(tok)
        for b in writes:
            if not partial:
                for tok in b.writers.values():
                    if is_dma and b.ds is not None and tok[0] is b.ds.sem:
                        continue
                    add(tok)
            for tok in b.reads.values():
                add(tok)
        for k, (sem, val) in need.items():
            if sem is eng.sem:
                if eng.name == "pe":
                    continue
                if eng.count - val >= 2:
                    continue
            if eng.seen.get(k, 0) >= val:
                continue
            eng.h.wait_ge(sem, val)
            eng.seen[k] = val

    def _record(self, tok, reads, writes, partial=False):
        k = id(tok[0])
        for b in reads:
            old = b.reads.get(k)
            if old is None or old[1] < tok[1]:
                b.reads[k] = tok
        for b in writes:
            if partial:
                b.writers[k] = tok
            else:
                b.writers = {k: tok}
                b.reads = {}

    def op(self, en, fn, reads=(), writes=(), signal=True, partial=False):
        eng = self.E[en]
        self._deps(eng, reads, writes, partial=partial)
        ins = fn(eng.h)
        if signal:
            eng.count += 1
            ins.then_inc(eng.sem, 1)
            tok = (eng.sem, eng.count)
        else:
            tok = (eng.sem, eng.count + 1)
        self._record(tok, reads, writes, partial)
        return ins

    def dma(self, q, out_ap, in_ap, reads=(), writes=(), partial=False, **kw):
        eng = self.E[q]
        self._deps(eng, reads, writes, is_dma=True, partial=partial)
        dst = writes[0]
        if dst.ds is None:
            dst.ds = self._get_ds()
        ins = eng.h.dma_start(out=out_ap, in_=in_ap, **kw)
        dst.ds.cnt += 16
        ins.then_inc(dst.ds.sem, 16)
        tok = (dst.ds.sem, dst.ds.cnt)
        self._record(tok, reads, writes, partial)
        return ins

    def begin_stage(self):
        assert self.stage is None
        self.stage = ExitStack()
        self.stage_bufs = []

    def barrier(self, extra_bufs=()):
        toks = {}
        for e in self.E.values():
            if e.count > 0:
                toks[id(e.sem)] = (e.sem, e.count)
        for b in list(self.stage_bufs) + list(getattr(self, 'group_bufs', [])) + list(self.gbufs) + list(extra_bufs):
            if b.ds is not None and b.ds.cnt > 0:
                toks[id(b.ds.sem)] = (b.ds.sem, b.ds.cnt)
            for k, tok in b.reads.items():
                if k not in toks or toks[k][1] < tok[1]:
                    toks[k] = tok
            for k, tok in b.writers.items():
                if k not in toks or toks[k][1] < tok[1]:
                    toks[k] = tok
        for e in self.E.values():
            for k, (sem, val) in toks.items():
                if sem is e.sem:
                    continue
                if e.seen.get(k, 0) >= val:
                    continue
                e.h.wait_ge(sem, val)
                e.seen[k] = val

    def end_stage(self, extra_bufs=()):
        self.barrier(extra_bufs)
        for b in self.stage_bufs:
            if b.ds is not None:
                self.free_ds.append(b.ds)
                b.ds = None
        self.stage.close()
        self.stage = None
        self.stage_bufs = []

    def finish(self, out_bufs):
        self.barrier(out_bufs)


A_DIL = (1, 4, 16)


def a_tile_start(g, ti):
    dil = A_DIL[g]
    nb = 16 // dil
    r, n = ti // nb, ti % nb
    return n * 128 * dil + r, dil, nb


NTOK = 4096
SEQ = 2048
DM = 1024
EPS = 1e-6


class Stream:
    def __init__(self, P, ap, nb=4):
        self.ap = ap
        self.view = ap.rearrange("(k p) t -> p k t", p=128)
        self.nb = nb
        self.bsz = NTOK // nb
        self.bufs = [P.dbuf(ap, "strm") for _ in range(nb)]

    def buf(self, t0):
        return self.bufs[t0 // self.bsz]


class NormUnit:
    def __init__(self, P, TT, g_ap):
        self.P = P
        self.TT = TT
        self.gcol = P.sb([128, 8], F32, "gcol")
        P.dma("sp", self.gcol[:], g_ap, writes=[self.gcol])
        self.ones = P.sb([128, 128], BF16, "ones")
        P.op("dve", lambda e: e.memset(self.ones[:], 1.0), writes=[self.ones])
        self.epsc = P.sb([128, 1], F32, "epsc")
        P.op("dve", lambda e: e.memset(self.epsc[:], EPS), writes=[self.epsc])
        self.sq = [P.sb([128, 8, TT], BF16, "sq") for _ in range(2)]
        self.rs = [P.sb([128, TT], F32, "rs") for _ in range(2)]
        self.pss = P.ps(name="pss")
        self.i = 0

    def run(self, xt, xn):
        P, TT = self.P, self.TT
        sq = self.sq[self.i % 2]
        rs = self.rs[self.i % 2]
        self.i += 1
        pss, ones, gcol = self.pss, self.ones, self.gcol
        P.op("act", lambda e: e.activation(out=sq[:], in_=xt[:], func=AF.Square), reads=[xt], writes=[sq])
        for k in range(8):
            P.op("pe", lambda e, k=k: e.matmul(pss[:, :TT], lhsT=ones[:], rhs=sq[:, k, :], start=(k == 0), stop=(k == 7)),
                 reads=[ones, sq], writes=[pss], signal=(k == 7))
        P.op("act", lambda e: e.activation(out=rs[:], in_=pss[:, :TT], func=AF.Sqrt, scale=1.0 / DM, bias=self.epsc[:, 0:1]),
             reads=[pss, self.epsc], writes=[rs])
        P.op("dve", lambda e: e.reciprocal(out=rs[:], in_=rs[:]), reads=[rs], writes=[rs])
        for k in range(8):
            P.op("dve", lambda e, k=k: e.scalar_tensor_tensor(out=xn[:, k, :], in0=xt[:, k, :], scalar=gcol[:, k:k + 1], in1=rs[:],
                                                               op0=ALU.mult, op1=ALU.mult),
                 reads=[xt, gcol, rs], writes=[xn], partial=(k > 0))


def load_x(P, S, t0, TT, xt):
    P.dma("sp", xt[:], S.view[:, :, t0:t0 + TT], reads=[S.buf(t0)], writes=[xt])


def store_x(P, D, t0, TT, xt):
    P.dma("sp", D.view[:, :, t0:t0 + TT], xt[:], reads=[xt], writes=[D.buf(t0)], partial=True)


def stage_ffn(P, C, S, D, li, fi):
    TT = 256
    NT = NTOK // TT
    P.begin_stage()
    w_in = C["ffn_w_in"][li, fi].rearrange("(k p) n -> p k n", p=128)
    w_out = C["ffn_w_out"][li, fi].rearrange("(k p) n -> p k n", p=128)
    win = [P.sb([128, 8, 512], BF16, "win") for _ in range(11)]
    wout = [P.sb([128, 2, 1024], BF16, "wout") for _ in range(11)]
    N = NormUnit(P, TT, C["gains"][li, 0 if fi == 0 else 2])
    for g in [0, 5, 6, 1, 7, 2, 8, 3, 9, 4, 10]:
        P.dma("pool", win[g][:], w_in[:, :, g * 512:(g + 1) * 512], writes=[win[g]])
    for g in range(11):
        P.dma("pool", wout[g][:], w_out[:, 2 * g:2 * g + 2, :], writes=[wout[g]])
    xt = [P.sb([128, 8, TT], F32, "xt") for _ in range(2)]
    xn = [P.sb([128, 8, TT], BF16, "xn") for _ in range(2)]
    h = P.sb([128, 22, TT], BF16, "h")
    sg = [P.sb([128, TT], F32, "sg") for _ in range(2)]
    pg = [P.ps(name="pg") for _ in range(2)]
    pu = [P.ps(name="pu") for _ in range(2)]
    py = [P.ps(name="py") for _ in range(2)]

    load_x(P, S, 0, TT, xt[0])
    N.run(xt[0], xn[0])
    for t in range(NT):
        X, XN = xt[t % 2], xn[t % 2]
        if t + 1 < NT:
            load_x(P, S, (t + 1) * TT, TT, xt[(t + 1) % 2])
        for m in range(22):
            a, b, s = pg[m % 2], pu[m % 2], sg[m % 2]
            c0 = m * 128
            gi, go = c0 // 512, c0 % 512
            for k in range(8):
                P.op("pe", lambda e, k=k, gi=gi, go=go: e.matmul(a[:, :TT], lhsT=win[gi][:, k, go:go + 128], rhs=XN[:, k, :],
                                                                 start=(k == 0), stop=(k == 7)),
                     reads=[win[gi], XN], writes=[a], signal=(k == 7))
            c0 = 2816 + m * 128
            gi, go = c0 // 512, c0 % 512
            for k in range(8):
                P.op("pe", lambda e, k=k, gi=gi, go=go: e.matmul(b[:, :TT], lhsT=win[gi][:, k, go:go + 128], rhs=XN[:, k, :],
                                                                 start=(k == 0), stop=(k == 7)),
                     reads=[win[gi], XN], writes=[b], signal=(k == 7))
            P.op("act", lambda e: e.activation(out=s[:], in_=a[:, :TT], func=AF.Silu), reads=[a], writes=[s])
            P.op("dve", lambda e, m=m: e.tensor_tensor(out=h[:, m, :], in0=s[:], in1=b[:, :TT], op=ALU.mult),
                 reads=[s, b], writes=[h], partial=(m > 0))
        if t + 1 < NT:
            N.run(xt[(t + 1) % 2], xn[(t + 1) % 2])
        for mo in range(8):
            c = py[mo % 2]
            for kf in range(22):
                P.op("pe", lambda e, kf=kf, mo=mo: e.matmul(c[:, :TT], lhsT=wout[kf // 2][:, kf % 2, mo * 128:(mo + 1) * 128],
                                                            rhs=h[:, kf, :], start=(kf == 0), stop=(kf == 21)),
                     reads=[wout[kf // 2], h], writes=[c], signal=(kf == 21))
            P.op("dve", lambda e, mo=mo: e.scalar_tensor_tensor(out=X[:, mo, :], in0=c[:, :TT], scalar=0.5, in1=X[:, mo, :],
                                                                 op0=ALU.mult, op1=ALU.add),
                 reads=[c, X], writes=[X], partial=True)
        store_x(P, D, t * TT, TT, X)
    P.end_stage()


def small_specs():
    return [("d_small", [128, 8, 8]), ("ident", [128, 128]), ("c_zeta", [128, 8]), ("c_inv", [128, 64]),
            ("c_decT", [128, 8, 128]), ("c_xi", [128, 8, 128]),
            ("b_grow", [128, 832]), ("b_inv", [128, 16]), ("b_mask", [128, 4, 512]),
            ("a_grow", [128, 128]), ("a_inv", [128, 32]), ("a_mask", [128, 256])]


def int_specs():
    return [("pos_pm", [128, 32]), ("pos_a", [128, 96])]


def scratch_specs():
    return [("c_qkT", [16, 128, NTOK]), ("c_kz", [NTOK, 1024]), ("c_vg", [NTOK, 4096]), ("oT", [16, 128, NTOK]),
            ("b_qkT", [32, 96, NTOK]), ("b_v", [NTOK, 1024]),
            ("a_qkT", [6, 16, 128, SEQ]), ("a_v", [6, SEQ, 1024])]


def host_small(inputs):
    m = {}
    f = lambda a: np.asarray(a, dtype=np.float32)
    cols = [f(inputs["d_conv_w"])[0, j] for j in range(4)] + [f(inputs["d_conv_b"])[0], f(inputs["d_b_rg"])[0],
                                                            f(inputs["d_b_ig"])[0], f(inputs["d_lru_param"])[0]]
    ds = np.stack(cols, axis=-1)
    m["d_small"] = np.ascontiguousarray(ds.reshape(8, 128, 8).transpose(1, 0, 2))
    m["ident"] = np.eye(128, dtype=np.float32)
    lg = np.log(np.float32(1.0) - np.float32(2.0) ** (-5.0 - np.arange(8, dtype=np.float32))).astype(np.float32)
    idx = np.arange(128, dtype=np.float32)
    sc = np.float32(128.0 ** -0.5)
    m["c_zeta"] = np.ascontiguousarray(np.exp((127.0 - idx)[:, None] * lg[None, :]).astype(np.float32))
    m["c_inv"] = np.ascontiguousarray(np.broadcast_to((10000.0 ** (-np.arange(64, dtype=np.float32) * 2.0 / 128.0)).astype(np.float32), (128, 64)))
    diff = idx[None, :] - idx[:, None]
    dec = np.where((diff >= 0)[:, None, :], np.exp(np.where(diff >= 0, diff, 0.0)[:, None, :] * lg[None, :, None]), 0.0)
    m["c_decT"] = np.ascontiguousarray((dec * sc).astype(np.float32))
    xi = np.exp((idx + 1.0)[None, :] * lg[:, None]) * sc
    m["c_xi"] = np.ascontiguousarray(np.broadcast_to(xi[None].astype(np.float32), (128, 8, 128)))
    grow = np.concatenate([f(inputs["b_q_a_gain"])[0], f(inputs["b_kv_a_gain"])[0], f(inputs["b_q_gain"])[0], f(inputs["b_k_gain"])[0]])
    m["b_grow"] = np.ascontiguousarray(np.broadcast_to(grow[None, :], (128, 832)))
    m["b_inv"] = np.ascontiguousarray(np.broadcast_to((10000.0 ** (-np.arange(16, dtype=np.float32) * 2.0 / 32.0)).astype(np.float32), (128, 16)))
    key = np.arange(128)[:, None]
    col = np.arange(512)[None, :]
    m["b_mask"] = np.ascontiguousarray(np.stack([(col - i * 128 >= key) for i in range(4)], axis=1).astype(np.float32))
    agrow = np.concatenate([f(inputs["a_q_gain"])[0], f(inputs["a_k_gain"])[0]])
    m["a_grow"] = np.ascontiguousarray(np.broadcast_to(agrow[None, :], (128, 128)))
    m["a_inv"] = np.ascontiguousarray(np.broadcast_to((10000.0 ** (-np.arange(32, dtype=np.float32) * 2.0 / 64.0)).astype(np.float32), (128, 32)))
    q = np.arange(128)[None, :]
    m["a_mask"] = np.ascontiguousarray(np.concatenate([(key >= q), (key <= q)], axis=1).astype(np.float32))
    return m


def host_pos(pos):
    m = {}
    m["pos_pm"] = np.ascontiguousarray(pos.reshape(32, 128).T)
    cols = []
    for s_ in range(2):
        for g in range(3):
            for ti in range(16):
                st, dil, nb = a_tile_start(g, ti)
                cols.append(pos[s_, st:st + 127 * dil + 1:dil])
    m["pos_a"] = np.ascontiguousarray(np.stack(cols, axis=1).astype(np.int32))
    return m


MIXERS = {}


def stage_mix_d(P, C, S, D, li):
    TT = 512
    P.begin_stage()
    wi_d = C["d_w_in"][0].rearrange("(k p) n -> p k n", p=128)
    wo_d = C["d_w_out"][0].rearrange("(k p) n -> p k n", p=128)
    wrg_d = C["d_w_rg"][0].rearrange("n (cc p) d -> p (n cc) d", p=128)
    wig_d = C["d_w_ig"][0].rearrange("n (cc p) d -> p (n cc) d", p=128)
    N = NormUnit(P, TT, C["gains"][li, 1])
    wi = [P.sb([128, 8, 512], BF16, "wi") for _ in range(4)]
    for g in [2, 0, 3, 1]:
        P.dma("pool", wi[g][:], wi_d[:, :, g * 512:(g + 1) * 512], writes=[wi[g]])
    wrg = P.sb([128, 8, 256], BF16, "wrg")
    wig = P.sb([128, 8, 256], BF16, "wig")
    wo = P.sb([128, 8, 1024], BF16, "wo")
    P.dma("pool", wrg[:], wrg_d, writes=[wrg])
    P.dma("pool", wig[:], wig_d, writes=[wig])
    P.dma("pool", wo[:], wo_d, writes=[wo])
    sm = P.sb([128, 8, 8], F32, "dsm")
    P.dma("sp", sm[:], C["d_small"], writes=[sm])
    nsp = P.sb([128, 8], F32, "nsp")
    onec = P.sb([128, 1], F32, "onec")
    P.op("dve", lambda e: e.memset(onec[:], 1.0), writes=[onec])
    P.op("act", lambda e: e.activation(out=nsp[:], in_=sm[:, :, 7], func=AF.Exp, scale=-1.0), reads=[sm], writes=[nsp])
    P.op("act", lambda e: e.activation(out=nsp[:], in_=nsp[:], func=AF.Ln, bias=onec[:, 0:1]), reads=[nsp, onec], writes=[nsp])
    P.op("dve", lambda e: e.tensor_scalar(out=nsp[:], in0=nsp[:], scalar1=-8.0, scalar2=None, op0=ALU.mult), reads=[nsp], writes=[nsp])

    xt = [P.sb([128, 8, TT], F32, "xt") for _ in range(2)]
    xn = [P.sb([128, 8, TT], BF16, "xn") for _ in range(2)]
    og = P.sb([128, 8, TT], BF16, "og")
    ub = [P.sb([128, 2, TT + 3], F32, "ub") for _ in range(2)]
    uc = [P.sb([128, 2, TT], F32, "uc") for _ in range(2)]
    ucb = [P.sb([128, 2, TT], BF16, "ucb") for _ in range(2)]
    carry_u = P.sb([128, 8, 3], F32, "carry_u")
    carry_h = P.sb([128, 8], F32, "carry_h")
    tmp = {nm: [P.sb([128, TT], F32, nm) for _ in range(2)] for nm in ("r", "ig", "a", "s", "b", "hs", "gl")}
    pu = [P.ps(name="pu") for _ in range(2)]
    pr, pi, pgt = P.ps(name="pr"), P.ps(name="pi"), P.ps(name="pgt")
    py = [P.ps(name="py") for _ in range(2)]

    NT = NTOK // TT
    load_x(P, S, 0, TT, xt[0])
    N.run(xt[0], xn[0])
    it = 0
    for t in range(NT):
        X, XN = xt[t % 2], xn[t % 2]
        if t + 1 < NT:
            load_x(P, S, (t + 1) * TT, TT, xt[(t + 1) % 2])
        if t % (SEQ // TT) == 0:
            P.op("pool", lambda e: e.memset(carry_u[:], 0.0), writes=[carry_u])
            P.op("pool", lambda e: e.memset(carry_h[:], 0.0), writes=[carry_h])
        for n in range(4):
            UB, UC, UCB = ub[n % 2], uc[n % 2], ucb[n % 2]
            for cc in range(2):
                c = 2 * n + cc
                p_u = pu[cc]
                col = 1024 + c * 128
                gi, go = col // 512, col % 512
                for k in range(8):
                    P.op("pe", lambda e, k=k, gi=gi, go=go: e.matmul(p_u[:, :TT], lhsT=wi[gi][:, k, go:go + 128], rhs=XN[:, k, :],
                                                                     start=(k == 0), stop=(k == 7)),
                         reads=[wi[gi], XN], writes=[p_u], signal=(k == 7))
                P.op("pool", lambda e, cc=cc, c=c: e.tensor_copy(out=UB[:, cc, 0:3], in_=carry_u[:, c, :]), reads=[carry_u], writes=[UB], partial=(cc > 0))
                P.op("act", lambda e, cc=cc: e.copy(out=UB[:, cc, 3:3 + TT], in_=p_u[:, :TT]), reads=[p_u], writes=[UB], partial=True)
                P.op("pool", lambda e, cc=cc, c=c: e.tensor_copy(out=carry_u[:, c, :], in_=UB[:, cc, TT:TT + 3]), reads=[UB], writes=[carry_u], partial=True)
                P.op("dve", lambda e, cc=cc, c=c: e.tensor_scalar(out=UC[:, cc, :], in0=UB[:, cc, 3:3 + TT], scalar1=sm[:, c, 3:4], scalar2=sm[:, c, 4:5],
                                                                   op0=ALU.mult, op1=ALU.add), reads=[UB, sm], writes=[UC], partial=(cc > 0))
                for j in range(3):
                    P.op("dve", lambda e, cc=cc, c=c, j=j: e.scalar_tensor_tensor(out=UC[:, cc, :], in0=UB[:, cc, j:j + TT], scalar=sm[:, c, j:j + 1], in1=UC[:, cc, :],
                                                                                   op0=ALU.mult, op1=ALU.add), reads=[UB, sm, UC], writes=[UC], partial=True)
                P.op("act", lambda e, cc=cc: e.copy(out=UCB[:, cc, :], in_=UC[:, cc, :]), reads=[UC], writes=[UCB], partial=(cc > 0))
            for dc in range(2):
                c = 2 * n + dc
                i2 = it % 2
                it += 1
                r, ig, a, s, b, hs, gl = (tmp[nm][i2] for nm in ("r", "ig", "a", "s", "b", "hs", "gl"))
                for cc in range(2):
                    P.op("pe", lambda e, cc=cc, dc=dc, n=n: e.matmul(pr[:, :TT], lhsT=wrg[:, 2 * n + cc, dc * 128:(dc + 1) * 128], rhs=UCB[:, cc, :],
                                                                     start=(cc == 0), stop=(cc == 1)), reads=[wrg, UCB], writes=[pr], signal=(cc == 1))
                for cc in range(2):
                    P.op("pe", lambda e, cc=cc, dc=dc, n=n: e.matmul(pi[:, :TT], lhsT=wig[:, 2 * n + cc, dc * 128:(dc + 1) * 128], rhs=UCB[:, cc, :],
                                                                     start=(cc == 0), stop=(cc == 1)), reads=[wig, UCB], writes=[pi], signal=(cc == 1))
                gi, go = (c * 128) // 512, (c * 128) % 512
                for k in range(8):
                    P.op("pe", lambda e, k=k, gi=gi, go=go: e.matmul(pgt[:, :TT], lhsT=wi[gi][:, k, go:go + 128], rhs=XN[:, k, :],
                                                                     start=(k == 0), stop=(k == 7)),
                         reads=[wi[gi], XN], writes=[pgt], signal=(k == 7))
                P.op("act", lambda e, c=c: e.activation(out=r[:], in_=pr[:, :TT], func=AF.Sigmoid, bias=sm[:, c, 5:6]), reads=[pr, sm], writes=[r])
                P.op("act", lambda e, c=c: e.activation(out=ig[:], in_=pi[:, :TT], func=AF.Sigmoid, bias=sm[:, c, 6:7]), reads=[pi, sm], writes=[ig])
                P.op("act", lambda e, c=c: e.activation(out=a[:], in_=r[:], func=AF.Exp, scale=nsp[:, c:c + 1]), reads=[r, nsp], writes=[a])
                P.op("act", lambda e: e.activation(out=s[:], in_=a[:], func=AF.Square), reads=[a], writes=[s])
                P.op("act", lambda e: e.activation(out=s[:], in_=s[:], func=AF.Sqrt, scale=-1.0, bias=onec[:, 0:1]), reads=[s, onec], writes=[s])
                P.op("dve", lambda e, dc=dc: e.tensor_tensor(out=b[:], in0=ig[:], in1=UC[:, dc, :], op=ALU.mult), reads=[ig, UC], writes=[b])
                P.op("dve", lambda e: e.tensor_tensor(out=b[:], in0=b[:], in1=s[:], op=ALU.mult), reads=[b, s], writes=[b])
                P.op("dve", lambda e, c=c: e.tensor_tensor_scan(out=hs[:], data0=a[:], data1=b[:], initial=carry_h[:, c:c + 1], op0=ALU.mult, op1=ALU.add),
                     reads=[a, b, carry_h], writes=[hs])
                P.op("pool", lambda e, c=c: e.tensor_copy(out=carry_h[:, c:c + 1], in_=hs[:, TT - 1:TT]), reads=[hs], writes=[carry_h], partial=True)
                P.op("act", lambda e: e.activation(out=gl[:], in_=pgt[:, :TT], func=AF.Gelu_apprx_tanh), reads=[pgt], writes=[gl])
                P.op("dve", lambda e, c=c: e.tensor_tensor(out=og[:, c, :], in0=gl[:], in1=hs[:], op=ALU.mult), reads=[gl, hs], writes=[og], partial=(c > 0))
        if t + 1 < NT:
            N.run(xt[(t + 1) % 2], xn[(t + 1) % 2])
        for mo in range(8):
            cps = py[mo % 2]
            for k in range(8):
                P.op("pe", lambda e, k=k, mo=mo: e.matmul(cps[:, :TT], lhsT=wo[:, k, mo * 128:(mo + 1) * 128], rhs=og[:, k, :],
                                                          start=(k == 0), stop=(k == 7)), reads=[wo, og], writes=[cps], signal=(k == 7))
            P.op("dve", lambda e, mo=mo: e.tensor_tensor(out=X[:, mo, :], in0=cps[:, :TT], in1=X[:, mo, :], op=ALU.add),
                 reads=[cps, X], writes=[X], partial=True)
        store_x(P, D, t * TT, TT, X)
    P.end_stage()


TWO_PI = 6.283185307179586
C1_2PI = 6.28125
C2_2PI = TWO_PI - C1_2PI


def mm_group(P, ps_ap, ps_buf, pairs, reads_extra=()):
    n = len(pairs)
    for i, (l, r, bufs) in enumerate(pairs):
        P.op("pe", lambda e, l=l, r=r, i=i: e.matmul(ps_ap, lhsT=l, rhs=r, start=(i == 0), stop=(i == n - 1)),
             reads=list(bufs) + list(reads_extra), writes=[ps_buf], signal=(i == n - 1))


def rope_tables(P, posi, inv, nt, half, cos, sin):
    posf = P.sb([128, nt], F32, "posf")
    ang = P.sb([128, nt, half], F32, "ang")
    a2 = P.sb([128, nt, half], F32, "a2")
    ni = P.sb([128, nt, half], I32, "ni")
    nf = P.sb([128, nt, half], F32, "nf")
    P.op("dve", lambda e: e.tensor_copy(out=posf[:], in_=posi[:]), reads=[posi], writes=[posf])
    P.op("dve", lambda e: e.tensor_tensor(out=ang[:], in0=posf[:, :].unsqueeze(2).to_broadcast([128, nt, half]),
                                          in1=inv[:, :].unsqueeze(1).to_broadcast([128, nt, half]), op=ALU.mult),
         reads=[posf, inv], writes=[ang])
    for dst, shift in ((sin, 0.0), (cos, 1.5707963267948966)):
        P.op("dve", lambda e, shift=shift: e.tensor_scalar(out=a2[:], in0=ang[:], scalar1=shift, scalar2=None, op0=ALU.add), reads=[ang], writes=[a2])
        P.op("dve", lambda e: e.tensor_scalar(out=ni[:], in0=a2[:], scalar1=1.0 / TWO_PI, scalar2=None, op0=ALU.mult), reads=[a2], writes=[ni])
        P.op("dve", lambda e: e.tensor_copy(out=nf[:], in_=ni[:]), reads=[ni], writes=[nf])
        P.op("dve", lambda e: e.scalar_tensor_tensor(out=a2[:], in0=nf[:], scalar=-C1_2PI, in1=a2[:], op0=ALU.mult, op1=ALU.add), reads=[nf, a2], writes=[a2])
        P.op("dve", lambda e: e.scalar_tensor_tensor(out=a2[:], in0=nf[:], scalar=-C2_2PI, in1=a2[:], op0=ALU.mult, op1=ALU.add), reads=[nf, a2], writes=[a2])
        P.op("dve", lambda e: e.tensor_scalar(out=a2[:], in0=a2[:], scalar1=3.1415925, scalar2=-3.1415925, op0=ALU.min, op1=ALU.max), reads=[a2], writes=[a2])
        P.op("act", lambda e, dst=dst: e.activation(out=dst[:], in_=a2[:], func=AF.Sin), reads=[a2], writes=[dst])


def rope_apply(P, ps3, psbuf, out3, outbuf, cosb, sinb, tabs, tmp, shape, partial):
    H, half = shape
    x1, x2 = ps3[:, :, 0:half], ps3[:, :, half:2 * half]
    v = lambda b: b[:, :].rearrange("p (h d) -> p h d", d=half)
    t1, t2, t3, t4 = tmp
    P.op("dve", lambda e: e.tensor_tensor(out=v(t1), in0=x1, in1=cosb, op=ALU.mult), reads=[psbuf] + tabs, writes=[t1])
    P.op("dve", lambda e: e.tensor_tensor(out=v(t2), in0=x2, in1=sinb, op=ALU.mult), reads=[psbuf] + tabs, writes=[t2])
    P.op("dve", lambda e: e.tensor_tensor(out=v(t3), in0=x2, in1=cosb, op=ALU.mult), reads=[psbuf] + tabs, writes=[t3])
    P.op("dve", lambda e: e.tensor_tensor(out=v(t4), in0=x1, in1=sinb, op=ALU.mult), reads=[psbuf] + tabs, writes=[t4])
    P.op("pool", lambda e: e.tensor_tensor(out=out3[:, :, 0:half], in0=v(t1), in1=v(t2), op=ALU.subtract), reads=[t1, t2], writes=[outbuf], partial=partial)
    P.op("pool", lambda e: e.tensor_tensor(out=out3[:, :, half:2 * half], in0=v(t3), in1=v(t4), op=ALU.add), reads=[t3, t4], writes=[outbuf], partial=True)


def stage_tables(P, C, pos_key, inv_key, nt, half, cos, sin):
    P.begin_stage()
    inv = P.sb([128, half], F32, "inv")
    P.dma("sp", inv[:], C[inv_key], writes=[inv])
    posi = P.sb([128, nt], I32, "posi")
    P.dma("sp", posi[:], C[pos_key], writes=[posi])
    rope_tables(P, posi, inv, nt, half, cos, sin)
    P.end_stage()


def stage_c1a(P, C, S, li, cos, sin):
    TT = 512
    P.begin_stage()
    w_d = C["c_w_in"][0].rearrange("(k p) n -> p k n", p=128)
    wc = [P.sb([128, 8, 512], BF16, "wc") for _ in range(4)]
    for g in range(4):
        P.dma("pool", wc[g][:], w_d[:, :, g * 512:(g + 1) * 512], writes=[wc[g]])
    N = NormUnit(P, TT, C["gains"][li, 1])
    ident = P.sb([128, 128], BF16, "ident")
    P.dma("pool", ident[:], C["ident"], writes=[ident])
    zeta = P.sb([128, 8], F32, "zeta")
    P.dma("sp", zeta[:], C["c_zeta"], writes=[zeta])
    xt = P.sb([128, 8, TT], F32, "xt")
    xn = [P.sb([128, 8, TT], BF16, "xn") for _ in range(2)]
    qk = [P.sb([128, 2048], BF16, "qk") for _ in range(2)]
    kz = [P.sb([128, 1024], BF16, "kz") for _ in range(2)]
    tmp = [P.sb([128, 256], F32, "rt") for _ in range(8)]
    qTs = [P.sb([128, 16, TT], BF16, "qTs") for _ in range(2)]
    pp = [P.ps(name="pp") for _ in range(3)]
    ptr = [P.ps(name="ptr") for _ in range(2)]
    NT = NTOK // TT
    qkT_v = C["c_qkT"].rearrange("h d t -> d h t")
    ipp = 0
    itr = 0
    load_x(P, S, 0, TT, xt)
    N.run(xt, xn[0])
    for t in range(NT):
        XN = xn[t % 2]
        QT = qTs[t % 2]
        if t + 1 < NT:
            load_x(P, S, (t + 1) * TT, TT, xt)
        for sub in range(4):
            j = t * 4 + sub
            QK = qk[j % 2]
            KZ = kz[j % 2]
            for cg in range(4):
                ps = pp[ipp % 3]
                ipp += 1
                mm_group(P, ps[:, :], ps, [(XN[:, k, sub * 128:(sub + 1) * 128], wc[cg][:, k, :], [XN, wc[cg]]) for k in range(8)])
                cb = cos[:, j, :].unsqueeze(1).to_broadcast([128, 4, 64])
                sb_ = sin[:, j, :].unsqueeze(1).to_broadcast([128, 4, 64])
                ps3 = ps[:, :].rearrange("p (h d) -> p h d", d=128)
                out3 = QK[:, cg * 512:(cg + 1) * 512].rearrange("p (h d) -> p h d", d=128)
                rope_apply(P, ps3, ps, out3, QK, cb, sb_, [cos, sin], tmp[4 * (cg % 2):4 * (cg % 2) + 4], (4, 64), partial=(cg > 0))
            P.op("dve", lambda e: e.tensor_tensor(out=KZ[:, :].rearrange("p (h d) -> p h d", d=128),
                                                  in0=QK[:, 1024:2048].rearrange("p (h d) -> p h d", d=128),
                                                  in1=zeta[:, :].unsqueeze(2).to_broadcast([128, 8, 128]), op=ALU.mult),
                 reads=[QK, zeta], writes=[KZ])
            P.dma("sp", C["c_kz"][j * 128:(j + 1) * 128, :], KZ[:], reads=[KZ], writes=[C["c_kz_buf"]], partial=True)
            for b4 in range(4):
                pt = ptr[itr % 2]
                itr += 1
                for i in range(4):
                    hh = b4 * 4 + i
                    P.op("pe", lambda e, hh=hh, i=i: e.matmul(pt[:, i * 128:(i + 1) * 128], lhsT=QK[:, hh * 128:(hh + 1) * 128], rhs=ident[:],
                                                              start=True, stop=True), reads=[QK, ident], writes=[pt], signal=(i == 3))
                P.op("act", lambda e, b4=b4: e.copy(out=QT[:, b4 * 4:(b4 + 1) * 4, sub * 128:(sub + 1) * 128],
                                                    in_=pt[:, :].rearrange("p (h t) -> p h t", t=128)),
                     reads=[pt], writes=[QT], partial=not (sub == 0 and b4 == 0))
        P.dma("sp", qkT_v[:, :, t * TT:(t + 1) * TT], QT[:], reads=[QT], writes=[C["c_qkT_buf"]], partial=True)
        if t + 1 < NT:
            N.run(xt, xn[(t + 1) % 2])
    P.end_stage()


def stage_c1b(P, C, S, li):
    TT = 512
    P.begin_stage()
    w_d = C["c_w_in"][0].rearrange("(k p) n -> p k n", p=128)
    wc = [P.sb([128, 8, 512], BF16, "wc") for _ in range(8)]
    for g in range(8):
        P.dma("pool", wc[g][:], w_d[:, :, (4 + g) * 512:(5 + g) * 512], writes=[wc[g]])
    N = NormUnit(P, TT, C["gains"][li, 1])
    xt = P.sb([128, 8, TT], F32, "xt")
    xn = [P.sb([128, 8, TT], BF16, "xn") for _ in range(2)]
    vg = [P.sb([128, 4096], BF16, "vg") for _ in range(2)]
    pp = [P.ps(name="pp") for _ in range(4)]
    NT = NTOK // TT
    ipp = 0
    load_x(P, S, 0, TT, xt)
    N.run(xt, xn[0])
    for t in range(NT):
        XN = xn[t % 2]
        if t + 1 < NT:
            load_x(P, S, (t + 1) * TT, TT, xt)
        for sub in range(4):
            j = t * 4 + sub
            VG = vg[j % 2]
            for cg in range(8):
                ps = pp[ipp % 4]
                ipp += 1
                mm_group(P, ps[:, :], ps, [(XN[:, k, sub * 128:(sub + 1) * 128], wc[cg][:, k, :], [XN, wc[cg]]) for k in range(8)])
                if cg < 4:
                    P.op("dve", lambda e, cg=cg: e.tensor_copy(out=VG[:, cg * 512:(cg + 1) * 512], in_=ps[:, :]), reads=[ps], writes=[VG], partial=(cg > 0))
                else:
                    P.op("act", lambda e, cg=cg: e.activation(out=VG[:, cg * 512:(cg + 1) * 512], in_=ps[:, :], func=AF.Silu), reads=[ps], writes=[VG], partial=True)
            P.dma("sp", C["c_vg"][j * 128:(j + 1) * 128, :], VG[:], reads=[VG], writes=[C["c_vg_buf"]], partial=True)
        if t + 1 < NT:
            N.run(xt, xn[(t + 1) % 2])
    P.end_stage()


def stage_c2(P, C):
    P.begin_stage()
    ident = P.sb([128, 128], BF16, "ident")
    P.dma("pool", ident[:], C["ident"], writes=[ident])
    decT = P.sb([128, 8, 128], F32, "decT")
    P.dma("sp", decT[:], C["c_decT"], writes=[decT])
    xib = P.sb([128, 8, 128], F32, "xib")
    P.dma("sp", xib[:], C["c_xi"], writes=[xib])
    eps5 = P.sb([128, 1], F32, "eps5")
    P.op("dve", lambda e: e.memset(eps5[:], 1e-5), writes=[eps5])
    lg = np.log(np.float32(1.0) - np.float32(2.0) ** (-5.0 - np.arange(8, dtype=np.float32))).astype(np.float32)
    cd = [float(np.exp(np.float32(128.0) * lg[h])) for h in range(8)]
    qkc = [P.sb([128, 16, 128], BF16, "qkc") for _ in range(2)]
    kzc = [P.sb([128, 1024], BF16, "kzc") for _ in range(2)]
    vgc = [P.sb([128, 4096], BF16, "vgc") for _ in range(2)]
    qx = [P.sb([128, 8, 128], BF16, "qx") for _ in range(2)]
    R = P.sb([128, 8, 256], F32, "R")
    Rb = P.sb([128, 8, 256], BF16, "Rb")
    AT = [P.sb([128, 128], BF16, "AT") for _ in range(2)]
    st = [P.sb([128, 6], F32, "st") for _ in range(2)]
    mv = [P.sb([128, 2], F32, "mv") for _ in range(2)]
    rstd = [P.sb([128, 1], F32, "rstd") for _ in range(2)]
    yn = [P.sb([128, 256], F32, "yn") for _ in range(2)]
    ob = [P.sb([128, 2048], BF16, "ob") for _ in range(2)]
    oTs = [P.sb([128, 16, 512], BF16, "oTs") for _ in range(2)]
    ps_s = [P.ps(name="ps_s") for _ in range(2)]
    ps_y = [P.ps(name="ps_y") for _ in range(2)]
    ps_u = [P.ps(name="ps_u") for _ in range(2)]
    ptr = [P.ps(name="ptr") for _ in range(2)]
    qkT_v = C["c_qkT"].rearrange("h d t -> d h t")
    oT_v = C["oT"].rearrange("k f t -> f k t")
    it = 0
    itr = 0

    def load_chunk(g):
        i = g % 2
        P.dma("sp", qkc[i][:], qkT_v[:, :, g * 128:(g + 1) * 128], reads=[C["c_qkT_buf"]], writes=[qkc[i]])
        P.dma("sp", kzc[i][:], C["c_kz"][g * 128:(g + 1) * 128, :], reads=[C["c_kz_buf"]], writes=[kzc[i]])
        P.dma("sp", vgc[i][:], C["c_vg"][g * 128:(g + 1) * 128, :], reads=[C["c_vg_buf"]], writes=[vgc[i]])

    load_chunk(0)
    for g in range(32):
        c = g % 16
        QKC, KZC, VGC, QX, OB = qkc[g % 2], kzc[g % 2], vgc[g % 2], qx[g % 2], ob[g % 2]
        OT = oTs[(g // 4) % 2]
        if g + 1 < 32:
            load_chunk(g + 1)
        if c > 0:
            P.op("pool", lambda e: e.tensor_tensor(out=QX[:], in0=QKC[:, 0:8, :], in1=xib[:], op=ALU.mult), reads=[QKC, xib], writes=[QX])
        for h in range(8):
            i2 = it % 2
            it += 1
            pss, psy, psu = ps_s[i2], ps_y[i2], ps_u[i2]
            vh = VGC[:, h * 256:(h + 1) * 256]
            P.op("pe", lambda e, h=h: e.matmul(pss[:, 0:128], lhsT=QKC[:, 8 + h, :], rhs=QKC[:, h, :], start=True, stop=True), reads=[QKC], writes=[pss])
            P.op("dve", lambda e, h=h: e.tensor_tensor(out=AT[i2][:], in0=pss[:, 0:128], in1=decT[:, h, :], op=ALU.mult), reads=[pss, decT], writes=[AT[i2]])
            if c > 0:
                mm_group(P, psy[:, 0:256], psy, [(AT[i2][:], vh, [AT[i2], VGC]), (QX[:, h, :], Rb[:, h, :], [QX, Rb])])
            else:
                mm_group(P, psy[:, 0:256], psy, [(AT[i2][:], vh, [AT[i2], VGC])])
            if c < 15:
                P.op("pe", lambda e, h=h: e.matmul(psu[:, 0:256], lhsT=KZC[:, h * 128:(h + 1) * 128], rhs=VGC[:, h * 256:(h + 1) * 256], start=True, stop=True),
                     reads=[KZC, VGC], writes=[psu])
                if c == 0:
                    P.op("dve", lambda e, h=h: e.tensor_copy(out=R[:, h, :], in_=psu[:, 0:256]), reads=[psu, Rb], writes=[R], partial=True)
                else:
                    P.op("dve", lambda e, h=h: e.scalar_tensor_tensor(out=R[:, h, :], in0=R[:, h, :], scalar=cd[h], in1=psu[:, 0:256], op0=ALU.mult, op1=ALU.add),
                         reads=[psu, R], writes=[R], partial=True)
                P.op("act", lambda e, h=h: e.copy(out=Rb[:, h, :], in_=R[:, h, :]), reads=[R], writes=[Rb], partial=True)
            P.op("dve", lambda e: e.bn_stats(out=st[i2][:], in_=psy[:, 0:256]), reads=[psy], writes=[st[i2]])
            P.op("dve", lambda e: e.bn_aggr(out=mv[i2][:], in_=st[i2][:]), reads=[st[i2]], writes=[mv[i2]])
            P.op("act", lambda e: e.activation(out=rstd[i2][:], in_=mv[i2][:, 1:2], func=AF.Sqrt, bias=eps5[:, 0:1]), reads=[mv[i2], eps5], writes=[rstd[i2]])
            P.op("dve", lambda e: e.reciprocal(out=rstd[i2][:], in_=rstd[i2][:]), reads=[rstd[i2]], writes=[rstd[i2]])
            P.op("dve", lambda e: e.tensor_scalar(out=yn[i2][:], in0=psy[:, 0:256], scalar1=mv[i2][:, 0:1], scalar2=rstd[i2][:, 0:1], op0=ALU.subtract, op1=ALU.mult),
                 reads=[psy, mv[i2], rstd[i2]], writes=[yn[i2]])
            P.op("pool", lambda e, h=h: e.tensor_tensor(out=OB[:, h * 256:(h + 1) * 256], in0=yn[i2][:], in1=VGC[:, 2048 + h * 256:2048 + (h + 1) * 256], op=ALU.mult),
                 reads=[yn[i2], VGC], writes=[OB], partial=(h > 0))
        for b4 in range(4):
            pt = ptr[itr % 2]
            itr += 1
            for i in range(4):
                kk = b4 * 4 + i
                P.op("pe", lambda e, kk=kk, i=i: e.matmul(pt[:, i * 128:(i + 1) * 128], lhsT=OB[:, kk * 128:(kk + 1) * 128], rhs=ident[:], start=True, stop=True),
                     reads=[OB, ident], writes=[pt], signal=(i == 3))
            P.op("act", lambda e, b4=b4: e.copy(out=OT[:, b4 * 4:(b4 + 1) * 4, (g % 4) * 128:(g % 4 + 1) * 128], in_=pt[:, :].rearrange("p (k t) -> p k t", t=128)),
                 reads=[pt], writes=[OT], partial=not (g % 4 == 0 and b4 == 0))
        if g % 4 == 3:
            t0 = (g // 4) * 512
            P.dma("sp", oT_v[:, 0:16, t0:t0 + 512], OT[:], reads=[OT], writes=[C["oT_buf"]], partial=True)
    P.end_stage()


def stage_outproj(P, C, S, D, w_ap, nk):
    TT = 512
    P.begin_stage()
    w_d = w_ap.rearrange("(k p) n -> p k n", p=128)
    wo = [P.sb([128, 4, 1024], BF16, "wo") for _ in range(nk // 4)]
    for g in range(nk // 4):
        P.dma("pool", wo[g][:], w_d[:, 4 * g:4 * g + 4, :], writes=[wo[g]])
    oT_v = C["oT"].rearrange("k f t -> f k t")
    xt = [P.sb([128, 8, TT], F32, "xt") for _ in range(2)]
    ot = [P.sb([128, nk, TT], BF16, "ot") for _ in range(2)]
    py = [P.ps(name="py") for _ in range(2)]
    NT = NTOK // TT

    def ld(t):
        load_x(P, S, t * TT, TT, xt[t % 2])
        P.dma("sp", ot[t % 2][:], oT_v[:, 0:nk, t * TT:(t + 1) * TT], reads=[C["oT_buf"]], writes=[ot[t % 2]])

    ld(0)
    for t in range(NT):
        X, OTT = xt[t % 2], ot[t % 2]
        if t + 1 < NT:
            ld(t + 1)
        for mo in range(8):
            cps = py[mo % 2]
            mm_group(P, cps[:, :], cps, [(wo[k // 4][:, k % 4, mo * 128:(mo + 1) * 128], OTT[:, k, :], [wo[k // 4], OTT]) for k in range(nk)])
            P.op("dve", lambda e, mo=mo: e.tensor_tensor(out=X[:, mo, :], in0=cps[:, :], in1=X[:, mo, :], op=ALU.add), reads=[cps, X], writes=[X], partial=True)
        store_x(P, D, t * TT, TT, X)
    P.end_stage()


def stage_mix_c(P, C, S, D, li):
    P.begin_group()
    cos = P.sb([128, 32, 64], F32, "cos")
    sin = P.sb([128, 32, 64], F32, "sin")
    stage_tables(P, C, "pos_pm", "c_inv", 32, 64, cos, sin)
    stage_c1a(P, C, S, li, cos, sin)
    stage_c1b(P, C, S, li)
    stage_c2(P, C)
    stage_outproj(P, C, S, D, C["c_w_out"][0], 16)
    P.end_group()


def v3(ap, d):
    return ap.rearrange("p (h d) -> p h d", d=d)


def stage_b1(P, C, S, li, cos, sin, rk_all):
    TT = 512
    P.begin_stage()
    wbi = P.sb([128, 8, 672], BF16, "wbi")
    P.dma("pool", wbi[:], C["b_w_in"][0].rearrange("(k p) n -> p k n", p=128), writes=[wbi])
    wqu = P.sb([128, 3, 1536], BF16, "wqu")
    P.dma("pool", wqu[:], C["b_w_q_up"][0].rearrange("(k p) n -> p k n", p=128), writes=[wqu])
    wkvu = P.sb([128, 2, 2048], BF16, "wkvu")
    P.dma("pool", wkvu[:], C["b_w_kv_up"][0].rearrange("(k p) n -> p k n", p=128), writes=[wkvu])
    N = NormUnit(P, TT, C["gains"][li, 1])
    ident = P.sb([128, 128], BF16, "ident")
    P.dma("pool", ident[:], C["ident"], writes=[ident])
    grow = P.sb([128, 384 + 256 + 96 + 96], F32, "grow")
    P.dma("sp", grow[:], C["b_grow"], writes=[grow])
    gqa, gkva = grow[:, 0:384], grow[:, 384:640]
    GQ0, GK0 = 640, 736
    epsq = P.sb([128, 3], F32, "epsq")
    P.op("dve", lambda e: e.memset(epsq[:, 0:1], 1e-6), writes=[epsq])
    P.op("dve", lambda e: e.memset(epsq[:, 1:2], 96e-6), writes=[epsq], partial=True)
    xt = P.sb([128, 8, TT], F32, "xt")
    xn = [P.sb([128, 8, TT], BF16, "xn") for _ in range(2)]
    sq = [P.sb([128, 512], F32, "sq") for _ in range(2)]
    ssa = [P.sb([128, 4], F32, "ssa") for _ in range(2)]
    cn = [P.sb([128, 640], BF16, "cn") for _ in range(2)]
    cT = [P.sb([128, 5, 128], BF16, "cT") for _ in range(2)]
    qf = [P.sb([128, 1536], F32, "qf") for _ in range(2)]
    kvf = [P.sb([128, 2048], F32, "kvf") for _ in range(2)]
    krf = [P.sb([128, 32], F32, "krf") for _ in range(2)]
    ss16 = [P.sb([128, 32], F32, "ss16") for _ in range(2)]
    tq = [P.sb([128, 1536], F32, "tq") for _ in range(2)]
    gt = [P.sb([128, 8, 16], F32, "gt") for _ in range(2)]
    rt = [P.sb([128, 16 * 16], F32, "rt") for _ in range(4)]
    krr = [P.sb([128, 32], F32, "krr") for _ in range(2)]
    qb = [P.sb([128, 1536], BF16, "qb") for _ in range(2)]
    kb = [P.sb([128, 1536], BF16, "kb") for _ in range(2)]
    vb = [P.sb([128, 1024], BF16, "vb") for _ in range(2)]
    qTs = [P.sb([128, 32, 128], BF16, "qTs") for _ in range(2)]
    psA, psB = P.ps(name="psA"), P.ps(name="psB")
    pq = [P.ps(name="pq") for _ in range(3)]
    ptr = [P.ps(name="ptr") for _ in range(2)]
    qkT_v = C["b_qkT"].rearrange("h d t -> d h t")
    NT = NTOK // TT
    ipq = 0
    itr = 0
    import os
    B1CUT = int(os.environ.get("B1CUT", "99"))
    load_x(P, S, 0, TT, xt)
    N.run(xt, xn[0])
    for t in range(NT):
        XN = xn[t % 2]
        if t + 1 < NT:
            load_x(P, S, (t + 1) * TT, TT, xt)
        for sub in range(4):
            j = t * 4 + sub
            b = j % 2
            SQ, SSA, CN, CT, QF, KVF, KRF, SS16, TQ, GT, KRR, QB, KB, VB, QT = (sq[b], ssa[b], cn[b], cT[b], qf[b], kvf[b], krf[b], ss16[b], tq[b], gt[b],
                                                                                  krr[b], qb[b], kb[b], vb[b], qTs[b])
            xs_ = lambda k: XN[:, k, sub * 128:(sub + 1) * 128]
            mm_group(P, psA[:, 0:384], psA, [(xs_(k), wbi[:, k, 0:384], [XN, wbi]) for k in range(8)])
            mm_group(P, psB[:, 0:288], psB, [(xs_(k), wbi[:, k, 384:672], [XN, wbi]) for k in range(8)])
            P.op("act", lambda e: e.activation(out=SQ[:, 0:384], in_=psA[:, 0:384], func=AF.Square), reads=[psA], writes=[SQ])
            P.op("dve", lambda e: e.reduce_sum(out=SSA[:, 0:1], in_=SQ[:, 0:384], axis=AX.X), reads=[SQ], writes=[SSA])
            P.op("act", lambda e: e.activation(out=SQ[:, 0:288], in_=psB[:, 0:288], func=AF.Square), reads=[psB], writes=[SQ])
            P.op("dve", lambda e: e.reduce_sum(out=SSA[:, 1:2], in_=SQ[:, 0:256], axis=AX.X), reads=[SQ], writes=[SSA], partial=True)
            P.op("dve", lambda e: e.reduce_sum(out=SSA[:, 2:3], in_=SQ[:, 256:288], axis=AX.X), reads=[SQ], writes=[SSA], partial=True)
            P.op("act", lambda e: e.activation(out=SSA[:, 0:1], in_=SSA[:, 0:1], func=AF.Sqrt, scale=1.0 / 384, bias=epsq[:, 0:1]), reads=[SSA, epsq], writes=[SSA], partial=True)
            P.op("act", lambda e: e.activation(out=SSA[:, 1:2], in_=SSA[:, 1:2], func=AF.Sqrt, scale=1.0 / 256, bias=epsq[:, 0:1]), reads=[SSA, epsq], writes=[SSA], partial=True)
            P.op("dve", lambda e: e.reciprocal(out=SSA[:, 0:2], in_=SSA[:, 0:2]), reads=[SSA], writes=[SSA], partial=True)
            P.op("dve", lambda e: e.scalar_tensor_tensor(out=CN[:, 0:384], in0=psA[:, 0:384], scalar=SSA[:, 0:1], in1=gqa, op0=ALU.mult, op1=ALU.mult),
                 reads=[psA, SSA, grow], writes=[CN])
            P.op("dve", lambda e: e.scalar_tensor_tensor(out=CN[:, 384:640], in0=psB[:, 0:256], scalar=SSA[:, 1:2], in1=gkva, op0=ALU.mult, op1=ALU.mult),
                 reads=[psB, SSA, grow], writes=[CN], partial=True)
            P.op("act", lambda e: e.copy(out=KRF[:], in_=psB[:, 256:288]), reads=[psB], writes=[KRF])
            if B1CUT <= 1:
                continue
            pt = ptr[itr % 2]
            itr += 1
            for i in range(4):
                P.op("pe", lambda e, i=i: e.matmul(pt[:, i * 128:(i + 1) * 128], lhsT=CN[:, i * 128:(i + 1) * 128], rhs=ident[:], start=True, stop=True),
                     reads=[CN, ident], writes=[pt], signal=(i == 3))
            P.op("act", lambda e: e.copy(out=CT[:, 0:4, :], in_=v3(pt[:, :], 128)), reads=[pt], writes=[CT])
            pt = ptr[itr % 2]
            itr += 1
            P.op("pe", lambda e: e.matmul(pt[:, 0:128], lhsT=CN[:, 512:640], rhs=ident[:], start=True, stop=True), reads=[CN, ident], writes=[pt])
            P.op("act", lambda e: e.copy(out=CT[:, 4, :], in_=pt[:, 0:128]), reads=[pt], writes=[CT], partial=True)
            if B1CUT <= 2:
                continue
            cs, sn = cos[:, j, :], sin[:, j, :]
            for o, g0 in ((0, GQ0), (4, GK0)):
                ga, gb = grow[:, g0 + 64:g0 + 80], grow[:, g0 + 80:g0 + 96]
                for idx, (gg, tt) in enumerate(((ga, cs), (gb, sn), (gb, cs), (ga, sn))):
                    P.op("pool", lambda e, o=o, idx=idx, gg=gg, tt=tt: e.tensor_tensor(out=GT[:, o + idx, :], in0=gg, in1=tt, op=ALU.mult),
                         reads=[grow, cos, sin], writes=[GT], partial=not (o == 0 and idx == 0))
            if B1CUT <= 3:
                continue
            for hb in range(4):
                ps = pq[ipq % 3]
                ipq += 1
                mm_group(P, ps[:, 0:384], ps, [(CT[:, kk, :], wqu[:, kk, hb * 384:(hb + 1) * 384], [CT, wqu]) for kk in range(3)])
                P.op("act", lambda e, hb=hb: e.copy(out=QF[:, hb * 384:(hb + 1) * 384], in_=ps[:, 0:384]), reads=[ps], writes=[QF], partial=(hb > 0))
                P.op("act", lambda e: e.activation(out=SQ[:, 0:384], in_=ps[:, 0:384], func=AF.Square), reads=[ps], writes=[SQ])
                P.op("dve", lambda e, hb=hb: e.reduce_sum(out=SS16[:, hb * 4:(hb + 1) * 4], in_=v3(SQ[:, 0:384], 96), axis=AX.X), reads=[SQ], writes=[SS16],
                     partial=(hb > 0))
            for hb in range(4):
                ps = pq[ipq % 3]
                ipq += 1
                mm_group(P, ps[:, :], ps, [(CT[:, 3 + kk, :], wkvu[:, kk, hb * 512:(hb + 1) * 512], [CT, wkvu]) for kk in range(2)])
                P.op("act", lambda e, hb=hb: e.copy(out=KVF[:, hb * 512:(hb + 1) * 512], in_=ps[:, :]), reads=[ps], writes=[KVF], partial=(hb > 0))
                P.op("act", lambda e: e.activation(out=v3(SQ[:, 0:256], 64), in_=v3(ps[:, :], 128)[:, :, 0:64], func=AF.Square), reads=[ps], writes=[SQ])
                P.op("dve", lambda e, hb=hb: e.reduce_sum(out=SS16[:, 16 + hb * 4:16 + (hb + 1) * 4], in_=v3(SQ[:, 0:256], 64), axis=AX.X), reads=[SQ], writes=[SS16],
                     partial=True)
            if B1CUT <= 4:
                continue
            P.op("act", lambda e: e.activation(out=SS16[:, 0:16], in_=SS16[:, 0:16], func=AF.Sqrt, scale=1.0 / 96, bias=epsq[:, 0:1]), reads=[SS16, epsq], writes=[SS16], partial=True)
            P.op("dve", lambda e: e.tensor_scalar(out=SS16[:, 16:32], in0=SS16[:, 16:32], scalar1=SSA[:, 2:3], scalar2=None, op0=ALU.add), reads=[SS16, SSA], writes=[SS16], partial=True)
            P.op("act", lambda e: e.activation(out=SS16[:, 16:32], in_=SS16[:, 16:32], func=AF.Sqrt, bias=epsq[:, 1:2]), reads=[SS16, epsq], writes=[SS16], partial=True)
            P.op("dve", lambda e: e.reciprocal(out=SS16[:, 0:16], in_=SS16[:, 0:16]), reads=[SS16], writes=[SS16], partial=True)
            P.op("dve", lambda e, j=j: e.reciprocal(out=rk_all[:, j, :], in_=SS16[:, 16:32]), reads=[SS16], writes=[rk_all], partial=True)
            if B1CUT <= 5:
                continue
            QF3, TQ3 = v3(QF[:, :], 96), v3(TQ[:, :], 96)
            P.op("dve", lambda e: e.tensor_tensor(out=TQ3[:, :, 0:64], in0=QF3[:, :, 0:64], in1=grow[:, GQ0:GQ0 + 64].unsqueeze(1).to_broadcast([128, 16, 64]), op=ALU.mult),
                 reads=[QF, grow], writes=[TQ])
            x1, x2 = QF3[:, :, 64:80], QF3[:, :, 80:96]
            bc = lambda i: GT[:, i, :].unsqueeze(1).to_broadcast([128, 16, 16])
            r3 = [v3(r_[:, :], 16) for r_ in rt]
            P.op("dve", lambda e: e.tensor_tensor(out=r3[0], in0=x1, in1=bc(0), op=ALU.mult), reads=[QF, GT], writes=[rt[0]])
            P.op("dve", lambda e: e.tensor_tensor(out=r3[1], in0=x2, in1=bc(1), op=ALU.mult), reads=[QF, GT], writes=[rt[1]])
            P.op("dve", lambda e: e.tensor_tensor(out=r3[2], in0=x2, in1=bc(2), op=ALU.mult), reads=[QF, GT], writes=[rt[2]])
            P.op("dve", lambda e: e.tensor_tensor(out=r3[3], in0=x1, in1=bc(3), op=ALU.mult), reads=[QF, GT], writes=[rt[3]])
            P.op("pool", lambda e: e.tensor_tensor(out=TQ3[:, :, 64:80], in0=r3[0], in1=r3[1], op=ALU.subtract), reads=[rt[0], rt[1]], writes=[TQ], partial=True)
            P.op("pool", lambda e: e.tensor_tensor(out=TQ3[:, :, 80:96], in0=r3[2], in1=r3[3], op=ALU.add), reads=[rt[2], rt[3]], writes=[TQ], partial=True)
            P.op("dve", lambda e: e.tensor_tensor(out=v3(QB[:, :], 96), in0=TQ3, in1=SS16[:, 0:16].unsqueeze(2).to_broadcast([128, 16, 96]), op=ALU.mult),
                 reads=[TQ, SS16], writes=[QB])
            if B1CUT <= 6:
                continue
            KV3, KB3 = v3(KVF[:, :], 128), v3(KB[:, :], 96)
            P.op("pool", lambda e: e.tensor_tensor(out=KB3[:, :, 0:64], in0=KV3[:, :, 0:64], in1=grow[:, GK0:GK0 + 64].unsqueeze(1).to_broadcast([128, 16, 64]), op=ALU.mult),
                 reads=[KVF, grow], writes=[KB])
            k1, k2 = KRF[:, 0:16], KRF[:, 16:32]
            P.op("dve", lambda e: e.tensor_tensor(out=rt[0][:, 0:16], in0=k1, in1=GT[:, 4, :], op=ALU.mult), reads=[KRF, GT], writes=[rt[0]])
            P.op("dve", lambda e: e.tensor_tensor(out=rt[1][:, 0:16], in0=k2, in1=GT[:, 5, :], op=ALU.mult), reads=[KRF, GT], writes=[rt[1]])
            P.op("dve", lambda e: e.tensor_tensor(out=rt[2][:, 0:16], in0=k2, in1=GT[:, 6, :], op=ALU.mult), reads=[KRF, GT], writes=[rt[2]])
            P.op("dve", lambda e: e.tensor_tensor(out=rt[3][:, 0:16], in0=k1, in1=GT[:, 7, :], op=ALU.mult), reads=[KRF, GT], writes=[rt[3]])
            P.op("pool", lambda e: e.tensor_tensor(out=KRR[:, 0:16], in0=rt[0][:, 0:16], in1=rt[1][:, 0:16], op=ALU.subtract), reads=[rt[0], rt[1]], writes=[KRR])
            P.op("pool", lambda e: e.tensor_tensor(out=KRR[:, 16:32], in0=rt[2][:, 0:16], in1=rt[3][:, 0:16], op=ALU.add), reads=[rt[2], rt[3]], writes=[KRR], partial=True)
            P.op("pool", lambda e: e.tensor_copy(out=KB3[:, :, 64:96], in_=KRR[:, :].unsqueeze(1).to_broadcast([128, 16, 32])), reads=[KRR], writes=[KB], partial=True)
            P.op("act", lambda e: e.copy(out=v3(VB[:, :], 64), in_=KV3[:, :, 64:128]), reads=[KVF], writes=[VB])
            P.dma("sp", C["b_v"][j * 128:(j + 1) * 128, :], VB[:], reads=[VB], writes=[C["b_v_buf"]], partial=True)
            if B1CUT <= 7:
                continue
            for src, base in ((QB, 0), (KB, 16)):
                for b4 in range(4):
                    pt = ptr[itr % 2]
                    itr += 1
                    for i in range(4):
                        hh = b4 * 4 + i
                        P.op("pe", lambda e, hh=hh, i=i, src=src: e.matmul(pt[0:96, i * 128:(i + 1) * 128], lhsT=src[:, hh * 96:(hh + 1) * 96], rhs=ident[:], start=True, stop=True),
                             reads=[src, ident], writes=[pt], signal=(i == 3))
                    P.op("act", lambda e, b4=b4, base=base: e.copy(out=QT[0:96, base + b4 * 4:base + (b4 + 1) * 4, :], in_=v3(pt[0:96, :], 128)),
                         reads=[pt], writes=[QT], partial=not (base == 0 and b4 == 0))
            for q4 in range(4):
                P.dma("sp", qkT_v[:, q4 * 8:(q4 + 1) * 8, j * 128:(j + 1) * 128], QT[0:96, q4 * 8:(q4 + 1) * 8, :], reads=[QT], writes=[C["b_qkT_buf"]], partial=True)
        if t + 1 < NT:
            N.run(xt, xn[(t + 1) % 2])
    P.end_stage()


def stage_b2(P, C, rk_all):
    P.begin_stage()
    mask = P.sb([128, 4, 512], BF16, "mask")
    P.dma("pool", mask[:], C["b_mask"], writes=[mask])
    ones = P.sb([128, 64], BF16, "ones64")
    P.op("dve", lambda e: e.memset(ones[:], 1.0), writes=[ones])
    V = P.sb([128, 16, 1024], BF16, "V")
    qT = [P.sb([128, 2048], BF16, "qT") for _ in range(2)]
    kT = [P.sb([128, 2048], BF16, "kT") for _ in range(2)]
    PT = [P.sb([128, 512], BF16, "PT") for _ in range(3)]
    rden = [P.sb([128, 512], F32, "rden") for _ in range(2)]
    oTs = [P.sb([128, 2048], BF16, "oTs") for _ in range(2)]
    ps_s = [P.ps(name="ps_s") for _ in range(2)]
    psn = [P.ps(name="psn") for _ in range(2)]
    psd = [P.ps(name="psd") for _ in range(2)]
    ist = 0
    iq = 0

    def load_head(s, h, i):
        P.dma("sp", qT[i][0:96, :], C["b_qkT"][h, :, s * 2048:(s + 1) * 2048], reads=[C["b_qkT_buf"]], writes=[qT[i]])
        P.dma("sp", kT[i][0:96, :], C["b_qkT"][16 + h, :, s * 2048:(s + 1) * 2048], reads=[C["b_qkT_buf"]], writes=[kT[i]])

    for s in range(2):
        P.dma("sp", V[:], C["b_v"][s * 2048:(s + 1) * 2048, :].rearrange("(j p) n -> p j n", p=128), reads=[C["b_v_buf"]], writes=[V])
        load_head(s, 0, 0)
        steps = [(h, qb, j) for h in range(16) for qb in range(4) for j in range(4 * qb + 4)]
        banks = {}

        def emit_qk(idx):
            nonlocal ist
            h, qb, j = steps[idx]
            if qb == 0 and j == 0 and h + 1 < 16:
                load_head(s, h + 1, (h + 1) % 2)
            QT, KT = qT[h % 2], kT[h % 2]
            pss = ps_s[ist % 2]
            pt_ = PT[ist % 3]
            ist += 1
            P.op("pe", lambda e: e.matmul(pss[:, :], lhsT=KT[0:96, j * 128:(j + 1) * 128], rhs=QT[0:96, qb * 512:(qb + 1) * 512], start=True, stop=True),
                 reads=[KT, QT], writes=[pss])
            P.op("act", lambda e: e.activation(out=pt_[:], in_=pss[:, :], func=AF.Exp, scale=rk_all[:, s * 16 + j, h:h + 1]),
                 reads=[pss, rk_all], writes=[pt_])
            if j >= 4 * qb:
                P.op("dve", lambda e: e.tensor_tensor(out=pt_[:], in0=pt_[:], in1=mask[:, j - 4 * qb, :], op=ALU.mult), reads=[pt_, mask], writes=[pt_])
            return pt_

        def emit_pv(idx, pt_):
            nonlocal iq
            h, qb, j = steps[idx]
            e0 = (h % 2) * 64
            OT = oTs[(h // 2) % 2]
            nj = 4 * qb + 4
            if j == 0:
                banks[(h, qb)] = (psn[iq % 2], psd[iq % 2], rden[iq % 2])
                iq += 1
            pn, pd, RD = banks[(h, qb)]
            P.op("pe", lambda e: e.matmul(pn[e0:e0 + 64, :], lhsT=V[:, j, h * 64:(h + 1) * 64], rhs=pt_[:], start=(j == 0), stop=(j == nj - 1)),
                 reads=[V, pt_], writes=[pn], signal=False, partial=(j > 0))
            P.op("pe", lambda e: e.matmul(pd[e0:e0 + 64, :], lhsT=ones[:], rhs=pt_[:], start=(j == 0), stop=(j == nj - 1)),
                 reads=[ones, pt_], writes=[pd], signal=True, partial=(j > 0))
            if j == nj - 1:
                P.op("dve", lambda e: e.reciprocal(out=RD[e0:e0 + 64, :], in_=pd[e0:e0 + 64, :]), reads=[pd], writes=[RD])
                P.op("dve", lambda e: e.tensor_tensor(out=OT[e0:e0 + 64, qb * 512:(qb + 1) * 512], in0=pn[e0:e0 + 64, :], in1=RD[e0:e0 + 64, :], op=ALU.mult),
                     reads=[pn, RD], writes=[OT], partial=not (h % 2 == 0 and qb == 0))
                if h % 2 == 1 and qb == 3:
                    P.dma("sp", C["oT"][h // 2, :, s * 2048:(s + 1) * 2048], OT[:], reads=[OT], writes=[C["oT_buf"]], partial=True)

        pend = emit_qk(0)
        for idx in range(len(steps)):
            nxt = emit_qk(idx + 1) if idx + 1 < len(steps) else None
            emit_pv(idx, pend)
            pend = nxt
    P.end_stage()


def stage_mix_b(P, C, S, D, li):
    P.begin_group()
    cos = P.sb([128, 32, 16], F32, "cos")
    sin = P.sb([128, 32, 16], F32, "sin")
    rk_all = P.sb([128, 32, 16], F32, "rk_all")
    stage_tables(P, C, "pos_pm", "b_inv", 32, 16, cos, sin)
    import os
    dbg = os.environ.get("BDBG", "123")
    if "1" in dbg:
        stage_b1(P, C, S, li, cos, sin, rk_all)
    if "2" in dbg:
        stage_b2(P, C, rk_all)
    if "3" in dbg:
        stage_outproj(P, C, S, D, C["b_w_out"][0], 8)
    P.end_group()


def stage_a1(P, C, S, li, cos, sin):
    TT = 512
    P.begin_stage()
    w_d = C["a_w_in"][0].rearrange("(k p) n -> p k n", p=128)
    wa = [P.sb([128, 8, 512], BF16, "wa") for _ in range(6)]
    N = NormUnit(P, TT, C["gains"][li, 1])
    ident = P.sb([128, 128], BF16, "ident")
    P.dma("pool", ident[:], C["ident"], writes=[ident])
    grow = P.sb([128, 128], F32, "grow")
    P.dma("sp", grow[:], C["a_grow"], writes=[grow])
    epsq = P.sb([128, 2], F32, "epsq")
    P.op("dve", lambda e: e.memset(epsq[:, 0:1], 1e-6), writes=[epsq])
    P.op("dve", lambda e: e.memset(epsq[:, 1:2], 64e-6), writes=[epsq], partial=True)
    xt = P.sb([128, 8, TT], F32, "xt")
    xn = P.sb([128, 8, SEQ], BF16, "xnseq")
    xn_parts = [P.sb([128, 8, TT], BF16, "xnp") for _ in range(2)]
    sq = [P.sb([128, 512], F32, "sq") for _ in range(2)]
    SSb = [P.sb([128, 32], F32, "ss") for _ in range(2)]
    Tb = [P.sb([128, 2048], F32, "T") for _ in range(2)]
    GTb = [P.sb([128, 8, 32], F32, "gt") for _ in range(2)]
    rt = [P.sb([128, 256], F32, "rt") for _ in range(8)]
    qkb = [P.sb([128, 2048], BF16, "qkb") for _ in range(2)]
    vb = [P.sb([128, 1024], BF16, "vb") for _ in range(2)]
    qTs = [P.sb([128, 16, 128], BF16, "qTs") for _ in range(2)]
    pp = [P.ps(name="pp") for _ in range(4)]
    ptr = [P.ps(name="ptr") for _ in range(2)]
    ipp = 0
    itr = 0
    irt = 0
    jt = 0
    for s in range(2):
        for t4 in range(4):
            load_x(P, S, s * SEQ + t4 * TT, TT, xt)
            XP = xn_parts[t4 % 2]
            N.run(xt, XP)
            P.op("pool", lambda e, t4=t4, XP=XP: e.tensor_copy(out=xn[:, :, t4 * TT:(t4 + 1) * TT], in_=XP[:]), reads=[XP], writes=[xn], partial=(t4 > 0))
        for g in range(3):
            for c6 in range(6):
                col = (c6 // 2) * 3072 + g * 1024 + (c6 % 2) * 512
                P.dma("pool", wa[c6][:], w_d[:, :, col:col + 512], writes=[wa[c6]])
            qkT_v = C["a_qkT"][s * 3 + g].rearrange("h d t -> d h t")
            for ti in range(16):
                b = jt % 2
                jt += 1
                SS, T, GT, QKB, VB, QT = SSb[b], Tb[b], GTb[b], qkb[b], vb[b], qTs[b]
                tix = (s * 3 + g) * 16 + ti
                st, dil, nb = a_tile_start(g, ti)
                tok = slice(st, st + 127 * dil + 1, dil)
                cs, sn = cos[:, tix, :], sin[:, tix, :]
                for o, g0 in ((0, 0), (4, 64)):
                    ga, gb = grow[:, g0:g0 + 32], grow[:, g0 + 32:g0 + 64]
                    for idx, (gg, tt) in enumerate(((ga, cs), (gb, sn), (gb, cs), (ga, sn))):
                        P.op("pool", lambda e, o=o, idx=idx, gg=gg, tt=tt: e.tensor_tensor(out=GT[:, o + idx, :], in0=gg, in1=tt, op=ALU.mult),
                             reads=[grow, cos, sin], writes=[GT], partial=not (o == 0 and idx == 0))
                for c6 in range(6):
                    ps = pp[ipp % 4]
                    ipp += 1
                    mm_group(P, ps[:, :], ps, [(xn[:, k, tok], wa[c6][:, k, :], [xn, wa[c6]]) for k in range(8)])
                    if c6 >= 4:
                        P.op("act", lambda e, c6=c6: e.copy(out=VB[:, (c6 - 4) * 512:(c6 - 3) * 512], in_=ps[:, :]), reads=[ps], writes=[VB], partial=(c6 == 5))
                        continue
                    SQ = sq[c6 % 2]
                    P.op("act", lambda e: e.activation(out=SQ[:], in_=ps[:, :], func=AF.Square), reads=[ps], writes=[SQ])
                    P.op("dve", lambda e, c6=c6: e.reduce_sum(out=SS[:, c6 * 8:(c6 + 1) * 8], in_=SQ[:, :].rearrange("p (h d) -> p h d", d=64), axis=AX.X),
                         reads=[SQ], writes=[SS], partial=(c6 > 0))
                    o = 0 if c6 < 2 else 4
                    ps3 = ps[:, :].rearrange("p (h d) -> p h d", d=64)
                    x1, x2 = ps3[:, :, 0:32], ps3[:, :, 32:64]
                    bc = lambda i: GT[:, o + i, :].unsqueeze(1).to_broadcast([128, 8, 32])
                    r = [rt[(irt + i) % 8] for i in range(4)]
                    irt += 4
                    r3 = [r_[:, :].rearrange("p (h d) -> p h d", d=32) for r_ in r]
                    P.op("dve", lambda e: e.tensor_tensor(out=r3[0], in0=x1, in1=bc(0), op=ALU.mult), reads=[ps, GT], writes=[r[0]])
                    P.op("dve", lambda e: e.tensor_tensor(out=r3[1], in0=x2, in1=bc(1), op=ALU.mult), reads=[ps, GT], writes=[r[1]])
                    P.op("dve", lambda e: e.tensor_tensor(out=r3[2], in0=x2, in1=bc(2), op=ALU.mult), reads=[ps, GT], writes=[r[2]])
                    P.op("dve", lambda e: e.tensor_tensor(out=r3[3], in0=x1, in1=bc(3), op=ALU.mult), reads=[ps, GT], writes=[r[3]])
                    T3 = T[:, c6 * 512:(c6 + 1) * 512].rearrange("p (h d) -> p h d", d=64)
                    P.op("pool", lambda e: e.tensor_tensor(out=T3[:, :, 0:32], in0=r3[0], in1=r3[1], op=ALU.subtract), reads=[r[0], r[1]], writes=[T], partial=(c6 > 0))
                    P.op("pool", lambda e: e.tensor_tensor(out=T3[:, :, 32:64], in0=r3[2], in1=r3[3], op=ALU.add), reads=[r[2], r[3]], writes=[T], partial=True)
                P.op("act", lambda e: e.activation(out=SS[:, 0:16], in_=SS[:, 0:16], func=AF.Sqrt, scale=1.0 / 64, bias=epsq[:, 0:1]), reads=[SS, epsq], writes=[SS], partial=True)
                P.op("act", lambda e: e.activation(out=SS[:, 16:32], in_=SS[:, 16:32], func=AF.Sqrt, bias=epsq[:, 1:2]), reads=[SS, epsq], writes=[SS], partial=True)
                P.op("dve", lambda e: e.reciprocal(out=SS[:, :], in_=SS[:, :]), reads=[SS], writes=[SS], partial=True)
                v64 = lambda ap: ap.rearrange("p (h d) -> p h d", d=64)
                P.op("dve", lambda e: e.tensor_tensor(out=v64(QKB[:, 0:1024]), in0=v64(T[:, 0:1024]), in1=SS[:, 0:16].unsqueeze(2).to_broadcast([128, 16, 64]), op=ALU.mult),
                     reads=[T, SS], writes=[QKB])
                P.op("pool", lambda e: e.tensor_tensor(out=v64(QKB[:, 1024:2048]), in0=v64(T[:, 1024:2048]), in1=SS[:, 16:32].unsqueeze(2).to_broadcast([128, 16, 64]), op=ALU.mult),
                     reads=[T, SS], writes=[QKB], partial=True)
                P.dma("sp", C["a_v"][s * 3 + g, ti * 128:(ti + 1) * 128, :], VB[:], reads=[VB], writes=[C["a_v_buf"]], partial=True)
                for b4 in range(4):
                    pt = ptr[itr % 2]
                    itr += 1
                    for i in range(4):
                        hh = b4 * 4 + i
                        P.op("pe", lambda e, hh=hh, i=i: e.matmul(pt[:, i * 128:(i + 1) * 128], lhsT=QKB[:, hh * 128:(hh + 1) * 128], rhs=ident[:], start=True, stop=True),
                             reads=[QKB, ident], writes=[pt], signal=(i == 3))
                    P.op("act", lambda e, b4=b4: e.copy(out=QT[:, b4 * 4:(b4 + 1) * 4, :], in_=pt[:, :].rearrange("p (h t) -> p h t", t=128)),
                         reads=[pt], writes=[QT], partial=(b4 > 0))
                for q2 in range(2):
                    P.dma("sp", qkT_v[:, q2 * 8:(q2 + 1) * 8, ti * 128:(ti + 1) * 128], QT[:, q2 * 8:(q2 + 1) * 8, :], reads=[QT], writes=[C["a_qkT_buf"]], partial=True)
    P.end_stage()


def stage_a2(P, C):
    P.begin_stage()
    mask = P.sb([128, 256], BF16, "mask")
    P.dma("pool", mask[:], C["a_mask"], writes=[mask])
    ones = P.sb([128, 64], BF16, "ones64")
    P.op("dve", lambda e: e.memset(ones[:], 1.0), writes=[ones])
    qT = [[P.sb([64, 2048], BF16, "qT") for _ in range(2)] for _ in range(2)]
    kT = [[P.sb([64, 2048], BF16, "kT") for _ in range(2)] for _ in range(2)]
    Vp = [P.sb([128, 16, 128], BF16, "Vp") for _ in range(2)]
    PT = [P.sb([128, 256], BF16, "PT") for _ in range(3)]
    accn = [P.sb([128, 2048], F32, "accn") for _ in range(2)]
    accd = [P.sb([128, 2048], F32, "accd") for _ in range(2)]
    oTs = [P.sb([128, 2048], BF16, "oTs") for _ in range(2)]
    ps_s = [P.ps(name="ps_s") for _ in range(2)]
    psn = [P.ps(name="psn") for _ in range(2)]
    psd = [P.ps(name="psd") for _ in range(2)]
    ist = 0
    iq = 0
    units = [(s, hp, g) for s in range(2) for hp in range(8) for g in range(3)]

    def load_unit(u, i):
        s, hp, g = u
        sg = s * 3 + g
        for e_ in range(2):
            P.dma("sp", qT[i][e_][:], C["a_qkT"][sg, hp, e_ * 64:(e_ + 1) * 64, :], reads=[C["a_qkT_buf"]], writes=[qT[i][e_]])
            P.dma("sp", kT[i][e_][:], C["a_qkT"][sg, 8 + hp, e_ * 64:(e_ + 1) * 64, :], reads=[C["a_qkT_buf"]], writes=[kT[i][e_]])
        vsrc = C["a_v"][sg, :, hp * 128:(hp + 1) * 128].rearrange("(j p) n -> p j n", p=128)
        for h2 in range(2):
            P.dma("sp", Vp[i][:, h2 * 8:(h2 + 1) * 8, :], vsrc[:, h2 * 8:(h2 + 1) * 8, :], reads=[C["a_v_buf"]], writes=[Vp[i]], partial=(h2 > 0))

    load_unit(units[0], 0)
    steps = [(ui, e_, qb, blk) for ui in range(len(units)) for e_ in range(2) for qb in range(4) for blk in range(4)]
    banks = {}

    def emit_qk(idx):
        nonlocal ist
        ui, e_, qb, blk = steps[idx]
        s, hp, g = units[ui]
        i = ui % 2
        nb = 16 // A_DIL[g]
        QT, KT = qT[i][e_], kT[i][e_]
        tq = 4 * qb + blk
        has_prev = (tq % nb) != 0
        pss = ps_s[ist % 2]
        pt_ = PT[ist % 3]
        ist += 1
        c0 = 0 if has_prev else 128
        if has_prev:
            P.op("pe", lambda e: e.matmul(pss[:, 0:128], lhsT=KT[:, (tq - 1) * 128:tq * 128], rhs=QT[:, tq * 128:(tq + 1) * 128], start=True, stop=True),
                 reads=[KT, QT], writes=[pss], signal=False)
        P.op("pe", lambda e: e.matmul(pss[:, 128:256], lhsT=KT[:, tq * 128:(tq + 1) * 128], rhs=QT[:, tq * 128:(tq + 1) * 128], start=True, stop=True),
             reads=[KT, QT], writes=[pss], partial=has_prev)
        P.op("act", lambda e: e.activation(out=pt_[:, c0:256], in_=pss[:, c0:256], func=AF.Exp), reads=[pss], writes=[pt_])
        P.op("dve", lambda e: e.tensor_tensor(out=pt_[:, c0:256], in0=pt_[:, c0:256], in1=mask[:, c0:256], op=ALU.mult), reads=[pt_, mask], writes=[pt_])
        return pt_

    def emit_pv(idx, pt_):
        nonlocal iq
        ui, e_, qb, blk = steps[idx]
        s, hp, g = units[ui]
        i = ui % 2
        if e_ == 0 and qb == 0 and blk == 0 and ui + 1 < len(units):
            load_unit(units[ui + 1], (ui + 1) % 2)
        nb = 16 // A_DIL[g]
        V = Vp[i]
        e0 = e_ * 64
        tq = 4 * qb + blk
        has_prev = (tq % nb) != 0
        if blk == 0:
            banks[(ui, e_, qb)] = (psn[iq % 2], psd[iq % 2])
            iq += 1
        pn, pd = banks[(ui, e_, qb)]
        AN, AD = accn[(ui // 3) % 2], accd[(ui // 3) % 2]
        osl = slice(blk * 128, (blk + 1) * 128)
        if has_prev:
            P.op("pe", lambda e: e.matmul(pn[e0:e0 + 64, osl], lhsT=V[:, tq - 1, e0:e0 + 64], rhs=pt_[:, 0:128], start=True, stop=False),
                 reads=[V, pt_], writes=[pn], signal=False, partial=True)
        P.op("pe", lambda e: e.matmul(pn[e0:e0 + 64, osl], lhsT=V[:, tq, e0:e0 + 64], rhs=pt_[:, 128:256], start=(not has_prev), stop=True),
             reads=[V, pt_], writes=[pn], signal=False, partial=True)
        if has_prev:
            P.op("pe", lambda e: e.matmul(pd[e0:e0 + 64, osl], lhsT=ones[:], rhs=pt_[:, 0:128], start=True, stop=False),
                 reads=[ones, pt_], writes=[pd], signal=False, partial=True)
        P.op("pe", lambda e: e.matmul(pd[e0:e0 + 64, osl], lhsT=ones[:], rhs=pt_[:, 128:256], start=(not has_prev), stop=True),
             reads=[ones, pt_], writes=[pd], signal=True, partial=True)
        if blk == 3:
            for acc, src in ((AN, pn), (AD, pd)):
                if g == 0:
                    dst = acc[e0:e0 + 64, qb * 512:(qb + 1) * 512]
                    sv = src[e0:e0 + 64, :]
                    P.op("act", lambda e, dst=dst, sv=sv: e.copy(out=dst, in_=sv), reads=[src], writes=[acc], partial=True)
                else:
                    if g == 1:
                        dst = acc[e0:e0 + 64, :].rearrange("p (l r) -> p r l", r=4)[:, qb, :]
                        sv = src[e0:e0 + 64, :]
                    else:
                        dst = acc[e0:e0 + 64, :].rearrange("p (u r) -> p r u", r=16)[:, 4 * qb:4 * qb + 4, :]
                        sv = src[e0:e0 + 64, :].rearrange("p (b u) -> p b u", u=128)
                    P.op("dve", lambda e, dst=dst, sv=sv: e.tensor_tensor(out=dst, in0=dst, in1=sv, op=ALU.add), reads=[src, acc], writes=[acc], partial=True)
            if g == 2 and e_ == 1 and qb == 3:
                OT = oTs[(ui // 3) % 2]
                P.op("dve", lambda e: e.reciprocal(out=AD[:], in_=AD[:]), reads=[AD], writes=[AD])
                P.op("dve", lambda e: e.tensor_tensor(out=OT[:], in0=AN[:], in1=AD[:], op=ALU.mult), reads=[AN, AD], writes=[OT])
                P.dma("sp", C["oT"][hp, :, s * 2048:(s + 1) * 2048], OT[:], reads=[OT, AN, AD], writes=[C["oT_buf"]], partial=True)

    pend = emit_qk(0)
    for idx in range(len(steps)):
        nxt = emit_qk(idx + 1) if idx + 1 < len(steps) else None
        emit_pv(idx, pend)
        pend = nxt
    P.end_stage()


def stage_mix_a(P, C, S, D, li):
    import os
    dbg = os.environ.get("ADBG", "123")
    P.begin_group()
    cos = P.sb([128, 96, 32], F32, "cos")
    sin = P.sb([128, 96, 32], F32, "sin")
    stage_tables(P, C, "pos_a", "a_inv", 96, 32, cos, sin)
    if "1" in dbg:
        stage_a1(P, C, S, li, cos, sin)
    if "2" in dbg:
        stage_a2(P, C)
    if "3" in dbg:
        stage_outproj(P, C, S, D, C["a_w_out"][0], 8)
    P.end_group()


MIXERS[3] = stage_mix_d
MIXERS[2] = stage_mix_c
MIXERS[1] = stage_mix_b
MIXERS[0] = stage_mix_a


FULL_STAGES = []
for _li in range(4):
    FULL_STAGES += [("ffn", _li, 0), ("mix", _li), ("ffn", _li, 1)]

WEIGHT_SPECS = [
    ("ffn_w_in", [4, 2, 1024, 5632]), ("ffn_w_out", [4, 2, 2816, 1024]),
    ("a_w_in", [1, 1024, 9216]), ("a_w_out", [1, 1024, 1024]),
    ("b_w_in", [1, 1024, 672]), ("b_w_q_up", [1, 384, 1536]), ("b_w_kv_up", [1, 256, 2048]), ("b_w_out", [1, 1024, 1024]),
    ("c_w_in", [1, 1024, 6144]), ("c_w_out", [1, 2048, 1024]),
    ("d_w_in", [1, 1024, 2048]), ("d_w_rg", [1, 4, 256, 256]), ("d_w_ig", [1, 4, 256, 256]), ("d_w_out", [1, 1024, 1024]),
]


def build_program(stages):
    P = Prog()
    nc = P.nc
    C = {}
    x_in = nc.dram_tensor("x", [DM, NTOK], F32, kind="ExternalInput").ap()
    out = nc.dram_tensor("out", [DM, NTOK], F32, kind="ExternalOutput").ap()
    xs = nc.dram_tensor("xs", [DM, NTOK], F32, kind="Internal").ap()
    C["positions"] = nc.dram_tensor("positions", [2, SEQ], I32, kind="ExternalInput").ap()
    C["gains"] = nc.dram_tensor("gains", [4, 3, 128, 8], F32, kind="ExternalInput").ap()
    for name, shape in WEIGHT_SPECS:
        C[name] = nc.dram_tensor(name, shape, F32, kind="ExternalInput").ap()
    for name, shape in small_specs():
        C[name] = nc.dram_tensor(name, shape, F32, kind="ExternalInput").ap()
    for name, shape in int_specs():
        C[name] = nc.dram_tensor(name, shape, I32, kind="ExternalInput").ap()
    for name, shape in scratch_specs():
        C[name] = nc.dram_tensor(name, shape, BF16, kind="Internal").ap()
        C[name + "_buf"] = P.dbuf(C[name], name)
    Sx, So, Ss = Stream(P, x_in), Stream(P, out), Stream(P, xs)
    n = len(stages)
    for i, st in enumerate(stages):
        S = Sx if i == 0 else Ss
        D = So if i == n - 1 else Ss
        if st[0] == "ffn":
            stage_ffn(P, C, S, D, st[1], st[2])
        else:
            MIXERS[st[1] % 4](P, C, S, D, st[1])
    P.finish(So.bufs)
    return P


def host_inputs(inputs, core):
    m = {}
    xc = np.asarray(inputs["x"][2 * core:2 * core + 2], dtype=np.float32).reshape(NTOK, DM)
    m["x"] = np.ascontiguousarray(xc.T)
    m["positions"] = np.ascontiguousarray(np.asarray(inputs["positions"][2 * core:2 * core + 2], dtype=np.int32))
    g = np.asarray(inputs["norm_gains"], dtype=np.float32).reshape(4, 3, 8, 128)
    m["gains"] = np.ascontiguousarray(g.transpose(0, 1, 3, 2))
    for name, shape in WEIGHT_SPECS:
        m[name] = np.ascontiguousarray(np.asarray(inputs[name], dtype=np.float32))
    m.update(host_small(inputs))
    m.update(host_pos(m["positions"]))
    return m


_PROG_CACHE = {}


def run_stages(inputs, stages, cores):
    key = tuple(stages)
    if key not in _PROG_CACHE:
        _PROG_CACHE[key] = build_program(stages)
    P = _PROG_CACHE[key]
    shared = None
    in_maps = []
    for c in cores:
        m = host_inputs(inputs, c) if shared is None else dict(shared)
        if shared is None:
            shared = m
        else:
            xc = np.asarray(inputs["x"][2 * c:2 * c + 2], dtype=np.float32).reshape(NTOK, DM)
            m["x"] = np.ascontiguousarray(xc.T)
            m["positions"] = np.ascontiguousarray(np.asarray(inputs["positions"][2 * c:2 * c + 2], dtype=np.int32))
            m.update(host_pos(m["positions"]))
        in_maps.append(m)
    res = run_bass_kernel_spmd(P.nc, in_maps, core_ids=list(range(len(cores))))
    outs = [np.asarray(r["out"]).T.reshape(2, SEQ, DM) for r in res.results]
    return outs


def kernel(**inputs):
    outs = run_stages(inputs, FULL_STAGES, list(range(8)))
    return np.concatenate(outs, axis=0).astype(np.float32)
```

```python
import numpy as np
import concourse.bass as bass
import concourse.mybir as mybir
from concourse.bass_utils import run_bass_kernel_spmd
from contextlib import ExitStack

F32 = mybir.dt.float32
BF16 = mybir.dt.bfloat16
I32 = mybir.dt.int32
AF = mybir.ActivationFunctionType
ALU = mybir.AluOpType
AX = mybir.AxisListType


class DSem:
    __slots__ = ("sem", "cnt")

    def __init__(self, sem):
        self.sem = sem
        self.cnt = 0


class Buf:
    __slots__ = ("t", "writers", "reads", "ds", "name")

    def __init__(self, t, name=""):
        self.t = t
        self.writers = {}
        self.reads = {}
        self.ds = None
        self.name = name

    def __getitem__(self, k):
        return self.t[k]


class Eng:
    def __init__(self, name, h, sem):
        self.name = name
        self.h = h
        self.sem = sem
        self.count = 0
        self.seen = {}


class Prog:
    def __init__(self):
        self.nc = bass.Bass("TRN2", target_bir_lowering=False)
        nc = self.nc
        self.root = ExitStack()
        self.E = {}
        for name, h in (("pe", nc.tensor), ("act", nc.scalar), ("dve", nc.vector),
                        ("pool", nc.gpsimd), ("sp", nc.sync)):
            sem = self.root.enter_context(nc.semaphore("es_" + name))
            self.E[name] = Eng(name, h, sem)
        self.free_ds = []
        self.n_ds = 0
        self.stage = None
        self.group = None
        self.stage_bufs = []
        self.uid = 0
        self.gbufs = []

    def _scope(self):
        if self.stage is not None:
            return self.stage
        if self.group is not None:
            return self.group
        return self.root

    def begin_group(self):
        assert self.group is None and self.stage is None
        self.group = ExitStack()
        self.group_bufs = []

    def end_group(self):
        assert self.stage is None
        for b in self.group_bufs:
            if b.ds is not None:
                self.free_ds.append(b.ds)
                b.ds = None
        self.group.close()
        self.group = None
        self.group_bufs = []

    def sb(self, shape, dtype, name=None):
        self.uid += 1
        name = (name or "t") + "_%d" % self.uid
        t = self._scope().enter_context(self.nc.sbuf_tensor(name, list(shape), dtype))
        b = Buf(t, name)
        if self.stage is not None:
            self.stage_bufs.append(b)
        elif self.group is not None:
            self.group_bufs.append(b)
        return b

    def ps(self, shape=(128, 512), dtype=F32, name=None):
        self.uid += 1
        name = (name or "p") + "_%d" % self.uid
        t = self._scope().enter_context(self.nc.psum_tensor(name, list(shape), dtype))
        b = Buf(t, name)
        self.stage_bufs.append(b)
        return b

    def dram(self, name, shape, dtype, kind="Internal"):
        t = self.nc.dram_tensor(name, list(shape), dtype, kind=kind)
        return t

    def dbuf(self, ap, name=""):
        b = Buf(ap, name)
        self.gbufs.append(b)
        return b

    def _get_ds(self):
        if self.free_ds:
            return self.free_ds.pop()
        self.n_ds += 1
        sem = self.root.enter_context(self.nc.semaphore("ds_%d" % self.n_ds))
        return DSem(sem)

    def _deps(self, eng, reads, writes, is_dma=False, partial=False):
        need = {}

        def add(tok):
            if tok is None:
                return
            sem, val = tok
            k = id(sem)
            if k not in need or need[k][1] < val:
                need[k] = tok

        for b in reads:
            for tok in b.writers.values():
                add(tok)
        for b in writes:
            if not partial:
                for tok in b.writers.values():
                    if is_dma and b.ds is not None and tok[0] is b.ds.sem:
                        continue
                    add(tok)
            for tok in b.reads.values():
                add(tok)
        for k, (sem, val) in need.items():
            if sem is eng.sem:
                if eng.name == "pe":
                    continue
                if eng.count - val >= 2:
                    continue
            if eng.seen.get(k, 0) >= val:
                continue
            eng.h.wait_ge(sem, val)
            eng.seen[k] = val

    def _record(self, tok, reads, writes, partial=False):
        k = id(tok[0])
        for b in reads:
            old = b.reads.get(k)
            if old is None or old[1] < tok[1]:
                b.reads[k] = tok
        for b in writes:
            if partial:
                b.writers[k] = tok
            else:
                b.writers = {k: tok}
                b.reads = {}

    def op(self, en, fn, reads=(), writes=(), signal=True, partial=False):
        eng = self.E[en]
        self._deps(eng, reads, writes, partial=partial)
        ins = fn(eng.h)
        if signal:
            eng.count += 1
            ins.then_inc(eng.sem, 1)
            tok = (eng.sem, eng.count)
        else:
            tok = (eng.sem, eng.count + 1)
        self._record(tok, reads, writes, partial)
        return ins

    def dma(self, q, out_ap, in_ap, reads=(), writes=(), partial=False, **kw):
        eng = self.E[q]
        self._deps(eng, reads, writes, is_dma=True, partial=partial)
        dst = writes[0]
        if dst.ds is None:
            dst.ds = self._get_ds()
        ins = eng.h.dma_start(out=out_ap, in_=in_ap, **kw)
        dst.ds.cnt += 16
        ins.then_inc(dst.ds.sem, 16)
        tok = (dst.ds.sem, dst.ds.cnt)
        self._record(tok, reads, writes, partial)
        return ins

    def begin_stage(self):
        assert self.stage is None
        self.stage = ExitStack()
        self.stage_bufs = []

    def barrier(self, extra_bufs=()):
        toks = {}
        for e in self.E.values():
            if e.count > 0:
                toks[id(e.sem)] = (e.sem, e.count)
        for b in list(self.stage_bufs) + list(getattr(self, 'group_bufs', [])) + list(self.gbufs) + list(extra_bufs):
            if b.ds is not None and b.ds.cnt > 0:
                toks[id(b.ds.sem)] = (b.ds.sem, b.ds.cnt)
            for k, tok in b.reads.items():
                if k not in toks or toks[k][1] < tok[1]:
                    toks[k] = tok
            for k, tok in b.writers.items():
                if k not in toks or toks[k][1] < tok[1]:
                    toks[k] = tok
        for e in self.E.values():
            for k, (sem, val) in toks.items():
                if sem is e.sem:
                    continue
                if e.seen.get(k, 0) >= val:
                    continue
                e.h.wait_ge(sem, val)
                e.seen[k] = val

    def end_stage(self, extra_bufs=()):
        self.barrier(extra_bufs)
        for b in self.stage_bufs:
            if b.ds is not None:
                self.free_ds.append(b.ds)
                b.ds = None
        self.stage.close()
        self.stage = None
        self.stage_bufs = []

    def finish(self, out_bufs):
        self.barrier(out_bufs)


A_DIL = (1, 4, 16)


def a_tile_start(g, ti):
    dil = A_DIL[g]
    nb = 16 // dil
    r, n = ti // nb, ti % nb
    return n * 128 * dil + r, dil, nb


NTOK = 4096
SEQ = 2048
DM = 1024
EPS = 1e-6


class Stream:
    def __init__(self, P, ap, nb=4):
        self.ap = ap
        self.view = ap.rearrange("(k p) t -> p k t", p=128)
        self.nb = nb
        self.bsz = NTOK // nb
        self.bufs = [P.dbuf(ap, "strm") for _ in range(nb)]

    def buf(self, t0):
        return self.bufs[t0 // self.bsz]


class NormUnit:
    def __init__(self, P, TT, g_ap):
        self.P = P
        self.TT = TT
        self.gcol = P.sb([128, 8], F32, "gcol")
        P.dma("sp", self.gcol[:], g_ap, writes=[self.gcol])
        self.ones = P.sb([128, 128], BF16, "ones")
        P.op("dve", lambda e: e.memset(self.ones[:], 1.0), writes=[self.ones])
        self.epsc = P.sb([128, 1], F32, "epsc")
        P.op("dve", lambda e: e.memset(self.epsc[:], EPS), writes=[self.epsc])
        self.sq = [P.sb([128, 8, TT], BF16, "sq") for _ in range(2)]
        self.rs = [P.sb([128, TT], F32, "rs") for _ in range(2)]
        self.pss = P.ps(name="pss")
        self.i = 0

    def run(self, xt, xn):
        P, TT = self.P, self.TT
        sq = self.sq[self.i % 2]
        rs = self.rs[self.i % 2]
        self.i += 1
        pss, ones, gcol = self.pss, self.ones, self.gcol
        P.op("act", lambda e: e.activation(out=sq[:], in_=xt[:], func=AF.Square), reads=[xt], writes=[sq])
        for k in range(8):
            P.op("pe", lambda e, k=k: e.matmul(pss[:, :TT], lhsT=ones[:], rhs=sq[:, k, :], start=(k == 0), stop=(k == 7)),
                 reads=[ones, sq], writes=[pss], signal=(k == 7))
        P.op("act", lambda e: e.activation(out=rs[:], in_=pss[:, :TT], func=AF.Sqrt, scale=1.0 / DM, bias=self.epsc[:, 0:1]),
             reads=[pss, self.epsc], writes=[rs])
        P.op("dve", lambda e: e.reciprocal(out=rs[:], in_=rs[:]), reads=[rs], writes=[rs])
        for k in range(8):
            P.op("dve", lambda e, k=k: e.scalar_tensor_tensor(out=xn[:, k, :], in0=xt[:, k, :], scalar=gcol[:, k:k + 1], in1=rs[:],
                                                               op0=ALU.mult, op1=ALU.mult),
                 reads=[xt, gcol, rs], writes=[xn], partial=(k > 0))


def load_x(P, S, t0, TT, xt):
    P.dma("sp", xt[:], S.view[:, :, t0:t0 + TT], reads=[S.buf(t0)], writes=[xt])


def store_x(P, D, t0, TT, xt):
    P.dma("sp", D.view[:, :, t0:t0 + TT], xt[:], reads=[xt], writes=[D.buf(t0)], partial=True)


def stage_ffn(P, C, S, D, li, fi):
    TT = 256
    NT = NTOK // TT
    P.begin_stage()
    w_in = C["ffn_w_in"][li, fi].rearrange("(k p) n -> p k n", p=128)
    w_out = C["ffn_w_out"][li, fi].rearrange("(k p) n -> p k n", p=128)
    win = [P.sb([128, 8, 512], BF16, "win") for _ in range(11)]
    wout = [P.sb([128, 2, 1024], BF16, "wout") for _ in range(11)]
    N = NormUnit(P, TT, C["gains"][li, 0 if fi == 0 else 2])
    for g in [0, 5, 6, 1, 7, 2, 8, 3, 9, 4, 10]:
        P.dma("pool", win[g][:], w_in[:, :, g * 512:(g + 1) * 512], writes=[win[g]])
    for g in range(11):
        P.dma("pool", wout[g][:], w_out[:, 2 * g:2 * g + 2, :], writes=[wout[g]])
    xt = [P.sb([128, 8, TT], F32, "xt") for _ in range(2)]
    xn = [P.sb([128, 8, TT], BF16, "xn") for _ in range(2)]
    h = P.sb([128, 22, TT], BF16, "h")
    sg = [P.sb([128, TT], F32, "sg") for _ in range(2)]
    pg = [P.ps(name="pg") for _ in range(2)]
    pu = [P.ps(name="pu") for _ in range(2)]
    py = [P.ps(name="py") for _ in range(2)]

    load_x(P, S, 0, TT, xt[0])
    N.run(xt[0], xn[0])
    for t in range(NT):
        X, XN = xt[t % 2], xn[t % 2]
        if t + 1 < NT:
            load_x(P, S, (t + 1) * TT, TT, xt[(t + 1) % 2])
        for m in range(22):
            a, b, s = pg[m % 2], pu[m % 2], sg[m % 2]
            c0 = m * 128
            gi, go = c0 // 512, c0 % 512
            for k in range(8):
                P.op("pe", lambda e, k=k, gi=gi, go=go: e.matmul(a[:, :TT], lhsT=win[gi][:, k, go:go + 128], rhs=XN[:, k, :],
                                                                 start=(k == 0), stop=(k == 7)),
                     reads=[win[gi], XN], writes=[a], signal=(k == 7))
            c0 = 2816 + m * 128
            gi, go = c0 // 512, c0 % 512
            for k in range(8):
                P.op("pe", lambda e, k=k, gi=gi, go=go: e.matmul(b[:, :TT], lhsT=win[gi][:, k, go:go + 128], rhs=XN[:, k, :],
                                                                 start=(k == 0), stop=(k == 7)),
                     reads=[win[gi], XN], writes=[b], signal=(k == 7))
            P.op("act", lambda e: e.activation(out=s[:], in_=a[:, :TT], func=AF.Silu), reads=[a], writes=[s])
            P.op("dve", lambda e, m=m: e.tensor_tensor(out=h[:, m, :], in0=s[:], in1=b[:, :TT], op=ALU.mult),
                 reads=[s, b], writes=[h], partial=(m > 0))
        if t + 1 < NT:
            N.run(xt[(t + 1) % 2], xn[(t + 1) % 2])
        for mo in range(8):
            c = py[mo % 2]
            for kf in range(22):
                P.op("pe", lambda e, kf=kf, mo=mo: e.matmul(c[:, :TT], lhsT=wout[kf // 2][:, kf % 2, mo * 128:(mo + 1) * 128],
                                                            rhs=h[:, kf, :], start=(kf == 0), stop=(kf == 21)),
                     reads=[wout[kf // 2], h], writes=[c], signal=(kf == 21))
            P.op("dve", lambda e, mo=mo: e.scalar_tensor_tensor(out=X[:, mo, :], in0=c[:, :TT], scalar=0.5, in1=X[:, mo, :],
                                                                 op0=ALU.mult, op1=ALU.add),
                 reads=[c, X], writes=[X], partial=True)
        store_x(P, D, t * TT, TT, X)
    P.end_stage()


def small_specs():
    return [("d_small", [128, 8, 8]), ("ident", [128, 128]), ("c_zeta", [128, 8]), ("c_inv", [128, 64]),
            ("c_decT", [128, 8, 128]), ("c_xi", [128, 8, 128]),
            ("b_grow", [128, 832]), ("b_inv", [128, 16]), ("b_mask", [128, 4, 512]),
            ("a_grow", [128, 128]), ("a_inv", [128, 32]), ("a_mask", [128, 256])]


def int_specs():
    return [("pos_pm", [128, 32]), ("pos_a", [128, 96])]


def scratch_specs():
    return [("c_qkT", [16, 128, NTOK]), ("c_kz", [NTOK, 1024]), ("c_vg", [NTOK, 4096]), ("oT", [16, 128, NTOK]),
            ("b_qkT", [32, 96, NTOK]), ("b_v", [NTOK, 1024]),
            ("a_qkT", [6, 16, 128, SEQ]), ("a_v", [6, SEQ, 1024])]


def host_small(inputs):
    m = {}
    f = lambda a: np.asarray(a, dtype=np.float32)
    cols = [f(inputs["d_conv_w"])[0, j] for j in range(4)] + [f(inputs["d_conv_b"])[0], f(inputs["d_b_rg"])[0],
                                                            f(inputs["d_b_ig"])[0], f(inputs["d_lru_param"])[0]]
    ds = np.stack(cols, axis=-1)
    m["d_small"] = np.ascontiguousarray(ds.reshape(8, 128, 8).transpose(1, 0, 2))
    m["ident"] = np.eye(128, dtype=np.float32)
    lg = np.log(np.float32(1.0) - np.float32(2.0) ** (-5.0 - np.arange(8, dtype=np.float32))).astype(np.float32)
    idx = np.arange(128, dtype=np.float32)
    sc = np.float32(128.0 ** -0.5)
    m["c_zeta"] = np.ascontiguousarray(np.exp((127.0 - idx)[:, None] * lg[None, :]).astype(np.float32))
    m["c_inv"] = np.ascontiguousarray(np.broadcast_to((10000.0 ** (-np.arange(64, dtype=np.float32) * 2.0 / 128.0)).astype(np.float32), (128, 64)))
    diff = idx[None, :] - idx[:, None]
    dec = np.where((diff >= 0)[:, None, :], np.exp(np.where(diff >= 0, diff, 0.0)[:, None, :] * lg[None, :, None]), 0.0)
    m["c_decT"] = np.ascontiguousarray((dec * sc).astype(np.float32))
    xi = np.exp((idx + 1.0)[None, :] * lg[:, None]) * sc
    m["c_xi"] = np.ascontiguousarray(np.broadcast_to(xi[None].astype(np.float32), (128, 8, 128)))
    grow = np.concatenate([f(inputs["b_q_a_gain"])[0], f(inputs["b_kv_a_gain"])[0], f(inputs["b_q_gain"])[0], f(inputs["b_k_gain"])[0]])
    m["b_grow"] = np.ascontiguousarray(np.broadcast_to(grow[None, :], (128, 832)))
    m["b_inv"] = np.ascontiguousarray(np.broadcast_to((10000.0 ** (-np.arange(16, dtype=np.float32) * 2.0 / 32.0)).astype(np.float32), (128, 16)))
    key = np.arange(128)[:, None]
    col = np.arange(512)[None, :]
    m["b_mask"] = np.ascontiguousarray(np.stack([(col - i * 128 >= key) for i in range(4)], axis=1).astype(np.float32))
    agrow = np.concatenate([f(inputs["a_q_gain"])[0], f(inputs["a_k_gain"])[0]])
    m["a_grow"] = np.ascontiguousarray(np.broadcast_to(agrow[None, :], (128, 128)))
    m["a_inv"] = np.ascontiguousarray(np.broadcast_to((10000.0 ** (-np.arange(32, dtype=np.float32) * 2.0 / 64.0)).astype(np.float32), (128, 32)))
    q = np.arange(128)[None, :]
    m["a_mask"] = np.ascontiguousarray(np.concatenate([(key >= q), (key <= q)], axis=1).astype(np.float32))
    return m


def host_pos(pos):
    m = {}
    m["pos_pm"] = np.ascontiguousarray(pos.reshape(32, 128).T)
    cols = []
    for s_ in range(2):
        for g in range(3):
            for ti in range(16):
                st, dil, nb = a_tile_start(g, ti)
                cols.append(pos[s_, st:st + 127 * dil + 1:dil])
    m["pos_a"] = np.ascontiguousarray(np.stack(cols, axis=1).astype(np.int32))
    return m


MIXERS = {}


def stage_mix_d(P, C, S, D, li):
    TT = 512
    P.begin_stage()
    wi_d = C["d_w_in"][0].rearrange("(k p) n -> p k n", p=128)
    wo_d = C["d_w_out"][0].rearrange("(k p) n -> p k n", p=128)
    wrg_d = C["d_w_rg"][0].rearrange("n (cc p) d -> p (n cc) d", p=128)
    wig_d = C["d_w_ig"][0].rearrange("n (cc p) d -> p (n cc) d", p=128)
    N = NormUnit(P, TT, C["gains"][li, 1])
    wi = [P.sb([128, 8, 512], BF16, "wi") for _ in range(4)]
    for g in [2, 0, 3, 1]:
        P.dma("pool", wi[g][:], wi_d[:, :, g * 512:(g + 1) * 512], writes=[wi[g]])
    wrg = P.sb([128, 8, 256], BF16, "wrg")
    wig = P.sb([128, 8, 256], BF16, "wig")
    wo = P.sb([128, 8, 1024], BF16, "wo")
    P.dma("pool", wrg[:], wrg_d, writes=[wrg])
    P.dma("pool", wig[:], wig_d, writes=[wig])
    P.dma("pool", wo[:], wo_d, writes=[wo])
    sm = P.sb([128, 8, 8], F32, "dsm")
    P.dma("sp", sm[:], C["d_small"], writes=[sm])
    nsp = P.sb([128, 8], F32, "nsp")
    onec = P.sb([128, 1], F32, "onec")
    P.op("dve", lambda e: e.memset(onec[:], 1.0), writes=[onec])
    P.op("act", lambda e: e.activation(out=nsp[:], in_=sm[:, :, 7], func=AF.Exp, scale=-1.0), reads=[sm], writes=[nsp])
    P.op("act", lambda e: e.activation(out=nsp[:], in_=nsp[:], func=AF.Ln, bias=onec[:, 0:1]), reads=[nsp, onec], writes=[nsp])
    P.op("dve", lambda e: e.tensor_scalar(out=nsp[:], in0=nsp[:], scalar1=-8.0, scalar2=None, op0=ALU.mult), reads=[nsp], writes=[nsp])

    xt = [P.sb([128, 8, TT], F32, "xt") for _ in range(2)]
    xn = [P.sb([128, 8, TT], BF16, "xn") for _ in range(2)]
    og = P.sb([128, 8, TT], BF16, "og")
    ub = [P.sb([128, 2, TT + 3], F32, "ub") for _ in range(2)]
    uc = [P.sb([128, 2, TT], F32, "uc") for _ in range(2)]
    ucb = [P.sb([128, 2, TT], BF16, "ucb") for _ in range(2)]
    carry_u = P.sb([128, 8, 3], F32, "carry_u")
    carry_h = P.sb([128, 8], F32, "carry_h")
    tmp = {nm: [P.sb([128, TT], F32, nm) for _ in range(2)] for nm in ("r", "ig", "a", "s", "b", "hs", "gl")}
    pu = [P.ps(name="pu") for _ in range(2)]
    pr, pi, pgt = P.ps(name="pr"), P.ps(name="pi"), P.ps(name="pgt")
    py = [P.ps(name="py") for _ in range(2)]

    NT = NTOK // TT
    load_x(P, S, 0, TT, xt[0])
    N.run(xt[0], xn[0])
    it = 0
    for t in range(NT):
        X, XN = xt[t % 2], xn[t % 2]
        if t + 1 < NT:
            load_x(P, S, (t + 1) * TT, TT, xt[(t + 1) % 2])
        if t % (SEQ // TT) == 0:
            P.op("pool", lambda e: e.memset(carry_u[:], 0.0), writes=[carry_u])
            P.op("pool", lambda e: e.memset(carry_h[:], 0.0), writes=[carry_h])
        for n in range(4):
            UB, UC, UCB = ub[n % 2], uc[n % 2], ucb[n % 2]
            for cc in range(2):
                c = 2 * n + cc
                p_u = pu[cc]
                col = 1024 + c * 128
                gi, go = col // 512, col % 512
                for k in range(8):
                    P.op("pe", lambda e, k=k, gi=gi, go=go: e.matmul(p_u[:, :TT], lhsT=wi[gi][:, k, go:go + 128], rhs=XN[:, k, :],
                                                                     start=(k == 0), stop=(k == 7)),
                         reads=[wi[gi], XN], writes=[p_u], signal=(k == 7))
                P.op("pool", lambda e, cc=cc, c=c: e.tensor_copy(out=UB[:, cc, 0:3], in_=carry_u[:, c, :]), reads=[carry_u], writes=[UB], partial=(cc > 0))
                P.op("act", lambda e, cc=cc: e.copy(out=UB[:, cc, 3:3 + TT], in_=p_u[:, :TT]), reads=[p_u], writes=[UB], partial=True)
                P.op("pool", lambda e, cc=cc, c=c: e.tensor_copy(out=carry_u[:, c, :], in_=UB[:, cc, TT:TT + 3]), reads=[UB], writes=[carry_u], partial=True)
                P.op("dve", lambda e, cc=cc, c=c: e.tensor_scalar(out=UC[:, cc, :], in0=UB[:, cc, 3:3 + TT], scalar1=sm[:, c, 3:4], scalar2=sm[:, c, 4:5],
                                                                   op0=ALU.mult, op1=ALU.add), reads=[UB, sm], writes=[UC], partial=(cc > 0))
                for j in range(3):
                    P.op("dve", lambda e, cc=cc, c=c, j=j: e.scalar_tensor_tensor(out=UC[:, cc, :], in0=UB[:, cc, j:j + TT], scalar=sm[:, c, j:j + 1], in1=UC[:, cc, :],
                                                                                   op0=ALU.mult, op1=ALU.add), reads=[UB, sm, UC], writes=[UC], partial=True)
                P.op("act", lambda e, cc=cc: e.copy(out=UCB[:, cc, :], in_=UC[:, cc, :]), reads=[UC], writes=[UCB], partial=(cc > 0))
            for dc in range(2):
                c = 2 * n + dc
                i2 = it % 2
                it += 1
                r, ig, a, s, b, hs, gl = (tmp[nm][i2] for nm in ("r", "ig", "a", "s", "b", "hs", "gl"))
                for cc in range(2):
                    P.op("pe", lambda e, cc=cc, dc=dc, n=n: e.matmul(pr[:, :TT], lhsT=wrg[:, 2 * n + cc, dc * 128:(dc + 1) * 128], rhs=UCB[:, cc, :],
                                                                     start=(cc == 0), stop=(cc == 1)), reads=[wrg, UCB], writes=[pr], signal=(cc == 1))
                for cc in range(2):
                    P.op("pe", lambda e, cc=cc, dc=dc, n=n: e.matmul(pi[:, :TT], lhsT=wig[:, 2 * n + cc, dc * 128:(dc + 1) * 128], rhs=UCB[:, cc, :],
                                                                     start=(cc == 0), stop=(cc == 1)), reads=[wig, UCB], writes=[pi], signal=(cc == 1))
                gi, go = (c * 128) // 512, (c * 128) % 512
                for k in range(8):
                    P.op("pe", lambda e, k=k, gi=gi, go=go: e.matmul(pgt[:, :TT], lhsT=wi[gi][:, k, go:go + 128], rhs=XN[:, k, :],
                                                                     start=(k == 0), stop=(k == 7)),
                         reads=[wi[gi], XN], writes=[pgt], signal=(k == 7))
                P.op("act", lambda e, c=c: e.activation(out=r[:], in_=pr[:, :TT], func=AF.Sigmoid, bias=sm[:, c, 5:6]), reads=[pr, sm], writes=[r])
                P.op("act", lambda e, c=c: e.activation(out=ig[:], in_=pi[:, :TT], func=AF.Sigmoid, bias=sm[:, c, 6:7]), reads=[pi, sm], writes=[ig])
                P.op("act", lambda e, c=c: e.activation(out=a[:], in_=r[:], func=AF.Exp, scale=nsp[:, c:c + 1]), reads=[r, nsp], writes=[a])
                P.op("act", lambda e: e.activation(out=s[:], in_=a[:], func=AF.Square), reads=[a], writes=[s])
                P.op("act", lambda e: e.activation(out=s[:], in_=s[:], func=AF.Sqrt, scale=-1.0, bias=onec[:, 0:1]), reads=[s, onec], writes=[s])
                P.op("dve", lambda e, dc=dc: e.tensor_tensor(out=b[:], in0=ig[:], in1=UC[:, dc, :], op=ALU.mult), reads=[ig, UC], writes=[b])
                P.op("dve", lambda e: e.tensor_tensor(out=b[:], in0=b[:], in1=s[:], op=ALU.mult), reads=[b, s], writes=[b])
                P.op("dve", lambda e, c=c: e.tensor_tensor_scan(out=hs[:], data0=a[:], data1=b[:], initial=carry_h[:, c:c + 1], op0=ALU.mult, op1=ALU.add),
                     reads=[a, b, carry_h], writes=[hs])
                P.op("pool", lambda e, c=c: e.tensor_copy(out=carry_h[:, c:c + 1], in_=hs[:, TT - 1:TT]), reads=[hs], writes=[carry_h], partial=True)
                P.op("act", lambda e: e.activation(out=gl[:], in_=pgt[:, :TT], func=AF.Gelu_apprx_tanh), reads=[pgt], writes=[gl])
                P.op("dve", lambda e, c=c: e.tensor_tensor(out=og[:, c, :], in0=gl[:], in1=hs[:], op=ALU.mult), reads=[gl, hs], writes=[og], partial=(c > 0))
        if t + 1 < NT:
            N.run(xt[(t + 1) % 2], xn[(t + 1) % 2])
        for mo in range(8):
            cps = py[mo % 2]
            for k in range(8):
                P.op("pe", lambda e, k=k, mo=mo: e.matmul(cps[:, :TT], lhsT=wo[:, k, mo * 128:(mo + 1) * 128], rhs=og[:, k, :],
                                                          start=(k == 0), stop=(k == 7)), reads=[wo, og], writes=[cps], signal=(k == 7))
            P.op("dve", lambda e, mo=mo: e.tensor_tensor(out=X[:, mo, :], in0=cps[:, :TT], in1=X[:, mo, :], op=ALU.add),
                 reads=[cps, X], writes=[X], partial=True)
        store_x(P, D, t * TT, TT, X)
    P.end_stage()


TWO_PI = 6.283185307179586
C1_2PI = 6.28125
C2_2PI = TWO_PI - C1_2PI


def mm_group(P, ps_ap, ps_buf, pairs, reads_extra=()):
    n = len(pairs)
    for i, (l, r, bufs) in enumerate(pairs):
        P.op("pe", lambda e, l=l, r=r, i=i: e.matmul(ps_ap, lhsT=l, rhs=r, start=(i == 0), stop=(i == n - 1)),
             reads=list(bufs) + list(reads_extra), writes=[ps_buf], signal=(i == n - 1))


def rope_tables(P, posi, inv, nt, half, cos, sin):
    posf = P.sb([128, nt], F32, "posf")
    ang = P.sb([128, nt, half], F32, "ang")
    a2 = P.sb([128, nt, half], F32, "a2")
    ni = P.sb([128, nt, half], I32, "ni")
    nf = P.sb([128, nt, half], F32, "nf")
    P.op("dve", lambda e: e.tensor_copy(out=posf[:], in_=posi[:]), reads=[posi], writes=[posf])
    P.op("dve", lambda e: e.tensor_tensor(out=ang[:], in0=posf[:, :].unsqueeze(2).to_broadcast([128, nt, half]),
                                          in1=inv[:, :].unsqueeze(1).to_broadcast([128, nt, half]), op=ALU.mult),
         reads=[posf, inv], writes=[ang])
    for dst, shift in ((sin, 0.0), (cos, 1.5707963267948966)):
        P.op("dve", lambda e, shift=shift: e.tensor_scalar(out=a2[:], in0=ang[:], scalar1=shift, scalar2=None, op0=ALU.add), reads=[ang], writes=[a2])
        P.op("dve", lambda e: e.tensor_scalar(out=ni[:], in0=a2[:], scalar1=1.0 / TWO_PI, scalar2=None, op0=ALU.mult), reads=[a2], writes=[ni])
        P.op("dve", lambda e: e.tensor_copy(out=nf[:], in_=ni[:]), reads=[ni], writes=[nf])
        P.op("dve", lambda e: e.scalar_tensor_tensor(out=a2[:], in0=nf[:], scalar=-C1_2PI, in1=a2[:], op0=ALU.mult, op1=ALU.add), reads=[nf, a2], writes=[a2])
        P.op("dve", lambda e: e.scalar_tensor_tensor(out=a2[:], in0=nf[:], scalar=-C2_2PI, in1=a2[:], op0=ALU.mult, op1=ALU.add), reads=[nf, a2], writes=[a2])
        P.op("dve", lambda e: e.tensor_scalar(out=a2[:], in0=a2[:], scalar1=3.1415925, scalar2=-3.1415925, op0=ALU.min, op1=ALU.max), reads=[a2], writes=[a2])
        P.op("act", lambda e, dst=dst: e.activation(out=dst[:], in_=a2[:], func=AF.Sin), reads=[a2], writes=[dst])


def rope_apply(P, ps3, psbuf, out3, outbuf, cosb, sinb, tabs, tmp, shape, partial):
    H, half = shape
    x1, x2 = ps3[:, :, 0:half], ps3[:, :, half:2 * half]
    v = lambda b: b[:, :].rearrange("p (h d) -> p h d", d=half)
    t1, t2, t3, t4 = tmp
    P.op("dve", lambda e: e.tensor_tensor(out=v(t1), in0=x1, in1=cosb, op=ALU.mult), reads=[psbuf] + tabs, writes=[t1])
    P.op("dve", lambda e: e.tensor_tensor(out=v(t2), in0=x2, in1=sinb, op=ALU.mult), reads=[psbuf] + tabs, writes=[t2])
    P.op("dve", lambda e: e.tensor_tensor(out=v(t3), in0=x2, in1=cosb, op=ALU.mult), reads=[psbuf] + tabs, writes=[t3])
    P.op("dve", lambda e: e.tensor_tensor(out=v(t4), in0=x1, in1=sinb, op=ALU.mult), reads=[psbuf] + tabs, writes=[t4])
    P.op("pool", lambda e: e.tensor_tensor(out=out3[:, :, 0:half], in0=v(t1), in1=v(t2), op=ALU.subtract), reads=[t1, t2], writes=[outbuf], partial=partial)
    P.op("pool", lambda e: e.tensor_tensor(out=out3[:, :, half:2 * half], in0=v(t3), in1=v(t4), op=ALU.add), reads=[t3, t4], writes=[outbuf], partial=True)


def stage_tables(P, C, pos_key, inv_key, nt, half, cos, sin):
    P.begin_stage()
    inv = P.sb([128, half], F32, "inv")
    P.dma("sp", inv[:], C[inv_key], writes=[inv])
    posi = P.sb([128, nt], I32, "posi")
    P.dma("sp", posi[:], C[pos_key], writes=[posi])
    rope_tables(P, posi, inv, nt, half, cos, sin)
    P.end_stage()


def stage_c1a(P, C, S, li, cos, sin):
    TT = 512
    P.begin_stage()
    w_d = C["c_w_in"][0].rearrange("(k p) n -> p k n", p=128)
    wc = [P.sb([128, 8, 512], BF16, "wc") for _ in range(4)]
    for g in range(4):
        P.dma("pool", wc[g][:], w_d[:, :, g * 512:(g + 1) * 512], writes=[wc[g]])
    N = NormUnit(P, TT, C["gains"][li, 1])
    ident = P.sb([128, 128], BF16, "ident")
    P.dma("pool", ident[:], C["ident"], writes=[ident])
    zeta = P.sb([128, 8], F32, "zeta")
    P.dma("sp", zeta[:], C["c_zeta"], writes=[zeta])
    xt = P.sb([128, 8, TT], F32, "xt")
    xn = [P.sb([128, 8, TT], BF16, "xn") for _ in range(2)]
    qk = [P.sb([128, 2048], BF16, "qk") for _ in range(2)]
    kz = [P.sb([128, 1024], BF16, "kz") for _ in range(2)]
    tmp = [P.sb([128, 256], F32, "rt") for _ in range(8)]
    qTs = [P.sb([128, 16, TT], BF16, "qTs") for _ in range(2)]
    pp = [P.ps(name="pp") for _ in range(3)]
    ptr = [P.ps(name="ptr") for _ in range(2)]
    NT = NTOK // TT
    qkT_v = C["c_qkT"].rearrange("h d t -> d h t")
    ipp = 0
    itr = 0
    load_x(P, S, 0, TT, xt)
    N.run(xt, xn[0])
    for t in range(NT):
        XN = xn[t % 2]
        QT = qTs[t % 2]
        if t + 1 < NT:
            load_x(P, S, (t + 1) * TT, TT, xt)
        for sub in range(4):
            j = t * 4 + sub
            QK = qk[j % 2]
            KZ = kz[j % 2]
            for cg in range(4):
                ps = pp[ipp % 3]
                ipp += 1
                mm_group(P, ps[:, :], ps, [(XN[:, k, sub * 128:(sub + 1) * 128], wc[cg][:, k, :], [XN, wc[cg]]) for k in range(8)])
                cb = cos[:, j, :].unsqueeze(1).to_broadcast([128, 4, 64])
                sb_ = sin[:, j, :].unsqueeze(1).to_broadcast([128, 4, 64])
                ps3 = ps[:, :].rearrange("p (h d) -> p h d", d=128)
                out3 = QK[:, cg * 512:(cg + 1) * 512].rearrange("p (h d) -> p h d", d=128)
                rope_apply(P, ps3, ps, out3, QK, cb, sb_, [cos, sin], tmp[4 * (cg % 2):4 * (cg % 2) + 4], (4, 64), partial=(cg > 0))
            P.op("dve", lambda e: e.tensor_tensor(out=KZ[:, :].rearrange("p (h d) -> p h d", d=128),
                                                  in0=QK[:, 1024:2048].rearrange("p (h d) -> p h d", d=128),
                                                  in1=zeta[:, :].unsqueeze(2).to_broadcast([128, 8, 128]), op=ALU.mult),
                 reads=[QK, zeta], writes=[KZ])
            P.dma("sp", C["c_kz"][j * 128:(j + 1) * 128, :], KZ[:], reads=[KZ], writes=[C["c_kz_buf"]], partial=True)
            for b4 in range(4):
                pt = ptr[itr % 2]
                itr += 1
                for i in range(4):
                    hh = b4 * 4 + i
                    P.op("pe", lambda e, hh=hh, i=i: e.matmul(pt[:, i * 128:(i + 1) * 128], lhsT=QK[:, hh * 128:(hh + 1) * 128], rhs=ident[:],
                                                              start=True, stop=True), reads=[QK, ident], writes=[pt], signal=(i == 3))
                P.op("act", lambda e, b4=b4: e.copy(out=QT[:, b4 * 4:(b4 + 1) * 4, sub * 128:(sub + 1) * 128],
                                                    in_=pt[:, :].rearrange("p (h t) -> p h t", t=128)),
                     reads=[pt], writes=[QT], partial=not (sub == 0 and b4 == 0))
        P.dma("sp", qkT_v[:, :, t * TT:(t + 1) * TT], QT[:], reads=[QT], writes=[C["c_qkT_buf"]], partial=True)
        if t + 1 < NT:
            N.run(xt, xn[(t + 1) % 2])
    P.end_stage()


def stage_c1b(P, C, S, li):
    TT = 512
    P.begin_stage()
    w_d = C["c_w_in"][0].rearrange("(k p) n -> p k n", p=128)
    wc = [P.sb([128, 8, 512], BF16, "wc") for _ in range(8)]
    for g in range(8):
        P.dma("pool", wc[g][:], w_d[:, :, (4 + g) * 512:(5 + g) * 512], writes=[wc[g]])
    N = NormUnit(P, TT, C["gains"][li, 1])
    xt = P.sb([128, 8, TT], F32, "xt")
    xn = [P.sb([128, 8, TT], BF16, "xn") for _ in range(2)]
    vg = [P.sb([128, 4096], BF16, "vg") for _ in range(2)]
    pp = [P.ps(name="pp") for _ in range(4)]
    NT = NTOK // TT
    ipp = 0
    load_x(P, S, 0, TT, xt)
    N.run(xt, xn[0])
    for t in range(NT):
        XN = xn[t % 2]
        if t + 1 < NT:
            load_x(P, S, (t + 1) * TT, TT, xt)
        for sub in range(4):
            j = t * 4 + sub
            VG = vg[j % 2]
            for cg in range(8):
                ps = pp[ipp % 4]
                ipp += 1
                mm_group(P, ps[:, :], ps, [(XN[:, k, sub * 128:(sub + 1) * 128], wc[cg][:, k, :], [XN, wc[cg]]) for k in range(8)])
                if cg < 4:
                    P.op("dve", lambda e, cg=cg: e.tensor_copy(out=VG[:, cg * 512:(cg + 1) * 512], in_=ps[:, :]), reads=[ps], writes=[VG], partial=(cg > 0))
                else:
                    P.op("act", lambda e, cg=cg: e.activation(out=VG[:, cg * 512:(cg + 1) * 512], in_=ps[:, :], func=AF.Silu), reads=[ps], writes=[VG], partial=True)
            P.dma("sp", C["c_vg"][j * 128:(j + 1) * 128, :], VG[:], reads=[VG], writes=[C["c_vg_buf"]], partial=True)
        if t + 1 < NT:
            N.run(xt, xn[(t + 1) % 2])
    P.end_stage()


def stage_c2(P, C):
    P.begin_stage()
    ident = P.sb([128, 128], BF16, "ident")
    P.dma("pool", ident[:], C["ident"], writes=[ident])
    decT = P.sb([128, 8, 128], F32, "decT")
    P.dma("sp", decT[:], C["c_decT"], writes=[decT])
    xib = P.sb([128, 8, 128], F32, "xib")
    P.dma("sp", xib[:], C["c_xi"], writes=[xib])
    eps5 = P.sb([128, 1], F32, "eps5")
    P.op("dve", lambda e: e.memset(eps5[:], 1e-5), writes=[eps5])
    lg = np.log(np.float32(1.0) - np.float32(2.0) ** (-5.0 - np.arange(8, dtype=np.float32))).astype(np.float32)
    cd = [float(np.exp(np.float32(128.0) * lg[h])) for h in range(8)]
    qkc = [P.sb([128, 16, 128], BF16, "qkc") for _ in range(2)]
    kzc = [P.sb([128, 1024], BF16, "kzc") for _ in range(2)]
    vgc = [P.sb([128, 4096], BF16, "vgc") for _ in range(2)]
    qx = [P.sb([128, 8, 128], BF16, "qx") for _ in range(2)]
    R = P.sb([128, 8, 256], F32, "R")
    Rb = P.sb([128, 8, 256], BF16, "Rb")
    AT = [P.sb([128, 128], BF16, "AT") for _ in range(2)]
    st = [P.sb([128, 6], F32, "st") for _ in range(2)]
    mv = [P.sb([128, 2], F32, "mv") for _ in range(2)]
    rstd = [P.sb([128, 1], F32, "rstd") for _ in range(2)]
    yn = [P.sb([128, 256], F32, "yn") for _ in range(2)]
    ob = [P.sb([128, 2048], BF16, "ob") for _ in range(2)]
    oTs = [P.sb([128, 16, 512], BF16, "oTs") for _ in range(2)]
    ps_s = [P.ps(name="ps_s") for _ in range(2)]
    ps_y = [P.ps(name="ps_y") for _ in range(2)]
    ps_u = [P.ps(name="ps_u") for _ in range(2)]
    ptr = [P.ps(name="ptr") for _ in range(2)]
    qkT_v = C["c_qkT"].rearrange("h d t -> d h t")
    oT_v = C["oT"].rearrange("k f t -> f k t")
    it = 0
    itr = 0

    def load_chunk(g):
        i = g % 2
        P.dma("sp", qkc[i][:], qkT_v[:, :, g * 128:(g + 1) * 128], reads=[C["c_qkT_buf"]], writes=[qkc[i]])
        P.dma("sp", kzc[i][:], C["c_kz"][g * 128:(g + 1) * 128, :], reads=[C["c_kz_buf"]], writes=[kzc[i]])
        P.dma("sp", vgc[i][:], C["c_vg"][g * 128:(g + 1) * 128, :], reads=[C["c_vg_buf"]], writes=[vgc[i]])

    load_chunk(0)
    for g in range(32):
        c = g % 16
        QKC, KZC, VGC, QX, OB = qkc[g % 2], kzc[g % 2], vgc[g % 2], qx[g % 2], ob[g % 2]
        OT = oTs[(g // 4) % 2]
        if g + 1 < 32:
            load_chunk(g + 1)
        if c > 0:
            P.op("pool", lambda e: e.tensor_tensor(out=QX[:], in0=QKC[:, 0:8, :], in1=xib[:], op=ALU.mult), reads=[QKC, xib], writes=[QX])
        for h in range(8):
            i2 = it % 2
            it += 1
            pss, psy, psu = ps_s[i2], ps_y[i2], ps_u[i2]
            vh = VGC[:, h * 256:(h + 1) * 256]
            P.op("pe", lambda e, h=h: e.matmul(pss[:, 0:128], lhsT=QKC[:, 8 + h, :], rhs=QKC[:, h, :], start=True, stop=True), reads=[QKC], writes=[pss])
            P.op("dve", lambda e, h=h: e.tensor_tensor(out=AT[i2][:], in0=pss[:, 0:128], in1=decT[:, h, :], op=ALU.mult), reads=[pss, decT], writes=[AT[i2]])
            if c > 0:
                mm_group(P, psy[:, 0:256], psy, [(AT[i2][:], vh, [AT[i2], VGC]), (QX[:, h, :], Rb[:, h, :], [QX, Rb])])
            else:
                mm_group(P, psy[:, 0:256], psy, [(AT[i2][:], vh, [AT[i2], VGC])])
            if c < 15:
                P.op("pe", lambda e, h=h: e.matmul(psu[:, 0:256], lhsT=KZC[:, h * 128:(h + 1) * 128], rhs=VGC[:, h * 256:(h + 1) * 256], start=True, stop=True),
                     reads=[KZC, VGC], writes=[psu])
                if c == 0:
                    P.op("dve", lambda e, h=h: e.tensor_copy(out=R[:, h, :], in_=psu[:, 0:256]), reads=[psu, Rb], writes=[R], partial=True)
                else:
                    P.op("dve", lambda e, h=h: e.scalar_tensor_tensor(out=R[:, h, :], in0=R[:, h, :], scalar=cd[h], in1=psu[:, 0:256], op0=ALU.mult, op1=ALU.add),
                         reads=[psu, R], writes=[R], partial=True)
                P.op("act", lambda e, h=h: e.copy(out=Rb[:, h, :], in_=R[:, h, :]), reads=[R], writes=[Rb], partial=True)
            P.op("dve", lambda e: e.bn_stats(out=st[i2][:], in_=psy[:, 0:256]), reads=[psy], writes=[st[i2]])
            P.op("dve", lambda e: e.bn_aggr(out=mv[i2][:], in_=st[i2][:]), reads=[st[i2]], writes=[mv[i2]])
            P.op("act", lambda e: e.activation(out=rstd[i2][:], in_=mv[i2][:, 1:2], func=AF.Sqrt, bias=eps5[:, 0:1]), reads=[mv[i2], eps5], writes=[rstd[i2]])
            P.op("dve", lambda e: e.reciprocal(out=rstd[i2][:], in_=rstd[i2][:]), reads=[rstd[i2]], writes=[rstd[i2]])
            P.op("dve", lambda e: e.tensor_scalar(out=yn[i2][:], in0=psy[:, 0:256], scalar1=mv[i2][:, 0:1], scalar2=rstd[i2][:, 0:1], op0=ALU.subtract, op1=ALU.mult),
                 reads=[psy, mv[i2], rstd[i2]], writes=[yn[i2]])
            P.op("pool", lambda e, h=h: e.tensor_tensor(out=OB[:, h * 256:(h + 1) * 256], in0=yn[i2][:], in1=VGC[:, 2048 + h * 256:2048 + (h + 1) * 256], op=ALU.mult),
                 reads=[yn[i2], VGC], writes=[OB], partial=(h > 0))
        for b4 in range(4):
            pt = ptr[itr % 2]
            itr += 1
            for i in range(4):
                kk = b4 * 4 + i
                P.op("pe", lambda e, kk=kk, i=i: e.matmul(pt[:, i * 128:(i + 1) * 128], lhsT=OB[:, kk * 128:(kk + 1) * 128], rhs=ident[:], start=True, stop=True),
                     reads=[OB, ident], writes=[pt], signal=(i == 3))
            P.op("act", lambda e, b4=b4: e.copy(out=OT[:, b4 * 4:(b4 + 1) * 4, (g % 4) * 128:(g % 4 + 1) * 128], in_=pt[:, :].rearrange("p (k t) -> p k t", t=128)),
                 reads=[pt], writes=[OT], partial=not (g % 4 == 0 and b4 == 0))
        if g % 4 == 3:
            t0 = (g // 4) * 512
            P.dma("sp", oT_v[:, 0:16, t0:t0 + 512], OT[:], reads=[OT], writes=[C["oT_buf"]], partial=True)
    P.end_stage()


def stage_outproj(P, C, S, D, w_ap, nk):
    TT = 512
    P.begin_stage()
    w_d = w_ap.rearrange("(k p) n -> p k n", p=128)
    wo = [P.sb([128, 4, 1024], BF16, "wo") for _ in range(nk // 4)]
    for g in range(nk // 4):
        P.dma("pool", wo[g][:], w_d[:, 4 * g:4 * g + 4, :], writes=[wo[g]])
    oT_v = C["oT"].rearrange("k f t -> f k t")
    xt = [P.sb([128, 8, TT], F32, "xt") for _ in range(2)]
    ot = [P.sb([128, nk, TT], BF16, "ot") for _ in range(2)]
    py = [P.ps(name="py") for _ in range(2)]
    NT = NTOK // TT

    def ld(t):
        load_x(P, S, t * TT, TT, xt[t % 2])
        P.dma("sp", ot[t % 2][:], oT_v[:, 0:nk, t * TT:(t + 1) * TT], reads=[C["oT_buf"]], writes=[ot[t % 2]])

    ld(0)
    for t in range(NT):
        X, OTT = xt[t % 2], ot[t % 2]
        if t + 1 < NT:
            ld(t + 1)
        for mo in range(8):
            cps = py[mo % 2]
            mm_group(P, cps[:, :], cps, [(wo[k // 4][:, k % 4, mo * 128:(mo + 1) * 128], OTT[:, k, :], [wo[k // 4], OTT]) for k in range(nk)])
            P.op("dve", lambda e, mo=mo: e.tensor_tensor(out=X[:, mo, :], in0=cps[:, :], in1=X[:, mo, :], op=ALU.add), reads=[cps, X], writes=[X], partial=True)
        store_x(P, D, t * TT, TT, X)
    P.end_stage()


def stage_mix_c(P, C, S, D, li):
    P.begin_group()
    cos = P.sb([128, 32, 64], F32, "cos")
    sin = P.sb([128, 32, 64], F32, "sin")
    stage_tables(P, C, "pos_pm", "c_inv", 32, 64, cos, sin)
    stage_c1a(P, C, S, li, cos, sin)
    stage_c1b(P, C, S, li)
    stage_c2(P, C)
    stage_outproj(P, C, S, D, C["c_w_out"][0], 16)
    P.end_group()


def v3(ap, d):
    return ap.rearrange("p (h d) -> p h d", d=d)


def stage_b1(P, C, S, li, cos, sin, rk_all):
    TT = 512
    P.begin_stage()
    wbi = P.sb([128, 8, 672], BF16, "wbi")
    P.dma("pool", wbi[:], C["b_w_in"][0].rearrange("(k p) n -> p k n", p=128), writes=[wbi])
    wqu = P.sb([128, 3, 1536], BF16, "wqu")
    P.dma("pool", wqu[:], C["b_w_q_up"][0].rearrange("(k p) n -> p k n", p=128), writes=[wqu])
    wkvu = P.sb([128, 2, 2048], BF16, "wkvu")
    P.dma("pool", wkvu[:], C["b_w_kv_up"][0].rearrange("(k p) n -> p k n", p=128), writes=[wkvu])
    N = NormUnit(P, TT, C["gains"][li, 1])
    ident = P.sb([128, 128], BF16, "ident")
    P.dma("pool", ident[:], C["ident"], writes=[ident])
    grow = P.sb([128, 384 + 256 + 96 + 96], F32, "grow")
    P.dma("sp", grow[:], C["b_grow"], writes=[grow])
    gqa, gkva = grow[:, 0:384], grow[:, 384:640]
    GQ0, GK0 = 640, 736
    epsq = P.sb([128, 3], F32, "epsq")
    P.op("dve", lambda e: e.memset(epsq[:, 0:1], 1e-6), writes=[epsq])
    P.op("dve", lambda e: e.memset(epsq[:, 1:2], 96e-6), writes=[epsq], partial=True)
    xt = P.sb([128, 8, TT], F32, "xt")
    xn = [P.sb([128, 8, TT], BF16, "xn") for _ in range(2)]
    sq = [P.sb([128, 512], F32, "sq") for _ in range(2)]
    ssa = [P.sb([128, 4], F32, "ssa") for _ in range(2)]
    cn = [P.sb([128, 640], BF16, "cn") for _ in range(2)]
    cT = [P.sb([128, 5, 128], BF16, "cT") for _ in range(2)]
    qf = [P.sb([128, 1536], F32, "qf") for _ in range(2)]
    kvf = [P.sb([128, 2048], F32, "kvf") for _ in range(2)]
    krf = [P.sb([128, 32], F32, "krf") for _ in range(2)]
    ss16 = [P.sb([128, 32], F32, "ss16") for _ in range(2)]
    tq = [P.sb([128, 1536], F32, "tq") for _ in range(2)]
    gt = [P.sb([128, 8, 16], F32, "gt") for _ in range(2)]
    rt = [P.sb([128, 16 * 16], F32, "rt") for _ in range(4)]
    krr = [P.sb([128, 32], F32, "krr") for _ in range(2)]
    qb = [P.sb([128, 1536], BF16, "qb") for _ in range(2)]
    kb = [P.sb([128, 1536], BF16, "kb") for _ in range(2)]
    vb = [P.sb([128, 1024], BF16, "vb") for _ in range(2)]
    qTs = [P.sb([128, 32, 128], BF16, "qTs") for _ in range(2)]
    psA, psB = P.ps(name="psA"), P.ps(name="psB")
    pq = [P.ps(name="pq") for _ in range(3)]
    ptr = [P.ps(name="ptr") for _ in range(2)]
    qkT_v = C["b_qkT"].rearrange("h d t -> d h t")
    NT = NTOK // TT
    ipq = 0
    itr = 0
    import os
    B1CUT = int(os.environ.get("B1CUT", "99"))
    load_x(P, S, 0, TT, xt)
    N.run(xt, xn[0])
    for t in range(NT):
        XN = xn[t % 2]
        if t + 1 < NT:
            load_x(P, S, (t + 1) * TT, TT, xt)
        for sub in range(4):
            j = t * 4 + sub
            b = j % 2
            SQ, SSA, CN, CT, QF, KVF, KRF, SS16, TQ, GT, KRR, QB, KB, VB, QT = (sq[b], ssa[b], cn[b], cT[b], qf[b], kvf[b], krf[b], ss16[b], tq[b], gt[b],
                                                                                  krr[b], qb[b], kb[b], vb[b], qTs[b])
            xs_ = lambda k: XN[:, k, sub * 128:(sub + 1) * 128]
            mm_group(P, psA[:, 0:384], psA, [(xs_(k), wbi[:, k, 0:384], [XN, wbi]) for k in range(8)])
            mm_group(P, psB[:, 0:288], psB, [(xs_(k), wbi[:, k, 384:672], [XN, wbi]) for k in range(8)])
            P.op("act", lambda e: e.activation(out=SQ[:, 0:384], in_=psA[:, 0:384], func=AF.Square), reads=[psA], writes=[SQ])
            P.op("dve", lambda e: e.reduce_sum(out=SSA[:, 0:1], in_=SQ[:, 0:384], axis=AX.X), reads=[SQ], writes=[SSA])
            P.op("act", lambda e: e.activation(out=SQ[:, 0:288], in_=psB[:, 0:288], func=AF.Square), reads=[psB], writes=[SQ])
            P.op("dve", lambda e: e.reduce_sum(out=SSA[:, 1:2], in_=SQ[:, 0:256], axis=AX.X), reads=[SQ], writes=[SSA], partial=True)
            P.op("dve", lambda e: e.reduce_sum(out=SSA[:, 2:3], in_=SQ[:, 256:288], axis=AX.X), reads=[SQ], writes=[SSA], partial=True)
            P.op("act", lambda e: e.activation(out=SSA[:, 0:1], in_=SSA[:, 0:1], func=AF.Sqrt, scale=1.0 / 384, bias=epsq[:, 0:1]), reads=[SSA, epsq], writes=[SSA], partial=True)
            P.op("act", lambda e: e.activation(out=SSA[:, 1:2], in_=SSA[:, 1:2], func=AF.Sqrt, scale=1.0 / 256, bias=epsq[:, 0:1]), reads=[SSA, epsq], writes=[SSA], partial=True)
            P.op("dve", lambda e: e.reciprocal(out=SSA[:, 0:2], in_=SSA[:, 0:2]), reads=[SSA], writes=[SSA], partial=True)
            P.op("dve", lambda e: e.scalar_tensor_tensor(out=CN[:, 0:384], in0=psA[:, 0:384], scalar=SSA[:, 0:1], in1=gqa, op0=ALU.mult, op1=ALU.mult),
                 reads=[psA, SSA, grow], writes=[CN])
            P.op("dve", lambda e: e.scalar_tensor_tensor(out=CN[:, 384:640], in0=psB[:, 0:256], scalar=SSA[:, 1:2], in1=gkva, op0=ALU.mult, op1=ALU.mult),
                 reads=[psB, SSA, grow], writes=[CN], partial=True)
            P.op("act", lambda e: e.copy(out=KRF[:], in_=psB[:, 256:288]), reads=[psB], writes=[KRF])
            if B1CUT <= 1:
                continue
            pt = ptr[itr % 2]
            itr += 1
            for i in range(4):
                P.op("pe", lambda e, i=i: e.matmul(pt[:, i * 128:(i + 1) * 128], lhsT=CN[:, i * 128:(i + 1) * 128], rhs=ident[:], start=True, stop=True),
                     reads=[CN, ident], writes=[pt], signal=(i == 3))
            P.op("act", lambda e: e.copy(out=CT[:, 0:4, :], in_=v3(pt[:, :], 128)), reads=[pt], writes=[CT])
            pt = ptr[itr % 2]
            itr += 1
            P.op("pe", lambda e: e.matmul(pt[:, 0:128], lhsT=CN[:, 512:640], rhs=ident[:], start=True, stop=True), reads=[CN, ident], writes=[pt])
            P.op("act", lambda e: e.copy(out=CT[:, 4, :], in_=pt[:, 0:128]), reads=[pt], writes=[CT], partial=True)
            if B1CUT <= 2:
                continue
            cs, sn = cos[:, j, :], sin[:, j, :]
            for o, g0 in ((0, GQ0), (4, GK0)):
                ga, gb = grow[:, g0 + 64:g0 + 80], grow[:, g0 + 80:g0 + 96]
                for idx, (gg, tt) in enumerate(((ga, cs), (gb, sn), (gb, cs), (ga, sn))):
                    P.op("pool", lambda e, o=o, idx=idx, gg=gg, tt=tt: e.tensor_tensor(out=GT[:, o + idx, :], in0=gg, in1=tt, op=ALU.mult),
                         reads=[grow, cos, sin], writes=[GT], partial=not (o == 0 and idx == 0))
            if B1CUT <= 3:
                continue
            for hb in range(4):
                ps = pq[ipq % 3]
                ipq += 1
                mm_group(P, ps[:, 0:384], ps, [(CT[:, kk, :], wqu[:, kk, hb * 384:(hb + 1) * 384], [CT, wqu]) for kk in range(3)])
                P.op("act", lambda e, hb=hb: e.copy(out=QF[:, hb * 384:(hb + 1) * 384], in_=ps[:, 0:384]), reads=[ps], writes=[QF], partial=(hb > 0))
                P.op("act", lambda e: e.activation(out=SQ[:, 0:384], in_=ps[:, 0:384], func=AF.Square), reads=[ps], writes=[SQ])
                P.op("dve", lambda e, hb=hb: e.reduce_sum(out=SS16[:, hb * 4:(hb + 1) * 4], in_=v3(SQ[:, 0:384], 96), axis=AX.X), reads=[SQ], writes=[SS16],
                     partial=(hb > 0))
            for hb in range(4):
                ps = pq[ipq % 3]
                ipq += 1
                mm_group(P, ps[:, :], ps, [(CT[:, 3 + kk, :], wkvu[:, kk, hb * 512:(hb + 1) * 512], [CT, wkvu]) for kk in range(2)])
                P.op("act", lambda e, hb=hb: e.copy(out=KVF[:, hb * 512:(hb + 1) * 512], in_=ps[:, :]), reads=[ps], writes=[KVF], partial=(hb > 0))
                P.op("act", lambda e: e.activation(out=v3(SQ[:, 0:256], 64), in_=v3(ps[:, :], 128)[:, :, 0:64], func=AF.Square), reads=[ps], writes=[SQ])
                P.op("dve", lambda e, hb=hb: e.reduce_sum(out=SS16[:, 16 + hb * 4:16 + (hb + 1) * 4], in_=v3(SQ[:, 0:256], 64), axis=AX.X), reads=[SQ], writes=[SS16],
                     partial=True)
            if B1CUT <= 4:
                continue
            P.op("act", lambda e: e.activation(out=SS16[:, 0:16], in_=SS16[:, 0:16], func=AF.Sqrt, scale=1.0 / 96, bias=epsq[:, 0:1]), reads=[SS16, epsq], writes=[SS16], partial=True)
            P.op("dve", lambda e: e.tensor_scalar(out=SS16[:, 16:32], in0=SS16[:, 16:32], scalar1=SSA[:, 2:3], scalar2=None, op0=ALU.add), reads=[SS16, SSA], writes=[SS16], partial=True)
            P.op("act", lambda e: e.activation(out=SS16[:, 16:32], in_=SS16[:, 16:32], func=AF.Sqrt, bias=epsq[:, 1:2]), reads=[SS16, epsq], writes=[SS16], partial=True)
            P.op("dve", lambda e: e.reciprocal(out=SS16[:, 0:16], in_=SS16[:, 0:16]), reads=[SS16], writes=[SS16], partial=True)
            P.op("dve", lambda e, j=j: e.reciprocal(out=rk_all[:, j, :], in_=SS16[:, 16:32]), reads=[SS16], writes=[rk_all], partial=True)
            if B1CUT <= 5:
                continue
            QF3, TQ3 = v3(QF[:, :], 96), v3(TQ[:, :], 96)
            P.op("dve", lambda e: e.tensor_tensor(out=TQ3[:, :, 0:64], in0=QF3[:, :, 0:64], in1=grow[:, GQ0:GQ0 + 64].unsqueeze(1).to_broadcast([128, 16, 64]), op=ALU.mult),
                 reads=[QF, grow], writes=[TQ])
            x1, x2 = QF3[:, :, 64:80], QF3[:, :, 80:96]
            bc = lambda i: GT[:, i, :].unsqueeze(1).to_broadcast([128, 16, 16])
            r3 = [v3(r_[:, :], 16) for r_ in rt]
            P.op("dve", lambda e: e.tensor_tensor(out=r3[0], in0=x1, in1=bc(0), op=ALU.mult), reads=[QF, GT], writes=[rt[0]])
            P.op("dve", lambda e: e.tensor_tensor(out=r3[1], in0=x2, in1=bc(1), op=ALU.mult), reads=[QF, GT], writes=[rt[1]])
            P.op("dve", lambda e: e.tensor_tensor(out=r3[2], in0=x2, in1=bc(2), op=ALU.mult), reads=[QF, GT], writes=[rt[2]])
            P.op("dve", lambda e: e.tensor_tensor(out=r3[3], in0=x1, in1=bc(3), op=ALU.mult), reads=[QF, GT], writes=[rt[3]])
            P.op("pool", lambda e: e.tensor_tensor(out=TQ3[:, :, 64:80], in0=r3[0], in1=r3[1], op=ALU.subtract), reads=[rt[0], rt[1]], writes=[TQ], partial=True)
            P.op("pool", lambda e: e.tensor_tensor(out=TQ3[:, :, 80:96], in0=r3[2], in1=r3[3], op=ALU.add), reads=[rt[2], rt[3]], writes=[TQ], partial=True)
            P.op("dve", lambda e: e.tensor_tensor(out=v3(QB[:, :], 96), in0=TQ3, in1=SS16[:, 0:16].unsqueeze(2).to_broadcast([128, 16, 96]), op=ALU.mult),
                 reads=[TQ, SS16], writes=[QB])
            if B1CUT <= 6:
                continue
            KV3, KB3 = v3(KVF[:, :], 128), v3(KB[:, :], 96)
            P.op("pool", lambda e: e.tensor_tensor(out=KB3[:, :, 0:64], in0=KV3[:, :, 0:64], in1=grow[:, GK0:GK0 + 64].unsqueeze(1).to_broadcast([128, 16, 64]), op=ALU.mult),
                 reads=[KVF, grow], writes=[KB])
            k1, k2 = KRF[:, 0:16], KRF[:, 16:32]
            P.op("dve", lambda e: e.tensor_tensor(out=rt[0][:, 0:16], in0=k1, in1=GT[:, 4, :], op=ALU.mult), reads=[KRF, GT], writes=[rt[0]])
            P.op("dve", lambda e: e.tensor_tensor(out=rt[1][:, 0:16], in0=k2, in1=GT[:, 5, :], op=ALU.mult), reads=[KRF, GT], writes=[rt[1]])
            P.op("dve", lambda e: e.tensor_tensor(out=rt[2][:, 0:16], in0=k2, in1=GT[:, 6, :], op=ALU.mult), reads=[KRF, GT], writes=[rt[2]])
            P.op("dve", lambda e: e.tensor_tensor(out=rt[3][:, 0:16], in0=k1, in1=GT[:, 7, :], op=ALU.mult), reads=[KRF, GT], writes=[rt[3]])
            P.op("pool", lambda e: e.tensor_tensor(out=KRR[:, 0:16], in0=rt[0][:, 0:16], in1=rt[1][:, 0:16], op=ALU.subtract), reads=[rt[0], rt[1]], writes=[KRR])
            P.op("pool", lambda e: e.tensor_tensor(out=KRR[:, 16:32], in0=rt[2][:, 0:16], in1=rt[3][:, 0:16], op=ALU.add), reads=[rt[2], rt[3]], writes=[KRR], partial=True)
            P.op("pool", lambda e: e.tensor_copy(out=KB3[:, :, 64:96], in_=KRR[:, :].unsqueeze(1).to_broadcast([128, 16, 32])), reads=[KRR], writes=[KB], partial=True)
            P.op("act", lambda e: e.copy(out=v3(VB[:, :], 64), in_=KV3[:, :, 64:128]), reads=[KVF], writes=[VB])
            P.dma("sp", C["b_v"][j * 128:(j + 1) * 128, :], VB[:], reads=[VB], writes=[C["b_v_buf"]], partial=True)
            if B1CUT <= 7:
                continue
            for src, base in ((QB, 0), (KB, 16)):
                for b4 in range(4):
                    pt = ptr[itr % 2]
                    itr += 1
                    for i in range(4):
                        hh = b4 * 4 + i
                        P.op("pe", lambda e, hh=hh, i=i, src=src: e.matmul(pt[0:96, i * 128:(i + 1) * 128], lhsT=src[:, hh * 96:(hh + 1) * 96], rhs=ident[:], start=True, stop=True),
                             reads=[src, ident], writes=[pt], signal=(i == 3))
                    P.op("act", lambda e, b4=b4, base=base: e.copy(out=QT[0:96, base + b4 * 4:base + (b4 + 1) * 4, :], in_=v3(pt[0:96, :], 128)),
                         reads=[pt], writes=[QT], partial=not (base == 0 and b4 == 0))
            for q4 in range(4):
                P.dma("sp", qkT_v[:, q4 * 8:(q4 + 1) * 8, j * 128:(j + 1) * 128], QT[0:96, q4 * 8:(q4 + 1) * 8, :], reads=[QT], writes=[C["b_qkT_buf"]], partial=True)
        if t + 1 < NT:
            N.run(xt, xn[(t + 1) % 2])
    P.end_stage()


def stage_b2(P, C, rk_all):
    P.begin_stage()
    mask = P.sb([128, 4, 512], BF16, "mask")
    P.dma("pool", mask[:], C["b_mask"], writes=[mask])
    ones = P.sb([128, 64], BF16, "ones64")
    P.op("dve", lambda e: e.memset(ones[:], 1.0), writes=[ones])
    V = P.sb([128, 16, 1024], BF16, "V")
    qT = [P.sb([128, 2048], BF16, "qT") for _ in range(2)]
    kT = [P.sb([128, 2048], BF16, "kT") for _ in range(2)]
    PT = [P.sb([128, 512], BF16, "PT") for _ in range(4)]
    rden = [P.sb([128, 512], F32, "rden") for _ in range(2)]
    oTs = [P.sb([128, 2048], BF16, "oTs") for _ in range(2)]
    ps_s = [P.ps(name="ps_s") for _ in range(3)]
    psn = [P.ps(name="psn") for _ in range(2)]
    psd = [P.ps(name="psd") for _ in range(2)]
    ist = 0
    iq = 0

    def load_head(s, h, i):
        P.dma("sp", qT[i][0:96, :], C["b_qkT"][h, :, s * 2048:(s + 1) * 2048], reads=[C["b_qkT_buf"]], writes=[qT[i]])
        P.dma("sp", kT[i][0:96, :], C["b_qkT"][16 + h, :, s * 2048:(s + 1) * 2048], reads=[C["b_qkT_buf"]], writes=[kT[i]])

    for s in range(2):
        P.dma("sp", V[:], C["b_v"][s * 2048:(s + 1) * 2048, :].rearrange("(j p) n -> p j n", p=128), reads=[C["b_v_buf"]], writes=[V])
        load_head(s, 0, 0)
        steps = [(h, qb, j) for h in range(16) for qb in range(4) for j in range(4 * qb + 4)]
        banks = {}

        def emit_qk(idx):
            nonlocal ist
            h, qb, j = steps[idx]
            if qb == 0 and j == 0 and h + 1 < 16:
                load_head(s, h + 1, (h + 1) % 2)
            QT, KT = qT[h % 2], kT[h % 2]
            pss = ps_s[ist % 3]
            pt_ = PT[ist % 4]
            ist += 1
            P.op("pe", lambda e: e.matmul(pss[:, :], lhsT=KT[0:96, j * 128:(j + 1) * 128], rhs=QT[0:96, qb * 512:(qb + 1) * 512], start=True, stop=True),
                 reads=[KT, QT], writes=[pss])
            P.op("act", lambda e: e.activation(out=pt_[:], in_=pss[:, :], func=AF.Exp, scale=rk_all[:, s * 16 + j, h:h + 1]),
                 reads=[pss, rk_all], writes=[pt_])
            if j >= 4 * qb:
                P.op("dve", lambda e: e.tensor_tensor(out=pt_[:], in0=pt_[:], in1=mask[:, j - 4 * qb, :], op=ALU.mult), reads=[pt_, mask], writes=[pt_])
            return pt_

        def emit_pv(idx, pt_):
            nonlocal iq
            h, qb, j = steps[idx]
            e0 = (h % 2) * 64
            OT = oTs[(h // 2) % 2]
            nj = 4 * qb + 4
            if j == 0:
                banks[(h, qb)] = (psn[iq % 2], psd[iq % 2], rden[iq % 2])
                iq += 1
            pn, pd, RD = banks[(h, qb)]
            P.op("pe", lambda e: e.matmul(pn[e0:e0 + 64, :], lhsT=V[:, j, h * 64:(h + 1) * 64], rhs=pt_[:], start=(j == 0), stop=(j == nj - 1)),
                 reads=[V, pt_], writes=[pn], signal=False, partial=(j > 0))
            P.op("pe", lambda e: e.matmul(pd[e0:e0 + 64, :], lhsT=ones[:], rhs=pt_[:], start=(j == 0), stop=(j == nj - 1)),
                 reads=[ones, pt_], writes=[pd], signal=True, partial=(j > 0))
            if j == nj - 1:
                P.op("dve", lambda e: e.reciprocal(out=RD[e0:e0 + 64, :], in_=pd[e0:e0 + 64, :]), reads=[pd], writes=[RD])
                P.op("dve", lambda e: e.tensor_tensor(out=OT[e0:e0 + 64, qb * 512:(qb + 1) * 512], in0=pn[e0:e0 + 64, :], in1=RD[e0:e0 + 64, :], op=ALU.mult),
                     reads=[pn, RD], writes=[OT], partial=not (h % 2 == 0 and qb == 0))
                if h % 2 == 1 and qb == 3:
                    P.dma("sp", C["oT"][h // 2, :, s * 2048:(s + 1) * 2048], OT[:], reads=[OT], writes=[C["oT_buf"]], partial=True)

        pend = [emit_qk(0), emit_qk(1)]
        for idx in range(len(steps)):
            if idx + 2 < len(steps):
                pend.append(emit_qk(idx + 2))
            emit_pv(idx, pend.pop(0))
    P.end_stage()


def stage_mix_b(P, C, S, D, li):
    P.begin_group()
    cos = P.sb([128, 32, 16], F32, "cos")
    sin = P.sb([128, 32, 16], F32, "sin")
    rk_all = P.sb([128, 32, 16], F32, "rk_all")
    stage_tables(P, C, "pos_pm", "b_inv", 32, 16, cos, sin)
    import os
    dbg = os.environ.get("BDBG", "123")
    if "1" in dbg:
        stage_b1(P, C, S, li, cos, sin, rk_all)
    if "2" in dbg:
        stage_b2(P, C, rk_all)
    if "3" in dbg:
        stage_outproj(P, C, S, D, C["b_w_out"][0], 8)
    P.end_group()


def stage_a1(P, C, S, li, cos, sin):
    TT = 512
    P.begin_stage()
    w_d = C["a_w_in"][0].rearrange("(k p) n -> p k n", p=128)
    wa = [P.sb([128, 8, 512], BF16, "wa") for _ in range(6)]
    N = NormUnit(P, TT, C["gains"][li, 1])
    ident = P.sb([128, 128], BF16, "ident")
    P.dma("pool", ident[:], C["ident"], writes=[ident])
    grow = P.sb([128, 128], F32, "grow")
    P.dma("sp", grow[:], C["a_grow"], writes=[grow])
    epsq = P.sb([128, 2], F32, "epsq")
    P.op("dve", lambda e: e.memset(epsq[:, 0:1], 1e-6), writes=[epsq])
    P.op("dve", lambda e: e.memset(epsq[:, 1:2], 64e-6), writes=[epsq], partial=True)
    xt = P.sb([128, 8, TT], F32, "xt")
    xn = P.sb([128, 8, SEQ], BF16, "xnseq")
    xn_parts = [P.sb([128, 8, TT], BF16, "xnp") for _ in range(2)]
    sq = [P.sb([128, 512], F32, "sq") for _ in range(2)]
    SSb = [P.sb([128, 32], F32, "ss") for _ in range(2)]
    Tb = [P.sb([128, 2048], F32, "T") for _ in range(2)]
    GTb = [P.sb([128, 8, 32], F32, "gt") for _ in range(2)]
    rt = [P.sb([128, 256], F32, "rt") for _ in range(8)]
    qkb = [P.sb([128, 2048], BF16, "qkb") for _ in range(2)]
    vb = [P.sb([128, 1024], BF16, "vb") for _ in range(2)]
    qTs = [P.sb([128, 16, 128], BF16, "qTs") for _ in range(2)]
    pp = [P.ps(name="pp") for _ in range(4)]
    ptr = [P.ps(name="ptr") for _ in range(2)]
    ipp = 0
    itr = 0
    irt = 0
    jt = 0
    deferred = []
    for s in range(2):
        for t4 in range(4):
            load_x(P, S, s * SEQ + t4 * TT, TT, xt)
            XP = xn_parts[t4 % 2]
            N.run(xt, XP)
            P.op("pool", lambda e, t4=t4, XP=XP: e.tensor_copy(out=xn[:, :, t4 * TT:(t4 + 1) * TT], in_=XP[:]), reads=[XP], writes=[xn], partial=(t4 > 0))
        for g in range(3):
            for c6 in range(6):
                col = (c6 // 2) * 3072 + g * 1024 + (c6 % 2) * 512
                P.dma("pool", wa[c6][:], w_d[:, :, col:col + 512], writes=[wa[c6]])
            qkT_v = C["a_qkT"][s * 3 + g].rearrange("h d t -> d h t")
            for ti in range(16):
                b = jt % 2
                jt += 1
                SS, T, GT, QKB, VB, QT = SSb[b], Tb[b], GTb[b], qkb[b], vb[b], qTs[b]
                tix = (s * 3 + g) * 16 + ti
                st, dil, nb = a_tile_start(g, ti)
                tok = slice(st, st + 127 * dil + 1, dil)
                cs, sn = cos[:, tix, :], sin[:, tix, :]
                for o, g0 in ((0, 0), (4, 64)):
                    ga, gb = grow[:, g0:g0 + 32], grow[:, g0 + 32:g0 + 64]
                    for idx, (gg, tt) in enumerate(((ga, cs), (gb, sn), (gb, cs), (ga, sn))):
                        P.op("pool", lambda e, o=o, idx=idx, gg=gg, tt=tt: e.tensor_tensor(out=GT[:, o + idx, :], in0=gg, in1=tt, op=ALU.mult),
                             reads=[grow, cos, sin], writes=[GT], partial=not (o == 0 and idx == 0))
                for c6 in range(6):
                    ps = pp[ipp % 4]
                    ipp += 1
                    mm_group(P, ps[:, :], ps, [(xn[:, k, tok], wa[c6][:, k, :], [xn, wa[c6]]) for k in range(8)])
                    if c6 >= 4:
                        P.op("act", lambda e, c6=c6: e.copy(out=VB[:, (c6 - 4) * 512:(c6 - 3) * 512], in_=ps[:, :]), reads=[ps], writes=[VB], partial=(c6 == 5))
                        continue
                    SQ = sq[c6 % 2]
                    P.op("act", lambda e: e.activation(out=SQ[:], in_=ps[:, :], func=AF.Square), reads=[ps], writes=[SQ])
                    P.op("dve", lambda e, c6=c6: e.reduce_sum(out=SS[:, c6 * 8:(c6 + 1) * 8], in_=SQ[:, :].rearrange("p (h d) -> p h d", d=64), axis=AX.X),
                         reads=[SQ], writes=[SS], partial=(c6 > 0))
                    o = 0 if c6 < 2 else 4
                    ps3 = ps[:, :].rearrange("p (h d) -> p h d", d=64)
                    x1, x2 = ps3[:, :, 0:32], ps3[:, :, 32:64]
                    bc = lambda i: GT[:, o + i, :].unsqueeze(1).to_broadcast([128, 8, 32])
                    r = [rt[(irt + i) % 8] for i in range(4)]
                    irt += 4
                    r3 = [r_[:, :].rearrange("p (h d) -> p h d", d=32) for r_ in r]
                    P.op("dve", lambda e: e.tensor_tensor(out=r3[0], in0=x1, in1=bc(0), op=ALU.mult), reads=[ps, GT], writes=[r[0]])
                    P.op("dve", lambda e: e.tensor_tensor(out=r3[1], in0=x2, in1=bc(1), op=ALU.mult), reads=[ps, GT], writes=[r[1]])
                    P.op("dve", lambda e: e.tensor_tensor(out=r3[2], in0=x2, in1=bc(2), op=ALU.mult), reads=[ps, GT], writes=[r[2]])
                    P.op("dve", lambda e: e.tensor_tensor(out=r3[3], in0=x1, in1=bc(3), op=ALU.mult), reads=[ps, GT], writes=[r[3]])
                    T3 = T[:, c6 * 512:(c6 + 1) * 512].rearrange("p (h d) -> p h d", d=64)
                    P.op("pool", lambda e: e.tensor_tensor(out=T3[:, :, 0:32], in0=r3[0], in1=r3[1], op=ALU.subtract), reads=[r[0], r[1]], writes=[T], partial=(c6 > 0))
                    P.op("pool", lambda e: e.tensor_tensor(out=T3[:, :, 32:64], in0=r3[2], in1=r3[3], op=ALU.add), reads=[r[2], r[3]], writes=[T], partial=True)
                while deferred:
                    deferred.pop(0)()
                P.op("act", lambda e: e.activation(out=SS[:, 0:16], in_=SS[:, 0:16], func=AF.Sqrt, scale=1.0 / 64, bias=epsq[:, 0:1]), reads=[SS, epsq], writes=[SS], partial=True)
                P.op("act", lambda e: e.activation(out=SS[:, 16:32], in_=SS[:, 16:32], func=AF.Sqrt, bias=epsq[:, 1:2]), reads=[SS, epsq], writes=[SS], partial=True)
                P.op("dve", lambda e: e.reciprocal(out=SS[:, :], in_=SS[:, :]), reads=[SS], writes=[SS], partial=True)
                v64 = lambda ap: ap.rearrange("p (h d) -> p h d", d=64)
                P.op("dve", lambda e: e.tensor_tensor(out=v64(QKB[:, 0:1024]), in0=v64(T[:, 0:1024]), in1=SS[:, 0:16].unsqueeze(2).to_broadcast([128, 16, 64]), op=ALU.mult),
                     reads=[T, SS], writes=[QKB])
                P.op("pool", lambda e: e.tensor_tensor(out=v64(QKB[:, 1024:2048]), in0=v64(T[:, 1024:2048]), in1=SS[:, 16:32].unsqueeze(2).to_broadcast([128, 16, 64]), op=ALU.mult),
                     reads=[T, SS], writes=[QKB], partial=True)
                P.dma("sp", C["a_v"][s * 3 + g, ti * 128:(ti + 1) * 128, :], VB[:], reads=[VB], writes=[C["a_v_buf"]], partial=True)
                def tr_fn(QKB=QKB, QT=QT, qkT_v=qkT_v, ti=ti):
                    nonlocal itr
                    for b4 in range(4):
                        pt = ptr[itr % 2]
                        itr += 1
                        for i in range(4):
                            hh = b4 * 4 + i
                            P.op("pe", lambda e, hh=hh, i=i: e.matmul(pt[:, i * 128:(i + 1) * 128], lhsT=QKB[:, hh * 128:(hh + 1) * 128], rhs=ident[:], start=True, stop=True),
                                 reads=[QKB, ident], writes=[pt], signal=(i == 3))
                        P.op("act", lambda e, b4=b4: e.copy(out=QT[:, b4 * 4:(b4 + 1) * 4, :], in_=pt[:, :].rearrange("p (h t) -> p h t", t=128)),
                             reads=[pt], writes=[QT], partial=(b4 > 0))
                    for q2 in range(2):
                        P.dma("sp", qkT_v[:, q2 * 8:(q2 + 1) * 8, ti * 128:(ti + 1) * 128], QT[:, q2 * 8:(q2 + 1) * 8, :], reads=[QT], writes=[C["a_qkT_buf"]], partial=True)
                deferred.append(tr_fn)
    while deferred:
        deferred.pop(0)()
    P.end_stage()


def stage_a2(P, C):
    P.begin_stage()
    mask = P.sb([128, 256], BF16, "mask")
    P.dma("pool", mask[:], C["a_mask"], writes=[mask])
    ones = P.sb([128, 64], BF16, "ones64")
    P.op("dve", lambda e: e.memset(ones[:], 1.0), writes=[ones])
    qT = [[P.sb([64, 2048], BF16, "qT") for _ in range(2)] for _ in range(2)]
    kT = [[P.sb([64, 2048], BF16, "kT") for _ in range(2)] for _ in range(2)]
    Vp = [P.sb([128, 16, 128], BF16, "Vp") for _ in range(2)]
    PT = [P.sb([128, 256], BF16, "PT") for _ in range(4)]
    accn = [P.sb([128, 2048], F32, "accn") for _ in range(2)]
    accd = [P.sb([128, 2048], F32, "accd") for _ in range(2)]
    oTs = [P.sb([128, 2048], BF16, "oTs") for _ in range(2)]
    ps_s = [P.ps(name="ps_s") for _ in range(3)]
    psn = [P.ps(name="psn") for _ in range(2)]
    psd = [P.ps(name="psd") for _ in range(2)]
    ist = 0
    iq = 0
    units = [(s, hp, g) for s in range(2) for hp in range(8) for g in range(3)]

    def load_unit(u, i):
        s, hp, g = u
        sg = s * 3 + g
        for e_ in range(2):
            P.dma("sp", qT[i][e_][:], C["a_qkT"][sg, hp, e_ * 64:(e_ + 1) * 64, :], reads=[C["a_qkT_buf"]], writes=[qT[i][e_]])
            P.dma("sp", kT[i][e_][:], C["a_qkT"][sg, 8 + hp, e_ * 64:(e_ + 1) * 64, :], reads=[C["a_qkT_buf"]], writes=[kT[i][e_]])
        vsrc = C["a_v"][sg, :, hp * 128:(hp + 1) * 128].rearrange("(j p) n -> p j n", p=128)
        for h2 in range(2):
            P.dma("sp", Vp[i][:, h2 * 8:(h2 + 1) * 8, :], vsrc[:, h2 * 8:(h2 + 1) * 8, :], reads=[C["a_v_buf"]], writes=[Vp[i]], partial=(h2 > 0))

    load_unit(units[0], 0)
    steps = [(ui, e_, qb, blk) for ui in range(len(units)) for e_ in range(2) for qb in range(4) for blk in range(4)]
    banks = {}

    def emit_qk(idx):
        nonlocal ist
        ui, e_, qb, blk = steps[idx]
        s, hp, g = units[ui]
        i = ui % 2
        nb = 16 // A_DIL[g]
        QT, KT = qT[i][e_], kT[i][e_]
        tq = 4 * qb + blk
        has_prev = (tq % nb) != 0
        pss = ps_s[ist % 3]
        pt_ = PT[ist % 4]
        ist += 1
        c0 = 0 if has_prev else 128
        if has_prev:
            P.op("pe", lambda e: e.matmul(pss[:, 0:128], lhsT=KT[:, (tq - 1) * 128:tq * 128], rhs=QT[:, tq * 128:(tq + 1) * 128], start=True, stop=True),
                 reads=[KT, QT], writes=[pss], signal=False)
        P.op("pe", lambda e: e.matmul(pss[:, 128:256], lhsT=KT[:, tq * 128:(tq + 1) * 128], rhs=QT[:, tq * 128:(tq + 1) * 128], start=True, stop=True),
             reads=[KT, QT], writes=[pss], partial=has_prev)
        P.op("act", lambda e: e.activation(out=pt_[:, c0:256], in_=pss[:, c0:256], func=AF.Exp), reads=[pss], writes=[pt_])
        P.op("dve", lambda e: e.tensor_tensor(out=pt_[:, c0:256], in0=pt_[:, c0:256], in1=mask[:, c0:256], op=ALU.mult), reads=[pt_, mask], writes=[pt_])
        return pt_

    def emit_pv(idx, pt_):
        nonlocal iq
        ui, e_, qb, blk = steps[idx]
        s, hp, g = units[ui]
        i = ui % 2
        if e_ == 0 and qb == 0 and blk == 0 and ui + 1 < len(units):
            load_unit(units[ui + 1], (ui + 1) % 2)
        nb = 16 // A_DIL[g]
        V = Vp[i]
        e0 = e_ * 64
        tq = 4 * qb + blk
        has_prev = (tq % nb) != 0
        if blk == 0:
            banks[(ui, e_, qb)] = (psn[iq % 2], psd[iq % 2])
            iq += 1
        pn, pd = banks[(ui, e_, qb)]
        AN, AD = accn[(ui // 3) % 2], accd[(ui // 3) % 2]
        osl = slice(blk * 128, (blk + 1) * 128)
        if has_prev:
            P.op("pe", lambda e: e.matmul(pn[e0:e0 + 64, osl], lhsT=V[:, tq - 1, e0:e0 + 64], rhs=pt_[:, 0:128], start=True, stop=False),
                 reads=[V, pt_], writes=[pn], signal=False, partial=True)
        P.op("pe", lambda e: e.matmul(pn[e0:e0 + 64, osl], lhsT=V[:, tq, e0:e0 + 64], rhs=pt_[:, 128:256], start=(not has_prev), stop=True),
             reads=[V, pt_], writes=[pn], signal=False, partial=True)
        if has_prev:
            P.op("pe", lambda e: e.matmul(pd[e0:e0 + 64, osl], lhsT=ones[:], rhs=pt_[:, 0:128], start=True, stop=False),
                 reads=[ones, pt_], writes=[pd], signal=False, partial=True)
        P.op("pe", lambda e: e.matmul(pd[e0:e0 + 64, osl], lhsT=ones[:], rhs=pt_[:, 128:256], start=(not has_prev), stop=True),
             reads=[ones, pt_], writes=[pd], signal=True, partial=True)
        if blk == 3:
            for acc, src in ((AN, pn), (AD, pd)):
                if g == 0:
                    dst = acc[e0:e0 + 64, qb * 512:(qb + 1) * 512]
                    sv = src[e0:e0 + 64, :]
                    P.op("act", lambda e, dst=dst, sv=sv: e.copy(out=dst, in_=sv), reads=[src], writes=[acc], partial=True)
                else:
                    if g == 1:
                        dst = acc[e0:e0 + 64, :].rearrange("p (l r) -> p r l", r=4)[:, qb, :]
                        sv = src[e0:e0 + 64, :]
                    else:
                        dst = acc[e0:e0 + 64, :].rearrange("p (u r) -> p r u", r=16)[:, 4 * qb:4 * qb + 4, :]
                        sv = src[e0:e0 + 64, :].rearrange("p (b u) -> p b u", u=128)
                    P.op("dve", lambda e, dst=dst, sv=sv: e.tensor_tensor(out=dst, in0=dst, in1=sv, op=ALU.add), reads=[src, acc], writes=[acc], partial=True)
            if g == 2 and e_ == 1 and qb == 3:
                OT = oTs[(ui // 3) % 2]
                P.op("dve", lambda e: e.reciprocal(out=AD[:], in_=AD[:]), reads=[AD], writes=[AD])
                P.op("dve", lambda e: e.tensor_tensor(out=OT[:], in0=AN[:], in1=AD[:], op=ALU.mult), reads=[AN, AD], writes=[OT])
                P.dma("sp", C["oT"][hp, :, s * 2048:(s + 1) * 2048], OT[:], reads=[OT, AN, AD], writes=[C["oT_buf"]], partial=True)

    pend = [emit_qk(0), emit_qk(1)]
    for idx in range(len(steps)):
        if idx + 2 < len(steps):
            pend.append(emit_qk(idx + 2))
        emit_pv(idx, pend.pop(0))
    P.end_stage()


def stage_mix_a(P, C, S, D, li):
    import os
    dbg = os.environ.get("ADBG", "123")
    P.begin_group()
    cos = P.sb([128, 96, 32], F32, "cos")
    sin = P.sb([128, 96, 32], F32, "sin")
    stage_tables(P, C, "pos_a", "a_inv", 96, 32, cos, sin)
    if "1" in dbg:
        stage_a1(P, C, S, li, cos, sin)
    if "2" in dbg:
        stage_a2(P, C)
    if "3" in dbg:
        stage_outproj(P, C, S, D, C["a_w_out"][0], 8)
    P.end_group()


MIXERS[3] = stage_mix_d
MIXERS[2] = stage_mix_c
MIXERS[1] = stage_mix_b
MIXERS[0] = stage_mix_a


FULL_STAGES = []
for _li in range(4):
    FULL_STAGES += [("ffn", _li, 0), ("mix", _li), ("ffn", _li, 1)]

WEIGHT_SPECS = [
    ("ffn_w_in", [4, 2, 1024, 5632]), ("ffn_w_out", [4, 2, 2816, 1024]),
    ("a_w_in", [1, 1024, 9216]), ("a_w_out", [1, 1024, 1024]),
    ("b_w_in", [1, 1024, 672]), ("b_w_q_up", [1, 384, 1536]), ("b_w_kv_up", [1, 256, 2048]), ("b_w_out", [1, 1024, 1024]),
    ("c_w_in", [1, 1024, 6144]), ("c_w_out", [1, 2048, 1024]),
    ("d_w_in", [1, 1024, 2048]), ("d_w_rg", [1, 4, 256, 256]), ("d_w_ig", [1, 4, 256, 256]), ("d_w_out", [1, 1024, 1024]),
]


def build_program(stages):
    P = Prog()
    nc = P.nc
    C = {}
    x_in = nc.dram_tensor("x", [DM, NTOK], F32, kind="ExternalInput").ap()
    out = nc.dram_tensor("out", [DM, NTOK], F32, kind="ExternalOutput").ap()
    xs = nc.dram_tensor("xs", [DM, NTOK], F32, kind="Internal").ap()
    C["positions"] = nc.dram_tensor("positions", [2, SEQ], I32, kind="ExternalInput").ap()
    C["gains"] = nc.dram_tensor("gains", [4, 3, 128, 8], F32, kind="ExternalInput").ap()
    for name, shape in WEIGHT_SPECS:
        C[name] = nc.dram_tensor(name, shape, F32, kind="ExternalInput").ap()
    for name, shape in small_specs():
        C[name] = nc.dram_tensor(name, shape, F32, kind="ExternalInput").ap()
    for name, shape in int_specs():
        C[name] = nc.dram_tensor(name, shape, I32, kind="ExternalInput").ap()
    for name, shape in scratch_specs():
        C[name] = nc.dram_tensor(name, shape, BF16, kind="Internal").ap()
        C[name + "_buf"] = P.dbuf(C[name], name)
    Sx, So, Ss = Stream(P, x_in), Stream(P, out), Stream(P, xs)
    n = len(stages)
    for i, st in enumerate(stages):
        S = Sx if i == 0 else Ss
        D = So if i == n - 1 else Ss
        if st[0] == "ffn":
            stage_ffn(P, C, S, D, st[1], st[2])
        else:
            MIXERS[st[1] % 4](P, C, S, D, st[1])
    P.finish(So.bufs)
    return P


def host_inputs(inputs, core):
    m = {}
    xc = np.asarray(inputs["x"][2 * core:2 * core + 2], dtype=np.float32).reshape(NTOK, DM)
    m["x"] = np.ascontiguousarray(xc.T)
    m["positions"] = np.ascontiguousarray(np.asarray(inputs["positions"][2 * core:2 * core + 2], dtype=np.int32))
    g = np.asarray(inputs["norm_gains"], dtype=np.float32).reshape(4, 3, 8, 128)
    m["gains"] = np.ascontiguousarray(g.transpose(0, 1, 3, 2))
    for name, shape in WEIGHT_SPECS:
        m[name] = np.ascontiguousarray(np.asarray(inputs[name], dtype=np.float32))
    m.update(host_small(inputs))
    m.update(host_pos(m["positions"]))
    return m


_PROG_CACHE = {}


def run_stages(inputs, stages, cores):
    key = tuple(stages)
    if key not in _PROG_CACHE:
        _PROG_CACHE[key] = build_program(stages)
    P = _PROG_CACHE[key]
    shared = None
    in_maps = []
    for c in cores:
        m = host_inputs(inputs, c) if shared is None else dict(shared)
        if shared is None:
            shared = m
        else:
            xc = np.asarray(inputs["x"][2 * c:2 * c + 2], dtype=np.float32).reshape(NTOK, DM)
            m["x"] = np.ascontiguousarray(xc.T)
            m["positions"] = np.ascontiguousarray(np.asarray(inputs["positions"][2 * c:2 * c + 2], dtype=np.int32))
            m.update(host_pos(m["positions"]))
        in_maps.append(m)
    res = run_bass_kernel_spmd(P.nc, in_maps, core_ids=list(range(len(cores))))
    outs = [np.asarray(r["out"]).T.reshape(2, SEQ, DM) for r in res.results]
    return outs


def kernel(**inputs):
    outs = run_stages(inputs, FULL_STAGES, list(range(8)))
    return np.concatenate(outs, axis=0).astype(np.float32)
```

```python
import numpy as np
import concourse.bass as bass
import concourse.mybir as mybir
from concourse.bass_utils import run_bass_kernel_spmd
from contextlib import ExitStack

F32 = mybir.dt.float32
BF16 = mybir.dt.bfloat16
I32 = mybir.dt.int32
AF = mybir.ActivationFunctionType
ALU = mybir.AluOpType
AX = mybir.AxisListType


class DSem:
    __slots__ = ("sem", "cnt")

    def __init__(self, sem):
        self.sem = sem
        self.cnt = 0


class Buf:
    __slots__ = ("t", "writers", "reads", "ds", "name")

    def __init__(self, t, name=""):
        self.t = t
        self.writers = {}
        self.reads = {}
        self.ds = None
        self.name = name

    def __getitem__(self, k):
        return self.t[k]


class Eng:
    def __init__(self, name, h, sem):
        self.name = name
        self.h = h
        self.sem = sem
        self.count = 0
        self.seen = {}


class Prog:
    def __init__(self):
        self.nc = bass.Bass("TRN2", target_bir_lowering=False)
        nc = self.nc
        self.root = ExitStack()
        self.E = {}
        for name, h in (("pe", nc.tensor), ("act", nc.scalar), ("dve", nc.vector),
                        ("pool", nc.gpsimd), ("sp", nc.sync)):
            sem = self.root.enter_context(nc.semaphore("es_" + name))
            self.E[name] = Eng(name, h, sem)
        self.free_ds = []
        self.n_ds = 0
        self.stage = None
        self.group = None
        self.stage_bufs = []
        self.uid = 0
        self.gbufs = []

    def _scope(self):
        if self.stage is not None:
            return self.stage
        if self.group is not None:
            return self.group
        return self.root

    def begin_group(self):
        assert self.group is None and self.stage is None
        self.group = ExitStack()
        self.group_bufs = []

    def end_group(self):
        assert self.stage is None
        for b in self.group_bufs:
            if b.ds is not None:
                self.free_ds.append(b.ds)
                b.ds = None
        self.group.close()
        self.group = None
        self.group_bufs = []

    def sb(self, shape, dtype, name=None):
        self.uid += 1
        name = (name or "t") + "_%d" % self.uid
        t = self._scope().enter_context(self.nc.sbuf_tensor(name, list(shape), dtype))
        b = Buf(t, name)
        if self.stage is not None:
            self.stage_bufs.append(b)
        elif self.group is not None:
            self.group_bufs.append(b)
        return b

    def ps(self, shape=(128, 512), dtype=F32, name=None):
        self.uid += 1
        name = (name or "p") + "_%d" % self.uid
        t = self._scope().enter_context(self.nc.psum_tensor(name, list(shape), dtype))
        b = Buf(t, name)
        self.stage_bufs.append(b)
        return b

    def dram(self, name, shape, dtype, kind="Internal"):
        t = self.nc.dram_tensor(name, list(shape), dtype, kind=kind)
        return t

    def dbuf(self, ap, name=""):
        b = Buf(ap, name)
        self.gbufs.append(b)
        return b

    def _get_ds(self):
        if self.free_ds:
            return self.free_ds.pop()
        self.n_ds += 1
        sem = self.root.enter_context(self.nc.semaphore("ds_%d" % self.n_ds))
        return DSem(sem)

    def _deps(self, eng, reads, writes, is_dma=False, partial=False):
        need = {}

        def add(tok):
            if tok is None:
                return
            sem, val = tok
            k = id(sem)
            if k not in need or need[k][1] < val:
                need[k] = tok

        for b in reads:
            for tok in b.writers.values():
                add(tok)
        for b in writes:
            if not partial:
                for tok in b.writers.values():
                    if is_dma and b.ds is not None and tok[0] is b.ds.sem:
                        continue
                    add(tok)
            for tok in b.reads.values():
                add(tok)
        for k, (sem, val) in need.items():
            if sem is eng.sem:
                if eng.name == "pe":
                    continue
                if eng.count - val >= 2:
                    continue
            if eng.seen.get(k, 0) >= val:
                continue
            eng.h.wait_ge(sem, val)
            eng.seen[k] = val

    def _record(self, tok, reads, writes, partial=False):
        k = id(tok[0])
        for b in reads:
            old = b.reads.get(k)
            if old is None or old[1] < tok[1]:
                b.reads[k] = tok
        for b in writes:
            if partial:
                b.writers[k] = tok
            else:
                b.writers = {k: tok}
                b.reads = {}

    def op(self, en, fn, reads=(), writes=(), signal=True, partial=False):
        eng = self.E[en]
        self._deps(eng, reads, writes, partial=partial)
        ins = fn(eng.h)
        if signal:
            eng.count += 1
            ins.then_inc(eng.sem, 1)
            tok = (eng.sem, eng.count)
        else:
            tok = (eng.sem, eng.count + 1)
        self._record(tok, reads, writes, partial)
        return ins

    def dma(self, q, out_ap, in_ap, reads=(), writes=(), partial=False, **kw):
        eng = self.E[q]
        self._deps(eng, reads, writes, is_dma=True, partial=partial)
        dst = writes[0]
        if dst.ds is None:
            dst.ds = self._get_ds()
        ins = eng.h.dma_start(out=out_ap, in_=in_ap, **kw)
        dst.ds.cnt += 16
        ins.then_inc(dst.ds.sem, 16)
        tok = (dst.ds.sem, dst.ds.cnt)
        self._record(tok, reads, writes, partial)
        return ins

    def begin_stage(self):
        assert self.stage is None
        self.stage = ExitStack()
        self.stage_bufs = []

    def barrier(self, extra_bufs=()):
        toks = {}
        for e in self.E.values():
            if e.count > 0:
                toks[id(e.sem)] = (e.sem, e.count)
        for b in list(self.stage_bufs) + list(getattr(self, 'group_bufs', [])) + list(self.gbufs) + list(extra_bufs):
            if b.ds is not None and b.ds.cnt > 0:
                toks[id(b.ds.sem)] = (b.ds.sem, b.ds.cnt)
            for k, tok in b.reads.items():
                if k not in toks or toks[k][1] < tok[1]:
                    toks[k] = tok
            for k, tok in b.writers.items():
                if k not in toks or toks[k][1] < tok[1]:
                    toks[k] = tok
        for e in self.E.values():
            for k, (sem, val) in toks.items():
                if sem is e.sem:
                    continue
                if e.seen.get(k, 0) >= val:
                    continue
                e.h.wait_ge(sem, val)
                e.seen[k] = val

    def end_stage(self, extra_bufs=()):
        self.barrier(extra_bufs)
        for b in self.stage_bufs:
            if b.ds is not None:
                self.free_ds.append(b.ds)
                b.ds = None
        self.stage.close()
        self.stage = None
        self.stage_bufs = []

    def finish(self, out_bufs):
        self.barrier(out_bufs)


A_DIL = (1, 4, 16)


def a_tile_start(g, ti):
    dil = A_DIL[g]
    nb = 16 // dil
    r, n = ti // nb, ti % nb
    return n * 128 * dil + r, dil, nb


NTOK = 4096
SEQ = 2048
DM = 1024
EPS = 1e-6


class Stream:
    def __init__(self, P, ap, nb=4):
        self.ap = ap
        self.view = ap.rearrange("(k p) t -> p k t", p=128)
        self.nb = nb
        self.bsz = NTOK // nb
        self.bufs = [P.dbuf(ap, "strm") for _ in range(nb)]

    def buf(self, t0):
        return self.bufs[t0 // self.bsz]


class NormUnit:
    def __init__(self, P, TT, g_ap):
        self.P = P
        self.TT = TT
        self.gcol = P.sb([128, 8], F32, "gcol")
        P.dma("sp", self.gcol[:], g_ap, writes=[self.gcol])
        self.ones = P.sb([128, 128], BF16, "ones")
        P.op("dve", lambda e: e.memset(self.ones[:], 1.0), writes=[self.ones])
        self.epsc = P.sb([128, 1], F32, "epsc")
        P.op("dve", lambda e: e.memset(self.epsc[:], EPS), writes=[self.epsc])
        self.sq = [P.sb([128, 8, TT], BF16, "sq") for _ in range(2)]
        self.rs = [P.sb([128, TT], F32, "rs") for _ in range(2)]
        self.pss = P.ps(name="pss")
        self.i = 0

    def run(self, xt, xn):
        P, TT = self.P, self.TT
        sq = self.sq[self.i % 2]
        rs = self.rs[self.i % 2]
        self.i += 1
        pss, ones, gcol = self.pss, self.ones, self.gcol
        P.op("act", lambda e: e.activation(out=sq[:], in_=xt[:], func=AF.Square), reads=[xt], writes=[sq])
        for k in range(8):
            P.op("pe", lambda e, k=k: e.matmul(pss[:, :TT], lhsT=ones[:], rhs=sq[:, k, :], start=(k == 0), stop=(k == 7)),
                 reads=[ones, sq], writes=[pss], signal=(k == 7))
        P.op("act", lambda e: e.activation(out=rs[:], in_=pss[:, :TT], func=AF.Sqrt, scale=1.0 / DM, bias=self.epsc[:, 0:1]),
             reads=[pss, self.epsc], writes=[rs])
        P.op("dve", lambda e: e.reciprocal(out=rs[:], in_=rs[:]), reads=[rs], writes=[rs])
        for k in range(8):
            P.op("dve", lambda e, k=k: e.scalar_tensor_tensor(out=xn[:, k, :], in0=xt[:, k, :], scalar=gcol[:, k:k + 1], in1=rs[:],
                                                               op0=ALU.mult, op1=ALU.mult),
                 reads=[xt, gcol, rs], writes=[xn], partial=(k > 0))


def load_x(P, S, t0, TT, xt):
    P.dma("sp", xt[:], S.view[:, :, t0:t0 + TT], reads=[S.buf(t0)], writes=[xt])


def store_x(P, D, t0, TT, xt):
    P.dma("sp", D.view[:, :, t0:t0 + TT], xt[:], reads=[xt], writes=[D.buf(t0)], partial=True)


def stage_ffn(P, C, S, D, li, fi):
    TT = 256
    NT = NTOK // TT
    P.begin_stage()
    w_in = C["ffn_w_in"][li, fi].rearrange("(k p) n -> p k n", p=128)
    w_out = C["ffn_w_out"][li, fi].rearrange("(k p) n -> p k n", p=128)
    win = [P.sb([128, 8, 512], BF16, "win") for _ in range(11)]
    wout = [P.sb([128, 2, 1024], BF16, "wout") for _ in range(11)]
    N = NormUnit(P, TT, C["gains"][li, 0 if fi == 0 else 2])
    for g in [0, 5, 6, 1, 7, 2, 8, 3, 9, 4, 10]:
        P.dma("pool", win[g][:], w_in[:, :, g * 512:(g + 1) * 512], writes=[win[g]])
    for g in range(11):
        P.dma("pool", wout[g][:], w_out[:, 2 * g:2 * g + 2, :], writes=[wout[g]])
    xt = [P.sb([128, 8, TT], F32, "xt") for _ in range(2)]
    xn = [P.sb([128, 8, TT], BF16, "xn") for _ in range(2)]
    h = P.sb([128, 22, TT], BF16, "h")
    sg = [P.sb([128, TT], F32, "sg") for _ in range(2)]
    pg = [P.ps(name="pg") for _ in range(2)]
    pu = [P.ps(name="pu") for _ in range(2)]
    py = [P.ps(name="py") for _ in range(2)]

    load_x(P, S, 0, TT, xt[0])
    N.run(xt[0], xn[0])
    for t in range(NT):
        X, XN = xt[t % 2], xn[t % 2]
        if t + 1 < NT:
            load_x(P, S, (t + 1) * TT, TT, xt[(t + 1) % 2])
        for m in range(22):
            a, b, s = pg[m % 2], pu[m % 2], sg[m % 2]
            c0 = m * 128
            gi, go = c0 // 512, c0 % 512
            for k in range(8):
                P.op("pe", lambda e, k=k, gi=gi, go=go: e.matmul(a[:, :TT], lhsT=win[gi][:, k, go:go + 128], rhs=XN[:, k, :],
                                                                 start=(k == 0), stop=(k == 7)),
                     reads=[win[gi], XN], writes=[a], signal=(k == 7))
            c0 = 2816 + m * 128
            gi, go = c0 // 512, c0 % 512
            for k in range(8):
                P.op("pe", lambda e, k=k, gi=gi, go=go: e.matmul(b[:, :TT], lhsT=win[gi][:, k, go:go + 128], rhs=XN[:, k, :],
                                                                 start=(k == 0), stop=(k == 7)),
                     reads=[win[gi], XN], writes=[b], signal=(k == 7))
            P.op("act", lambda e: e.activation(out=s[:], in_=a[:, :TT], func=AF.Silu), reads=[a], writes=[s])
            P.op("dve", lambda e, m=m: e.tensor_tensor(out=h[:, m, :], in0=s[:], in1=b[:, :TT], op=ALU.mult),
                 reads=[s, b], writes=[h], partial=(m > 0))
        if t + 1 < NT:
            N.run(xt[(t + 1) % 2], xn[(t + 1) % 2])
        for mo in range(8):
            c = py[mo % 2]
            for kf in range(22):
                P.op("pe", lambda e, kf=kf, mo=mo: e.matmul(c[:, :TT], lhsT=wout[kf // 2][:, kf % 2, mo * 128:(mo + 1) * 128],
                                                            rhs=h[:, kf, :], start=(kf == 0), stop=(kf == 21)),
                     reads=[wout[kf // 2], h], writes=[c], signal=(kf == 21))
            P.op("dve", lambda e, mo=mo: e.scalar_tensor_tensor(out=X[:, mo, :], in0=c[:, :TT], scalar=0.5, in1=X[:, mo, :],
                                                                 op0=ALU.mult, op1=ALU.add),
                 reads=[c, X], writes=[X], partial=True)
        store_x(P, D, t * TT, TT, X)
    P.end_stage()


def small_specs():
    return [("d_small", [128, 8, 8]), ("ident", [128, 128]), ("c_zeta", [128, 8]), ("c_inv", [128, 64]),
            ("c_decT", [128, 8, 128]), ("c_xi", [128, 8, 128]),
            ("b_grow", [128, 832]), ("b_inv", [128, 16]), ("b_mask", [128, 4, 512]),
            ("a_grow", [128, 128]), ("a_inv", [128, 32]), ("a_mask", [128, 256])]


def int_specs():
    return [("pos_pm", [128, 32]), ("pos_a", [128, 96])]


def scratch_specs():
    return [("c_qkT", [16, 128, NTOK]), ("c_kz", [NTOK, 1024]), ("c_vg", [NTOK, 4096]), ("oT", [16, 128, NTOK]),
            ("b_qkT", [32, 96, NTOK]), ("b_v", [NTOK, 1024]),
            ("a_qkT", [6, 16, 128, SEQ]), ("a_v", [6, SEQ, 1024])]


def host_small(inputs):
    m = {}
    f = lambda a: np.asarray(a, dtype=np.float32)
    cols = [f(inputs["d_conv_w"])[0, j] for j in range(4)] + [f(inputs["d_conv_b"])[0], f(inputs["d_b_rg"])[0],
                                                            f(inputs["d_b_ig"])[0], f(inputs["d_lru_param"])[0]]
    ds = np.stack(cols, axis=-1)
    m["d_small"] = np.ascontiguousarray(ds.reshape(8, 128, 8).transpose(1, 0, 2))
    m["ident"] = np.eye(128, dtype=np.float32)
    lg = np.log(np.float32(1.0) - np.float32(2.0) ** (-5.0 - np.arange(8, dtype=np.float32))).astype(np.float32)
    idx = np.arange(128, dtype=np.float32)
    sc = np.float32(128.0 ** -0.5)
    m["c_zeta"] = np.ascontiguousarray(np.exp((127.0 - idx)[:, None] * lg[None, :]).astype(np.float32))
    m["c_inv"] = np.ascontiguousarray(np.broadcast_to((10000.0 ** (-np.arange(64, dtype=np.float32) * 2.0 / 128.0)).astype(np.float32), (128, 64)))
    diff = idx[None, :] - idx[:, None]
    dec = np.where((diff >= 0)[:, None, :], np.exp(np.where(diff >= 0, diff, 0.0)[:, None, :] * lg[None, :, None]), 0.0)
    m["c_decT"] = np.ascontiguousarray((dec * sc).astype(np.float32))
    xi = np.exp((idx + 1.0)[None, :] * lg[:, None]) * sc
    m["c_xi"] = np.ascontiguousarray(np.broadcast_to(xi[None].astype(np.float32), (128, 8, 128)))
    grow = np.concatenate([f(inputs["b_q_a_gain"])[0], f(inputs["b_kv_a_gain"])[0], f(inputs["b_q_gain"])[0], f(inputs["b_k_gain"])[0]])
    m["b_grow"] = np.ascontiguousarray(np.broadcast_to(grow[None, :], (128, 832)))
    m["b_inv"] = np.ascontiguousarray(np.broadcast_to((10000.0 ** (-np.arange(16, dtype=np.float32) * 2.0 / 32.0)).astype(np.float32), (128, 16)))
    key = np.arange(128)[:, None]
    col = np.arange(512)[None, :]
    m["b_mask"] = np.ascontiguousarray(np.stack([(col - i * 128 >= key) for i in range(4)], axis=1).astype(np.float32))
    agrow = np.concatenate([f(inputs["a_q_gain"])[0], f(inputs["a_k_gain"])[0]])
    m["a_grow"] = np.ascontiguousarray(np.broadcast_to(agrow[None, :], (128, 128)))
    m["a_inv"] = np.ascontiguousarray(np.broadcast_to((10000.0 ** (-np.arange(32, dtype=np.float32) * 2.0 / 64.0)).astype(np.float32), (128, 32)))
    q = np.arange(128)[None, :]
    m["a_mask"] = np.ascontiguousarray(np.concatenate([(key >= q), (key <= q)], axis=1).astype(np.float32))
    return m


def host_pos(pos):
    m = {}
    m["pos_pm"] = np.ascontiguousarray(pos.reshape(32, 128).T)
    cols = []
    for s_ in range(2):
        for g in range(3):
            for ti in range(16):
                st, dil, nb = a_tile_start(g, ti)
                cols.append(pos[s_, st:st + 127 * dil + 1:dil])
    m["pos_a"] = np.ascontiguousarray(np.stack(cols, axis=1).astype(np.int32))
    return m


MIXERS = {}


def stage_mix_d(P, C, S, D, li):
    TT = 512
    P.begin_stage()
    wi_d = C["d_w_in"][0].rearrange("(k p) n -> p k n", p=128)
    wo_d = C["d_w_out"][0].rearrange("(k p) n -> p k n", p=128)
    wrg_d = C["d_w_rg"][0].rearrange("n (cc p) d -> p (n cc) d", p=128)
    wig_d = C["d_w_ig"][0].rearrange("n (cc p) d -> p (n cc) d", p=128)
    N = NormUnit(P, TT, C["gains"][li, 1])
    wi = [P.sb([128, 8, 512], BF16, "wi") for _ in range(4)]
    for g in [2, 0, 3, 1]:
        P.dma("pool", wi[g][:], wi_d[:, :, g * 512:(g + 1) * 512], writes=[wi[g]])
    wrg = P.sb([128, 8, 256], BF16, "wrg")
    wig = P.sb([128, 8, 256], BF16, "wig")
    wo = P.sb([128, 8, 1024], BF16, "wo")
    P.dma("pool", wrg[:], wrg_d, writes=[wrg])
    P.dma("pool", wig[:], wig_d, writes=[wig])
    P.dma("pool", wo[:], wo_d, writes=[wo])
    sm = P.sb([128, 8, 8], F32, "dsm")
    P.dma("sp", sm[:], C["d_small"], writes=[sm])
    nsp = P.sb([128, 8], F32, "nsp")
    onec = P.sb([128, 1], F32, "onec")
    P.op("dve", lambda e: e.memset(onec[:], 1.0), writes=[onec])
    P.op("act", lambda e: e.activation(out=nsp[:], in_=sm[:, :, 7], func=AF.Exp, scale=-1.0), reads=[sm], writes=[nsp])
    P.op("act", lambda e: e.activation(out=nsp[:], in_=nsp[:], func=AF.Ln, bias=onec[:, 0:1]), reads=[nsp, onec], writes=[nsp])
    P.op("dve", lambda e: e.tensor_scalar(out=nsp[:], in0=nsp[:], scalar1=-8.0, scalar2=None, op0=ALU.mult), reads=[nsp], writes=[nsp])

    xt = [P.sb([128, 8, TT], F32, "xt") for _ in range(2)]
    xn = [P.sb([128, 8, TT], BF16, "xn") for _ in range(2)]
    og = P.sb([128, 8, TT], BF16, "og")
    ub = [P.sb([128, 2, TT + 3], F32, "ub") for _ in range(2)]
    uc = [P.sb([128, 2, TT], F32, "uc") for _ in range(2)]
    ucb = [P.sb([128, 2, TT], BF16, "ucb") for _ in range(2)]
    carry_u = P.sb([128, 8, 3], F32, "carry_u")
    carry_h = P.sb([128, 8], F32, "carry_h")
    tmp = {nm: [P.sb([128, TT], F32, nm) for _ in range(2)] for nm in ("r", "ig", "a", "s", "b", "hs", "gl")}
    pu = [P.ps(name="pu") for _ in range(2)]
    pr, pi, pgt = P.ps(name="pr"), P.ps(name="pi"), P.ps(name="pgt")
    py = [P.ps(name="py") for _ in range(2)]

    NT = NTOK // TT
    load_x(P, S, 0, TT, xt[0])
    N.run(xt[0], xn[0])
    it = 0
    for t in range(NT):
        X, XN = xt[t % 2], xn[t % 2]
        if t + 1 < NT:
            load_x(P, S, (t + 1) * TT, TT, xt[(t + 1) % 2])
        if t % (SEQ // TT) == 0:
            P.op("pool", lambda e: e.memset(carry_u[:], 0.0), writes=[carry_u])
            P.op("pool", lambda e: e.memset(carry_h[:], 0.0), writes=[carry_h])
        for n in range(4):
            UB, UC, UCB = ub[n % 2], uc[n % 2], ucb[n % 2]
            for cc in range(2):
                c = 2 * n + cc
                p_u = pu[cc]
                col = 1024 + c * 128
                gi, go = col // 512, col % 512
                for k in range(8):
                    P.op("pe", lambda e, k=k, gi=gi, go=go: e.matmul(p_u[:, :TT], lhsT=wi[gi][:, k, go:go + 128], rhs=XN[:, k, :],
                                                                     start=(k == 0), stop=(k == 7)),
                         reads=[wi[gi], XN], writes=[p_u], signal=(k == 7))
                P.op("pool", lambda e, cc=cc, c=c: e.tensor_copy(out=UB[:, cc, 0:3], in_=carry_u[:, c, :]), reads=[carry_u], writes=[UB], partial=(cc > 0))
                P.op("act", lambda e, cc=cc: e.copy(out=UB[:, cc, 3:3 + TT], in_=p_u[:, :TT]), reads=[p_u], writes=[UB], partial=True)
                P.op("pool", lambda e, cc=cc, c=c: e.tensor_copy(out=carry_u[:, c, :], in_=UB[:, cc, TT:TT + 3]), reads=[UB], writes=[carry_u], partial=True)
                P.op("dve", lambda e, cc=cc, c=c: e.tensor_scalar(out=UC[:, cc, :], in0=UB[:, cc, 3:3 + TT], scalar1=sm[:, c, 3:4], scalar2=sm[:, c, 4:5],
                                                                   op0=ALU.mult, op1=ALU.add), reads=[UB, sm], writes=[UC], partial=(cc > 0))
                for j in range(3):
                    P.op("dve", lambda e, cc=cc, c=c, j=j: e.scalar_tensor_tensor(out=UC[:, cc, :], in0=UB[:, cc, j:j + TT], scalar=sm[:, c, j:j + 1], in1=UC[:, cc, :],
                                                                                   op0=ALU.mult, op1=ALU.add), reads=[UB, sm, UC], writes=[UC], partial=True)
                P.op("act", lambda e, cc=cc: e.copy(out=UCB[:, cc, :], in_=UC[:, cc, :]), reads=[UC], writes=[UCB], partial=(cc > 0))
            for dc in range(2):
                c = 2 * n + dc
                i2 = it % 2
                it += 1
                r, ig, a, s, b, hs, gl = (tmp[nm][i2] for nm in ("r", "ig", "a", "s", "b", "hs", "gl"))
                for cc in range(2):
                    P.op("pe", lambda e, cc=cc, dc=dc, n=n: e.matmul(pr[:, :TT], lhsT=wrg[:, 2 * n + cc, dc * 128:(dc + 1) * 128], rhs=UCB[:, cc, :],
                                                                     start=(cc == 0), stop=(cc == 1)), reads=[wrg, UCB], writes=[pr], signal=(cc == 1))
                for cc in range(2):
                    P.op("pe", lambda e, cc=cc, dc=dc, n=n: e.matmul(pi[:, :TT], lhsT=wig[:, 2 * n + cc, dc * 128:(dc + 1) * 128], rhs=UCB[:, cc, :],
                                                                     start=(cc == 0), stop=(cc == 1)), reads=[wig, UCB], writes=[pi], signal=(cc == 1))
                gi, go = (c * 128) // 512, (c * 128) % 512
                for k in range(8):
                    P.op("pe", lambda e, k=k, gi=gi, go=go: e.matmul(pgt[:, :TT], lhsT=wi[gi][:, k, go:go + 128], rhs=XN[:, k, :],
                                                                     start=(k == 0), stop=(k == 7)),
                         reads=[wi[gi], XN], writes=[pgt], signal=(k == 7))
                P.op("act", lambda e, c=c: e.activation(out=r[:], in_=pr[:, :TT], func=AF.Sigmoid, bias=sm[:, c, 5:6]), reads=[pr, sm], writes=[r])
                P.op("act", lambda e, c=c: e.activation(out=ig[:], in_=pi[:, :TT], func=AF.Sigmoid, bias=sm[:, c, 6:7]), reads=[pi, sm], writes=[ig])
                P.op("act", lambda e, c=c: e.activation(out=a[:], in_=r[:], func=AF.Exp, scale=nsp[:, c:c + 1]), reads=[r, nsp], writes=[a])
                P.op("act", lambda e: e.activation(out=s[:], in_=a[:], func=AF.Square), reads=[a], writes=[s])
                P.op("act", lambda e: e.activation(out=s[:], in_=s[:], func=AF.Sqrt, scale=-1.0, bias=onec[:, 0:1]), reads=[s, onec], writes=[s])
                P.op("dve", lambda e, dc=dc: e.tensor_tensor(out=b[:], in0=ig[:], in1=UC[:, dc, :], op=ALU.mult), reads=[ig, UC], writes=[b])
                P.op("dve", lambda e: e.tensor_tensor(out=b[:], in0=b[:], in1=s[:], op=ALU.mult), reads=[b, s], writes=[b])
                P.op("dve", lambda e, c=c: e.tensor_tensor_scan(out=hs[:], data0=a[:], data1=b[:], initial=carry_h[:, c:c + 1], op0=ALU.mult, op1=ALU.add),
                     reads=[a, b, carry_h], writes=[hs])
                P.op("pool", lambda e, c=c: e.tensor_copy(out=carry_h[:, c:c + 1], in_=hs[:, TT - 1:TT]), reads=[hs], writes=[carry_h], partial=True)
                P.op("act", lambda e: e.activation(out=gl[:], in_=pgt[:, :TT], func=AF.Gelu_apprx_tanh), reads=[pgt], writes=[gl])
                P.op("dve", lambda e, c=c: e.tensor_tensor(out=og[:, c, :], in0=gl[:], in1=hs[:], op=ALU.mult), reads=[gl, hs], writes=[og], partial=(c > 0))
        if t + 1 < NT:
            N.run(xt[(t + 1) % 2], xn[(t + 1) % 2])
        for mo in range(8):
            cps = py[mo % 2]
            for k in range(8):
                P.op("pe", lambda e, k=k, mo=mo: e.matmul(cps[:, :TT], lhsT=wo[:, k, mo * 128:(mo + 1) * 128], rhs=og[:, k, :],
                                                          start=(k == 0), stop=(k == 7)), reads=[wo, og], writes=[cps], signal=(k == 7))
            P.op("dve", lambda e, mo=mo: e.tensor_tensor(out=X[:, mo, :], in0=cps[:, :TT], in1=X[:, mo, :], op=ALU.add),
                 reads=[cps, X], writes=[X], partial=True)
        store_x(P, D, t * TT, TT, X)
    P.end_stage()


TWO_PI = 6.283185307179586
C1_2PI = 6.28125
C2_2PI = TWO_PI - C1_2PI


def mm_group(P, ps_ap, ps_buf, pairs, reads_extra=()):
    n = len(pairs)
    for i, (l, r, bufs) in enumerate(pairs):
        P.op("pe", lambda e, l=l, r=r, i=i: e.matmul(ps_ap, lhsT=l, rhs=r, start=(i == 0), stop=(i == n - 1)),
             reads=list(bufs) + list(reads_extra), writes=[ps_buf], signal=(i == n - 1))


def rope_tables(P, posi, inv, nt, half, cos, sin):
    posf = P.sb([128, nt], F32, "posf")
    ang = P.sb([128, nt, half], F32, "ang")
    a2 = P.sb([128, nt, half], F32, "a2")
    ni = P.sb([128, nt, half], I32, "ni")
    nf = P.sb([128, nt, half], F32, "nf")
    P.op("dve", lambda e: e.tensor_copy(out=posf[:], in_=posi[:]), reads=[posi], writes=[posf])
    P.op("dve", lambda e: e.tensor_tensor(out=ang[:], in0=posf[:, :].unsqueeze(2).to_broadcast([128, nt, half]),
                                          in1=inv[:, :].unsqueeze(1).to_broadcast([128, nt, half]), op=ALU.mult),
         reads=[posf, inv], writes=[ang])
    for dst, shift in ((sin, 0.0), (cos, 1.5707963267948966)):
        P.op("dve", lambda e, shift=shift: e.tensor_scalar(out=a2[:], in0=ang[:], scalar1=shift, scalar2=None, op0=ALU.add), reads=[ang], writes=[a2])
        P.op("dve", lambda e: e.tensor_scalar(out=ni[:], in0=a2[:], scalar1=1.0 / TWO_PI, scalar2=None, op0=ALU.mult), reads=[a2], writes=[ni])
        P.op("dve", lambda e: e.tensor_copy(out=nf[:], in_=ni[:]), reads=[ni], writes=[nf])
        P.op("dve", lambda e: e.scalar_tensor_tensor(out=a2[:], in0=nf[:], scalar=-C1_2PI, in1=a2[:], op0=ALU.mult, op1=ALU.add), reads=[nf, a2], writes=[a2])
        P.op("dve", lambda e: e.scalar_tensor_tensor(out=a2[:], in0=nf[:], scalar=-C2_2PI, in1=a2[:], op0=ALU.mult, op1=ALU.add), reads=[nf, a2], writes=[a2])
        P.op("dve", lambda e: e.tensor_scalar(out=a2[:], in0=a2[:], scalar1=3.1415925, scalar2=-3.1415925, op0=ALU.min, op1=ALU.max), reads=[a2], writes=[a2])
        P.op("act", lambda e, dst=dst: e.activation(out=dst[:], in_=a2[:], func=AF.Sin), reads=[a2], writes=[dst])


def rope_apply(P, ps3, psbuf, out3, outbuf, cosb, sinb, tabs, tmp, shape, partial):
    H, half = shape
    x1, x2 = ps3[:, :, 0:half], ps3[:, :, half:2 * half]
    v = lambda b: b[:, :].rearrange("p (h d) -> p h d", d=half)
    t1, t2, t3, t4 = tmp
    P.op("dve", lambda e: e.tensor_tensor(out=v(t1), in0=x1, in1=cosb, op=ALU.mult), reads=[psbuf] + tabs, writes=[t1])
    P.op("dve", lambda e: e.tensor_tensor(out=v(t2), in0=x2, in1=sinb, op=ALU.mult), reads=[psbuf] + tabs, writes=[t2])
    P.op("dve", lambda e: e.tensor_tensor(out=v(t3), in0=x2, in1=cosb, op=ALU.mult), reads=[psbuf] + tabs, writes=[t3])
    P.op("dve", lambda e: e.tensor_tensor(out=v(t4), in0=x1, in1=sinb, op=ALU.mult), reads=[psbuf] + tabs, writes=[t4])
    P.op("pool", lambda e: e.tensor_tensor(out=out3[:, :, 0:half], in0=v(t1), in1=v(t2), op=ALU.subtract), reads=[t1, t2], writes=[outbuf], partial=partial)
    P.op("pool", lambda e: e.tensor_tensor(out=out3[:, :, half:2 * half], in0=v(t3), in1=v(t4), op=ALU.add), reads=[t3, t4], writes=[outbuf], partial=True)


def stage_tables(P, C, pos_key, inv_key, nt, half, cos, sin):
    P.begin_stage()
    inv = P.sb([128, half], F32, "inv")
    P.dma("sp", inv[:], C[inv_key], writes=[inv])
    posi = P.sb([128, nt], I32, "posi")
    P.dma("sp", posi[:], C[pos_key], writes=[posi])
    rope_tables(P, posi, inv, nt, half, cos, sin)
    P.end_stage()


def stage_c1a(P, C, S, li, cos, sin):
    TT = 512
    P.begin_stage()
    w_d = C["c_w_in"][0].rearrange("(k p) n -> p k n", p=128)
    wc = [P.sb([128, 8, 512], BF16, "wc") for _ in range(4)]
    for g in range(4):
        P.dma("pool", wc[g][:], w_d[:, :, g * 512:(g + 1) * 512], writes=[wc[g]])
    N = NormUnit(P, TT, C["gains"][li, 1])
    ident = P.sb([128, 128], BF16, "ident")
    P.dma("pool", ident[:], C["ident"], writes=[ident])
    zeta = P.sb([128, 8], F32, "zeta")
    P.dma("sp", zeta[:], C["c_zeta"], writes=[zeta])
    xt = P.sb([128, 8, TT], F32, "xt")
    xn = [P.sb([128, 8, TT], BF16, "xn") for _ in range(2)]
    qk = [P.sb([128, 2048], BF16, "qk") for _ in range(2)]
    kz = [P.sb([128, 1024], BF16, "kz") for _ in range(2)]
    tmp = [P.sb([128, 256], F32, "rt") for _ in range(8)]
    qTs = [P.sb([128, 16, TT], BF16, "qTs") for _ in range(2)]
    pp = [P.ps(name="pp") for _ in range(3)]
    ptr = [P.ps(name="ptr") for _ in range(2)]
    NT = NTOK // TT
    qkT_v = C["c_qkT"].rearrange("h d t -> d h t")
    ipp = 0
    itr = 0
    deferred = []
    load_x(P, S, 0, TT, xt)
    N.run(xt, xn[0])
    for t in range(NT):
        XN = xn[t % 2]
        QT = qTs[t % 2]
        if t + 1 < NT:
            load_x(P, S, (t + 1) * TT, TT, xt)
        for sub in range(4):
            j = t * 4 + sub
            QK = qk[j % 2]
            KZ = kz[j % 2]
            for cg in range(4):
                ps = pp[ipp % 3]
                ipp += 1
                mm_group(P, ps[:, :], ps, [(XN[:, k, sub * 128:(sub + 1) * 128], wc[cg][:, k, :], [XN, wc[cg]]) for k in range(8)])
                cb = cos[:, j, :].unsqueeze(1).to_broadcast([128, 4, 64])
                sb_ = sin[:, j, :].unsqueeze(1).to_broadcast([128, 4, 64])
                ps3 = ps[:, :].rearrange("p (h d) -> p h d", d=128)
                out3 = QK[:, cg * 512:(cg + 1) * 512].rearrange("p (h d) -> p h d", d=128)
                rope_apply(P, ps3, ps, out3, QK, cb, sb_, [cos, sin], tmp[4 * (cg % 2):4 * (cg % 2) + 4], (4, 64), partial=(cg > 0))
            while deferred:
                deferred.pop(0)()
            P.op("dve", lambda e: e.tensor_tensor(out=KZ[:, :].rearrange("p (h d) -> p h d", d=128),
                                                  in0=QK[:, 1024:2048].rearrange("p (h d) -> p h d", d=128),
                                                  in1=zeta[:, :].unsqueeze(2).to_broadcast([128, 8, 128]), op=ALU.mult),
                 reads=[QK, zeta], writes=[KZ])
            P.dma("sp", C["c_kz"][j * 128:(j + 1) * 128, :], KZ[:], reads=[KZ], writes=[C["c_kz_buf"]], partial=True)
            def tr_fn(QK=QK, QT=QT, sub=sub, t=t):
                nonlocal itr
                for b4 in range(4):
                    pt = ptr[itr % 2]
                    itr += 1
                    for i in range(4):
                        hh = b4 * 4 + i
                        P.op("pe", lambda e, hh=hh, i=i: e.matmul(pt[:, i * 128:(i + 1) * 128], lhsT=QK[:, hh * 128:(hh + 1) * 128], rhs=ident[:],
                                                                  start=True, stop=True), reads=[QK, ident], writes=[pt], signal=(i == 3))
                    P.op("act", lambda e, b4=b4: e.copy(out=QT[:, b4 * 4:(b4 + 1) * 4, sub * 128:(sub + 1) * 128],
                                                        in_=pt[:, :].rearrange("p (h t) -> p h t", t=128)),
                         reads=[pt], writes=[QT], partial=not (sub == 0 and b4 == 0))
                if sub == 3:
                    P.dma("sp", qkT_v[:, :, t * TT:(t + 1) * TT], QT[:], reads=[QT], writes=[C["c_qkT_buf"]], partial=True)
            deferred.append(tr_fn)
        if t + 1 < NT:
            N.run(xt, xn[(t + 1) % 2])
    while deferred:
        deferred.pop(0)()
    P.end_stage()


def stage_c1b(P, C, S, li):
    TT = 512
    P.begin_stage()
    w_d = C["c_w_in"][0].rearrange("(k p) n -> p k n", p=128)
    wc = [P.sb([128, 8, 512], BF16, "wc") for _ in range(8)]
    for g in range(8):
        P.dma("pool", wc[g][:], w_d[:, :, (4 + g) * 512:(5 + g) * 512], writes=[wc[g]])
    N = NormUnit(P, TT, C["gains"][li, 1])
    xt = P.sb([128, 8, TT], F32, "xt")
    xn = [P.sb([128, 8, TT], BF16, "xn") for _ in range(2)]
    vg = [P.sb([128, 4096], BF16, "vg") for _ in range(2)]
    pp = [P.ps(name="pp") for _ in range(4)]
    NT = NTOK // TT
    ipp = 0
    load_x(P, S, 0, TT, xt)
    N.run(xt, xn[0])
    for t in range(NT):
        XN = xn[t % 2]
        if t + 1 < NT:
            load_x(P, S, (t + 1) * TT, TT, xt)
        for sub in range(4):
            j = t * 4 + sub
            VG = vg[j % 2]
            for cg in range(8):
                ps = pp[ipp % 4]
                ipp += 1
                mm_group(P, ps[:, :], ps, [(XN[:, k, sub * 128:(sub + 1) * 128], wc[cg][:, k, :], [XN, wc[cg]]) for k in range(8)])
                if cg < 4:
                    P.op("dve", lambda e, cg=cg: e.tensor_copy(out=VG[:, cg * 512:(cg + 1) * 512], in_=ps[:, :]), reads=[ps], writes=[VG], partial=(cg > 0))
                else:
                    P.op("act", lambda e, cg=cg: e.activation(out=VG[:, cg * 512:(cg + 1) * 512], in_=ps[:, :], func=AF.Silu), reads=[ps], writes=[VG], partial=True)
            P.dma("sp", C["c_vg"][j * 128:(j + 1) * 128, :], VG[:], reads=[VG], writes=[C["c_vg_buf"]], partial=True)
        if t + 1 < NT:
            N.run(xt, xn[(t + 1) % 2])
    P.end_stage()


def stage_c2(P, C):
    P.begin_stage()
    ident = P.sb([128, 128], BF16, "ident")
    P.dma("pool", ident[:], C["ident"], writes=[ident])
    decT = P.sb([128, 8, 128], F32, "decT")
    P.dma("sp", decT[:], C["c_decT"], writes=[decT])
    xib = P.sb([128, 8, 128], F32, "xib")
    P.dma("sp", xib[:], C["c_xi"], writes=[xib])
    eps5 = P.sb([128, 1], F32, "eps5")
    P.op("dve", lambda e: e.memset(eps5[:], 1e-5), writes=[eps5])
    lg = np.log(np.float32(1.0) - np.float32(2.0) ** (-5.0 - np.arange(8, dtype=np.float32))).astype(np.float32)
    cd = [float(np.exp(np.float32(128.0) * lg[h])) for h in range(8)]
    qkc = [P.sb([128, 16, 128], BF16, "qkc") for _ in range(2)]
    kzc = [P.sb([128, 1024], BF16, "kzc") for _ in range(2)]
    vgc = [P.sb([128, 4096], BF16, "vgc") for _ in range(2)]
    qx = [P.sb([128, 8, 128], BF16, "qx") for _ in range(2)]
    R = P.sb([128, 8, 256], F32, "R")
    Rb = P.sb([128, 8, 256], BF16, "Rb")
    AT = [P.sb([128, 128], BF16, "AT") for _ in range(2)]
    st = [P.sb([128, 6], F32, "st") for _ in range(2)]
    mv = [P.sb([128, 2], F32, "mv") for _ in range(2)]
    rstd = [P.sb([128, 1], F32, "rstd") for _ in range(2)]
    yn = [P.sb([128, 256], F32, "yn") for _ in range(2)]
    ob = [P.sb([128, 2048], BF16, "ob") for _ in range(2)]
    oTs = [P.sb([128, 16, 512], BF16, "oTs") for _ in range(2)]
    ps_s = [P.ps(name="ps_s") for _ in range(2)]
    ps_y = [P.ps(name="ps_y") for _ in range(2)]
    ps_u = [P.ps(name="ps_u") for _ in range(2)]
    ptr = [P.ps(name="ptr") for _ in range(2)]
    qkT_v = C["c_qkT"].rearrange("h d t -> d h t")
    oT_v = C["oT"].rearrange("k f t -> f k t")
    it = 0
    itr = 0

    def load_chunk(g):
        i = g % 2
        P.dma("sp", qkc[i][:], qkT_v[:, :, g * 128:(g + 1) * 128], reads=[C["c_qkT_buf"]], writes=[qkc[i]])
        P.dma("sp", kzc[i][:], C["c_kz"][g * 128:(g + 1) * 128, :], reads=[C["c_kz_buf"]], writes=[kzc[i]])
        P.dma("sp", vgc[i][:], C["c_vg"][g * 128:(g + 1) * 128, :], reads=[C["c_vg_buf"]], writes=[vgc[i]])

    load_chunk(0)
    for g in range(32):
        c = g % 16
        QKC, KZC, VGC, QX, OB = qkc[g % 2], kzc[g % 2], vgc[g % 2], qx[g % 2], ob[g % 2]
        OT = oTs[(g // 4) % 2]
        if g + 1 < 32:
            load_chunk(g + 1)
        if c > 0:
            P.op("pool", lambda e: e.tensor_tensor(out=QX[:], in0=QKC[:, 0:8, :], in1=xib[:], op=ALU.mult), reads=[QKC, xib], writes=[QX])
        for h in range(8):
            i2 = it % 2
            it += 1
            pss, psy, psu = ps_s[i2], ps_y[i2], ps_u[i2]
            vh = VGC[:, h * 256:(h + 1) * 256]
            P.op("pe", lambda e, h=h: e.matmul(pss[:, 0:128], lhsT=QKC[:, 8 + h, :], rhs=QKC[:, h, :], start=True, stop=True), reads=[QKC], writes=[pss])
            P.op("dve", lambda e, h=h: e.tensor_tensor(out=AT[i2][:], in0=pss[:, 0:128], in1=decT[:, h, :], op=ALU.mult), reads=[pss, decT], writes=[AT[i2]])
            if c > 0:
                mm_group(P, psy[:, 0:256], psy, [(AT[i2][:], vh, [AT[i2], VGC]), (QX[:, h, :], Rb[:, h, :], [QX, Rb])])
            else:
                mm_group(P, psy[:, 0:256], psy, [(AT[i2][:], vh, [AT[i2], VGC])])
            if c < 15:
                P.op("pe", lambda e, h=h: e.matmul(psu[:, 0:256], lhsT=KZC[:, h * 128:(h + 1) * 128], rhs=VGC[:, h * 256:(h + 1) * 256], start=True, stop=True),
                     reads=[KZC, VGC], writes=[psu])
                if c == 0:
                    P.op("dve", lambda e, h=h: e.tensor_copy(out=R[:, h, :], in_=psu[:, 0:256]), reads=[psu, Rb], writes=[R], partial=True)
                else:
                    P.op("dve", lambda e, h=h: e.scalar_tensor_tensor(out=R[:, h, :], in0=R[:, h, :], scalar=cd[h], in1=psu[:, 0:256], op0=ALU.mult, op1=ALU.add),
                         reads=[psu, R], writes=[R], partial=True)
                P.op("act", lambda e, h=h: e.copy(out=Rb[:, h, :], in_=R[:, h, :]), reads=[R], writes=[Rb], partial=True)
            P.op("dve", lambda e: e.bn_stats(out=st[i2][:], in_=psy[:, 0:256]), reads=[psy], writes=[st[i2]])
            P.op("dve", lambda e: e.bn_aggr(out=mv[i2][:], in_=st[i2][:]), reads=[st[i2]], writes=[mv[i2]])
            P.op("act", lambda e: e.activation(out=rstd[i2][:], in_=mv[i2][:, 1:2], func=AF.Sqrt, bias=eps5[:, 0:1]), reads=[mv[i2], eps5], writes=[rstd[i2]])
            P.op("dve", lambda e: e.reciprocal(out=rstd[i2][:], in_=rstd[i2][:]), reads=[rstd[i2]], writes=[rstd[i2]])
            P.op("dve", lambda e: e.tensor_scalar(out=yn[i2][:], in0=psy[:, 0:256], scalar1=mv[i2][:, 0:1], scalar2=rstd[i2][:, 0:1], op0=ALU.subtract, op1=ALU.mult),
                 reads=[psy, mv[i2], rstd[i2]], writes=[yn[i2]])
            P.op("pool", lambda e, h=h: e.tensor_tensor(out=OB[:, h * 256:(h + 1) * 256], in0=yn[i2][:], in1=VGC[:, 2048 + h * 256:2048 + (h + 1) * 256], op=ALU.mult),
                 reads=[yn[i2], VGC], writes=[OB], partial=(h > 0))
        for b4 in range(4):
            pt = ptr[itr % 2]
            itr += 1
            for i in range(4):
                kk = b4 * 4 + i
                P.op("pe", lambda e, kk=kk, i=i: e.matmul(pt[:, i * 128:(i + 1) * 128], lhsT=OB[:, kk * 128:(kk + 1) * 128], rhs=ident[:], start=True, stop=True),
                     reads=[OB, ident], writes=[pt], signal=(i == 3))
            P.op("act", lambda e, b4=b4: e.copy(out=OT[:, b4 * 4:(b4 + 1) * 4, (g % 4) * 128:(g % 4 + 1) * 128], in_=pt[:, :].rearrange("p (k t) -> p k t", t=128)),
                 reads=[pt], writes=[OT], partial=not (g % 4 == 0 and b4 == 0))
        if g % 4 == 3:
            t0 = (g // 4) * 512
            P.dma("sp", oT_v[:, 0:16, t0:t0 + 512], OT[:], reads=[OT], writes=[C["oT_buf"]], partial=True)
    P.end_stage()


def stage_outproj(P, C, S, D, w_ap, nk):
    TT = 512
    P.begin_stage()
    w_d = w_ap.rearrange("(k p) n -> p k n", p=128)
    wo = [P.sb([128, 4, 1024], BF16, "wo") for _ in range(nk // 4)]
    for g in range(nk // 4):
        P.dma("pool", wo[g][:], w_d[:, 4 * g:4 * g + 4, :], writes=[wo[g]])
    oT_v = C["oT"].rearrange("k f t -> f k t")
    xt = [P.sb([128, 8, TT], F32, "xt") for _ in range(2)]
    ot = [P.sb([128, nk, TT], BF16, "ot") for _ in range(2)]
    py = [P.ps(name="py") for _ in range(2)]
    NT = NTOK // TT

    def ld(t):
        load_x(P, S, t * TT, TT, xt[t % 2])
        P.dma("sp", ot[t % 2][:], oT_v[:, 0:nk, t * TT:(t + 1) * TT], reads=[C["oT_buf"]], writes=[ot[t % 2]])

    ld(0)
    for t in range(NT):
        X, OTT = xt[t % 2], ot[t % 2]
        if t + 1 < NT:
            ld(t + 1)
        for mo in range(8):
            cps = py[mo % 2]
            mm_group(P, cps[:, :], cps, [(wo[k // 4][:, k % 4, mo * 128:(mo + 1) * 128], OTT[:, k, :], [wo[k // 4], OTT]) for k in range(nk)])
            P.op("dve", lambda e, mo=mo: e.tensor_tensor(out=X[:, mo, :], in0=cps[:, :], in1=X[:, mo, :], op=ALU.add), reads=[cps, X], writes=[X], partial=True)
        store_x(P, D, t * TT, TT, X)
    P.end_stage()


def stage_mix_c(P, C, S, D, li):
    P.begin_group()
    cos = P.sb([128, 32, 64], F32, "cos")
    sin = P.sb([128, 32, 64], F32, "sin")
    stage_tables(P, C, "pos_pm", "c_inv", 32, 64, cos, sin)
    stage_c1a(P, C, S, li, cos, sin)
    stage_c1b(P, C, S, li)
    stage_c2(P, C)
    stage_outproj(P, C, S, D, C["c_w_out"][0], 16)
    P.end_group()


def v3(ap, d):
    return ap.rearrange("p (h d) -> p h d", d=d)


def stage_b1(P, C, S, li, cos, sin, rk_all):
    TT = 512
    P.begin_stage()
    wbi = P.sb([128, 8, 672], BF16, "wbi")
    P.dma("pool", wbi[:], C["b_w_in"][0].rearrange("(k p) n -> p k n", p=128), writes=[wbi])
    wqu = P.sb([128, 3, 1536], BF16, "wqu")
    P.dma("pool", wqu[:], C["b_w_q_up"][0].rearrange("(k p) n -> p k n", p=128), writes=[wqu])
    wkvu = P.sb([128, 2, 2048], BF16, "wkvu")
    P.dma("pool", wkvu[:], C["b_w_kv_up"][0].rearrange("(k p) n -> p k n", p=128), writes=[wkvu])
    N = NormUnit(P, TT, C["gains"][li, 1])
    ident = P.sb([128, 128], BF16, "ident")
    P.dma("pool", ident[:], C["ident"], writes=[ident])
    grow = P.sb([128, 384 + 256 + 96 + 96], F32, "grow")
    P.dma("sp", grow[:], C["b_grow"], writes=[grow])
    gqa, gkva = grow[:, 0:384], grow[:, 384:640]
    GQ0, GK0 = 640, 736
    epsq = P.sb([128, 3], F32, "epsq")
    P.op("dve", lambda e: e.memset(epsq[:, 0:1], 1e-6), writes=[epsq])
    P.op("dve", lambda e: e.memset(epsq[:, 1:2], 96e-6), writes=[epsq], partial=True)
    xt = P.sb([128, 8, TT], F32, "xt")
    xn = [P.sb([128, 8, TT], BF16, "xn") for _ in range(2)]
    sq = [P.sb([128, 512], F32, "sq") for _ in range(2)]
    ssa = [P.sb([128, 4], F32, "ssa") for _ in range(2)]
    cn = [P.sb([128, 640], BF16, "cn") for _ in range(2)]
    cT = [P.sb([128, 5, 128], BF16, "cT") for _ in range(2)]
    qf = [P.sb([128, 1536], F32, "qf") for _ in range(2)]
    kvf = [P.sb([128, 2048], F32, "kvf") for _ in range(2)]
    krf = [P.sb([128, 32], F32, "krf") for _ in range(2)]
    ss16 = [P.sb([128, 32], F32, "ss16") for _ in range(2)]
    tq = [P.sb([128, 1536], F32, "tq") for _ in range(2)]
    gt = [P.sb([128, 8, 16], F32, "gt") for _ in range(2)]
    rt = [P.sb([128, 16 * 16], F32, "rt") for _ in range(4)]
    krr = [P.sb([128, 32], F32, "krr") for _ in range(2)]
    qb = [P.sb([128, 1536], BF16, "qb") for _ in range(2)]
    kb = [P.sb([128, 1536], BF16, "kb") for _ in range(2)]
    vb = [P.sb([128, 1024], BF16, "vb") for _ in range(2)]
    qTs = [P.sb([128, 32, 128], BF16, "qTs") for _ in range(2)]
    psA, psB = P.ps(name="psA"), P.ps(name="psB")
    pq = [P.ps(name="pq") for _ in range(3)]
    ptr = [P.ps(name="ptr") for _ in range(2)]
    qkT_v = C["b_qkT"].rearrange("h d t -> d h t")
    NT = NTOK // TT
    ipq = 0
    itr = 0
    import os
    B1CUT = int(os.environ.get("B1CUT", "99"))
    deferred = []
    load_x(P, S, 0, TT, xt)
    N.run(xt, xn[0])
    for t in range(NT):
        XN = xn[t % 2]
        if t + 1 < NT:
            load_x(P, S, (t + 1) * TT, TT, xt)
        for sub in range(4):
            j = t * 4 + sub
            b = j % 2
            SQ, SSA, CN, CT, QF, KVF, KRF, SS16, TQ, GT, KRR, QB, KB, VB, QT = (sq[b], ssa[b], cn[b], cT[b], qf[b], kvf[b], krf[b], ss16[b], tq[b], gt[b],
                                                                                  krr[b], qb[b], kb[b], vb[b], qTs[b])
            xs_ = lambda k: XN[:, k, sub * 128:(sub + 1) * 128]
            mm_group(P, psA[:, 0:384], psA, [(xs_(k), wbi[:, k, 0:384], [XN, wbi]) for k in range(8)])
            mm_group(P, psB[:, 0:288], psB, [(xs_(k), wbi[:, k, 384:672], [XN, wbi]) for k in range(8)])
            P.op("act", lambda e: e.activation(out=SQ[:, 0:384], in_=psA[:, 0:384], func=AF.Square), reads=[psA], writes=[SQ])
            P.op("dve", lambda e: e.reduce_sum(out=SSA[:, 0:1], in_=SQ[:, 0:384], axis=AX.X), reads=[SQ], writes=[SSA])
            P.op("act", lambda e: e.activation(out=SQ[:, 0:288], in_=psB[:, 0:288], func=AF.Square), reads=[psB], writes=[SQ])
            P.op("dve", lambda e: e.reduce_sum(out=SSA[:, 1:2], in_=SQ[:, 0:256], axis=AX.X), reads=[SQ], writes=[SSA], partial=True)
            P.op("dve", lambda e: e.reduce_sum(out=SSA[:, 2:3], in_=SQ[:, 256:288], axis=AX.X), reads=[SQ], writes=[SSA], partial=True)
            P.op("act", lambda e: e.activation(out=SSA[:, 0:1], in_=SSA[:, 0:1], func=AF.Sqrt, scale=1.0 / 384, bias=epsq[:, 0:1]), reads=[SSA, epsq], writes=[SSA], partial=True)
            P.op("act", lambda e: e.activation(out=SSA[:, 1:2], in_=SSA[:, 1:2], func=AF.Sqrt, scale=1.0 / 256, bias=epsq[:, 0:1]), reads=[SSA, epsq], writes=[SSA], partial=True)
            P.op("dve", lambda e: e.reciprocal(out=SSA[:, 0:2], in_=SSA[:, 0:2]), reads=[SSA], writes=[SSA], partial=True)
            P.op("dve", lambda e: e.scalar_tensor_tensor(out=CN[:, 0:384], in0=psA[:, 0:384], scalar=SSA[:, 0:1], in1=gqa, op0=ALU.mult, op1=ALU.mult),
                 reads=[psA, SSA, grow], writes=[CN])
            P.op("dve", lambda e: e.scalar_tensor_tensor(out=CN[:, 384:640], in0=psB[:, 0:256], scalar=SSA[:, 1:2], in1=gkva, op0=ALU.mult, op1=ALU.mult),
                 reads=[psB, SSA, grow], writes=[CN], partial=True)
            P.op("act", lambda e: e.copy(out=KRF[:], in_=psB[:, 256:288]), reads=[psB], writes=[KRF])
            if B1CUT <= 1:
                continue
            pt = ptr[itr % 2]
            itr += 1
            for i in range(4):
                P.op("pe", lambda e, i=i: e.matmul(pt[:, i * 128:(i + 1) * 128], lhsT=CN[:, i * 128:(i + 1) * 128], rhs=ident[:], start=True, stop=True),
                     reads=[CN, ident], writes=[pt], signal=(i == 3))
            P.op("act", lambda e: e.copy(out=CT[:, 0:4, :], in_=v3(pt[:, :], 128)), reads=[pt], writes=[CT])
            pt = ptr[itr % 2]
            itr += 1
            P.op("pe", lambda e: e.matmul(pt[:, 0:128], lhsT=CN[:, 512:640], rhs=ident[:], start=True, stop=True), reads=[CN, ident], writes=[pt])
            P.op("act", lambda e: e.copy(out=CT[:, 4, :], in_=pt[:, 0:128]), reads=[pt], writes=[CT], partial=True)
            if B1CUT <= 2:
                continue
            cs, sn = cos[:, j, :], sin[:, j, :]
            for o, g0 in ((0, GQ0), (4, GK0)):
                ga, gb = grow[:, g0 + 64:g0 + 80], grow[:, g0 + 80:g0 + 96]
                for idx, (gg, tt) in enumerate(((ga, cs), (gb, sn), (gb, cs), (ga, sn))):
                    P.op("pool", lambda e, o=o, idx=idx, gg=gg, tt=tt: e.tensor_tensor(out=GT[:, o + idx, :], in0=gg, in1=tt, op=ALU.mult),
                         reads=[grow, cos, sin], writes=[GT], partial=not (o == 0 and idx == 0))
            if B1CUT <= 3:
                continue
            for hb in range(4):
                ps = pq[ipq % 3]
                ipq += 1
                mm_group(P, ps[:, 0:384], ps, [(CT[:, kk, :], wqu[:, kk, hb * 384:(hb + 1) * 384], [CT, wqu]) for kk in range(3)])
                P.op("act", lambda e, hb=hb: e.copy(out=QF[:, hb * 384:(hb + 1) * 384], in_=ps[:, 0:384]), reads=[ps], writes=[QF], partial=(hb > 0))
                P.op("act", lambda e: e.activation(out=SQ[:, 0:384], in_=ps[:, 0:384], func=AF.Square), reads=[ps], writes=[SQ])
                P.op("dve", lambda e, hb=hb: e.reduce_sum(out=SS16[:, hb * 4:(hb + 1) * 4], in_=v3(SQ[:, 0:384], 96), axis=AX.X), reads=[SQ], writes=[SS16],
                     partial=(hb > 0))
            for hb in range(4):
                ps = pq[ipq % 3]
                ipq += 1
                mm_group(P, ps[:, :], ps, [(CT[:, 3 + kk, :], wkvu[:, kk, hb * 512:(hb + 1) * 512], [CT, wkvu]) for kk in range(2)])
                P.op("act", lambda e, hb=hb: e.copy(out=KVF[:, hb * 512:(hb + 1) * 512], in_=ps[:, :]), reads=[ps], writes=[KVF], partial=(hb > 0))
                P.op("act", lambda e: e.activation(out=v3(SQ[:, 0:256], 64), in_=v3(ps[:, :], 128)[:, :, 0:64], func=AF.Square), reads=[ps], writes=[SQ])
                P.op("dve", lambda e, hb=hb: e.reduce_sum(out=SS16[:, 16 + hb * 4:16 + (hb + 1) * 4], in_=v3(SQ[:, 0:256], 64), axis=AX.X), reads=[SQ], writes=[SS16],
                     partial=True)
            if B1CUT <= 4:
                continue
            while deferred:
                deferred.pop(0)()
            P.op("act", lambda e: e.activation(out=SS16[:, 0:16], in_=SS16[:, 0:16], func=AF.Sqrt, scale=1.0 / 96, bias=epsq[:, 0:1]), reads=[SS16, epsq], writes=[SS16], partial=True)
            P.op("dve", lambda e: e.tensor_scalar(out=SS16[:, 16:32], in0=SS16[:, 16:32], scalar1=SSA[:, 2:3], scalar2=None, op0=ALU.add), reads=[SS16, SSA], writes=[SS16], partial=True)
            P.op("act", lambda e: e.activation(out=SS16[:, 16:32], in_=SS16[:, 16:32], func=AF.Sqrt, bias=epsq[:, 1:2]), reads=[SS16, epsq], writes=[SS16], partial=True)
            P.op("dve", lambda e: e.reciprocal(out=SS16[:, 0:16], in_=SS16[:, 0:16]), reads=[SS16], writes=[SS16], partial=True)
            P.op("dve", lambda e, j=j: e.reciprocal(out=rk_all[:, j, :], in_=SS16[:, 16:32]), reads=[SS16], writes=[rk_all], partial=True)
            if B1CUT <= 5:
                continue
            QF3, TQ3 = v3(QF[:, :], 96), v3(TQ[:, :], 96)
            P.op("dve", lambda e: e.tensor_tensor(out=TQ3[:, :, 0:64], in0=QF3[:, :, 0:64], in1=grow[:, GQ0:GQ0 + 64].unsqueeze(1).to_broadcast([128, 16, 64]), op=ALU.mult),
                 reads=[QF, grow], writes=[TQ])
            x1, x2 = QF3[:, :, 64:80], QF3[:, :, 80:96]
            bc = lambda i: GT[:, i, :].unsqueeze(1).to_broadcast([128, 16, 16])
            r3 = [v3(r_[:, :], 16) for r_ in rt]
            P.op("dve", lambda e: e.tensor_tensor(out=r3[0], in0=x1, in1=bc(0), op=ALU.mult), reads=[QF, GT], writes=[rt[0]])
            P.op("dve", lambda e: e.tensor_tensor(out=r3[1], in0=x2, in1=bc(1), op=ALU.mult), reads=[QF, GT], writes=[rt[1]])
            P.op("dve", lambda e: e.tensor_tensor(out=r3[2], in0=x2, in1=bc(2), op=ALU.mult), reads=[QF, GT], writes=[rt[2]])
            P.op("dve", lambda e: e.tensor_tensor(out=r3[3], in0=x1, in1=bc(3), op=ALU.mult), reads=[QF, GT], writes=[rt[3]])
            P.op("pool", lambda e: e.tensor_tensor(out=TQ3[:, :, 64:80], in0=r3[0], in1=r3[1], op=ALU.subtract), reads=[rt[0], rt[1]], writes=[TQ], partial=True)
            P.op("pool", lambda e: e.tensor_tensor(out=TQ3[:, :, 80:96], in0=r3[2], in1=r3[3], op=ALU.add), reads=[rt[2], rt[3]], writes=[TQ], partial=True)
            P.op("dve", lambda e: e.tensor_tensor(out=v3(QB[:, :], 96), in0=TQ3, in1=SS16[:, 0:16].unsqueeze(2).to_broadcast([128, 16, 96]), op=ALU.mult),
                 reads=[TQ, SS16], writes=[QB])
            if B1CUT <= 6:
                continue
            KV3, KB3 = v3(KVF[:, :], 128), v3(KB[:, :], 96)
            P.op("pool", lambda e: e.tensor_tensor(out=KB3[:, :, 0:64], in0=KV3[:, :, 0:64], in1=grow[:, GK0:GK0 + 64].unsqueeze(1).to_broadcast([128, 16, 64]), op=ALU.mult),
                 reads=[KVF, grow], writes=[KB])
            k1, k2 = KRF[:, 0:16], KRF[:, 16:32]
            P.op("dve", lambda e: e.tensor_tensor(out=rt[0][:, 0:16], in0=k1, in1=GT[:, 4, :], op=ALU.mult), reads=[KRF, GT], writes=[rt[0]])
            P.op("dve", lambda e: e.tensor_tensor(out=rt[1][:, 0:16], in0=k2, in1=GT[:, 5, :], op=ALU.mult), reads=[KRF, GT], writes=[rt[1]])
            P.op("dve", lambda e: e.tensor_tensor(out=rt[2][:, 0:16], in0=k2, in1=GT[:, 6, :], op=ALU.mult), reads=[KRF, GT], writes=[rt[2]])
            P.op("dve", lambda e: e.tensor_tensor(out=rt[3][:, 0:16], in0=k1, in1=GT[:, 7, :], op=ALU.mult), reads=[KRF, GT], writes=[rt[3]])
            P.op("pool", lambda e: e.tensor_tensor(out=KRR[:, 0:16], in0=rt[0][:, 0:16], in1=rt[1][:, 0:16], op=ALU.subtract), reads=[rt[0], rt[1]], writes=[KRR])
            P.op("pool", lambda e: e.tensor_tensor(out=KRR[:, 16:32], in0=rt[2][:, 0:16], in1=rt[3][:, 0:16], op=ALU.add), reads=[rt[2], rt[3]], writes=[KRR], partial=True)
            P.op("pool", lambda e: e.tensor_copy(out=KB3[:, :, 64:96], in_=KRR[:, :].unsqueeze(1).to_broadcast([128, 16, 32])), reads=[KRR], writes=[KB], partial=True)
            P.op("act", lambda e: e.copy(out=v3(VB[:, :], 64), in_=KV3[:, :, 64:128]), reads=[KVF], writes=[VB])
            P.dma("sp", C["b_v"][j * 128:(j + 1) * 128, :], VB[:], reads=[VB], writes=[C["b_v_buf"]], partial=True)
            if B1CUT <= 7:
                continue
            def tr_fn(QB=QB, KB=KB, QT=QT, j=j):
                nonlocal itr
                for src, base in ((QB, 0), (KB, 16)):
                    for b4 in range(4):
                        pt = ptr[itr % 2]
                        itr += 1
                        for i in range(4):
                            hh = b4 * 4 + i
                            P.op("pe", lambda e, hh=hh, i=i, src=src: e.matmul(pt[0:96, i * 128:(i + 1) * 128], lhsT=src[:, hh * 96:(hh + 1) * 96], rhs=ident[:], start=True, stop=True),
                                 reads=[src, ident], writes=[pt], signal=(i == 3))
                        P.op("act", lambda e, b4=b4, base=base: e.copy(out=QT[0:96, base + b4 * 4:base + (b4 + 1) * 4, :], in_=v3(pt[0:96, :], 128)),
                             reads=[pt], writes=[QT], partial=not (base == 0 and b4 == 0))
                for q4 in range(4):
                    P.dma("sp", qkT_v[:, q4 * 8:(q4 + 1) * 8, j * 128:(j + 1) * 128], QT[0:96, q4 * 8:(q4 + 1) * 8, :], reads=[QT], writes=[C["b_qkT_buf"]], partial=True)
            deferred.append(tr_fn)
        if t + 1 < NT:
            N.run(xt, xn[(t + 1) % 2])
    while deferred:
        deferred.pop(0)()
    P.end_stage()


def stage_b2(P, C, rk_all):
    P.begin_stage()
    mask = P.sb([128, 4, 512], BF16, "mask")
    P.dma("pool", mask[:], C["b_mask"], writes=[mask])
    ones = P.sb([128, 64], BF16, "ones64")
    P.op("dve", lambda e: e.memset(ones[:], 1.0), writes=[ones])
    V = P.sb([128, 16, 1024], BF16, "V")
    qT = [P.sb([128, 2048], BF16, "qT") for _ in range(2)]
    kT = [P.sb([128, 2048], BF16, "kT") for _ in range(2)]
    PT = [P.sb([128, 512], BF16, "PT") for _ in range(4)]
    rden = [P.sb([128, 512], F32, "rden") for _ in range(2)]
    oTs = [P.sb([128, 2048], BF16, "oTs") for _ in range(2)]
    ps_s = [P.ps(name="ps_s") for _ in range(3)]
    psn = [P.ps(name="psn") for _ in range(2)]
    psd = [P.ps(name="psd") for _ in range(2)]
    ist = 0
    iq = 0

    def load_head(s, h, i):
        P.dma("sp", qT[i][0:96, :], C["b_qkT"][h, :, s * 2048:(s + 1) * 2048], reads=[C["b_qkT_buf"]], writes=[qT[i]])
        P.dma("sp", kT[i][0:96, :], C["b_qkT"][16 + h, :, s * 2048:(s + 1) * 2048], reads=[C["b_qkT_buf"]], writes=[kT[i]])

    for s in range(2):
        P.dma("sp", V[:], C["b_v"][s * 2048:(s + 1) * 2048, :].rearrange("(j p) n -> p j n", p=128), reads=[C["b_v_buf"]], writes=[V])
        load_head(s, 0, 0)
        steps = [(h, qb, j) for h in range(16) for qb in range(4) for j in range(4 * qb + 4)]
        banks = {}

        def emit_qk(idx):
            nonlocal ist
            h, qb, j = steps[idx]
            if qb == 0 and j == 0 and h + 1 < 16:
                load_head(s, h + 1, (h + 1) % 2)
            QT, KT = qT[h % 2], kT[h % 2]
            pss = ps_s[ist % 3]
            pt_ = PT[ist % 4]
            ist += 1
            P.op("pe", lambda e: e.matmul(pss[:, :], lhsT=KT[0:96, j * 128:(j + 1) * 128], rhs=QT[0:96, qb * 512:(qb + 1) * 512], start=True, stop=True),
                 reads=[KT, QT], writes=[pss])
            P.op("act", lambda e: e.activation(out=pt_[:], in_=pss[:, :], func=AF.Exp, scale=rk_all[:, s * 16 + j, h:h + 1]),
                 reads=[pss, rk_all], writes=[pt_])
            if j >= 4 * qb:
                P.op("dve", lambda e: e.tensor_tensor(out=pt_[:], in0=pt_[:], in1=mask[:, j - 4 * qb, :], op=ALU.mult), reads=[pt_, mask], writes=[pt_])
            return pt_

        def emit_pv(idx, pt_):
            nonlocal iq
            h, qb, j = steps[idx]
            e0 = (h % 2) * 64
            OT = oTs[(h // 2) % 2]
            nj = 4 * qb + 4
            if j == 0:
                banks[(h, qb)] = (psn[iq % 2], psd[iq % 2], rden[iq % 2])
                iq += 1
            pn, pd, RD = banks[(h, qb)]
            P.op("pe", lambda e: e.matmul(pn[e0:e0 + 64, :], lhsT=V[:, j, h * 64:(h + 1) * 64], rhs=pt_[:], start=(j == 0), stop=(j == nj - 1)),
                 reads=[V, pt_], writes=[pn], signal=False, partial=(j > 0))
            P.op("pe", lambda e: e.matmul(pd[e0:e0 + 64, :], lhsT=ones[:], rhs=pt_[:], start=(j == 0), stop=(j == nj - 1)),
                 reads=[ones, pt_], writes=[pd], signal=True, partial=(j > 0))
            if j == nj - 1:
                P.op("dve", lambda e: e.reciprocal(out=RD[e0:e0 + 64, :], in_=pd[e0:e0 + 64, :]), reads=[pd], writes=[RD])
                P.op("dve", lambda e: e.tensor_tensor(out=OT[e0:e0 + 64, qb * 512:(qb + 1) * 512], in0=pn[e0:e0 + 64, :], in1=RD[e0:e0 + 64, :], op=ALU.mult),
                     reads=[pn, RD], writes=[OT], partial=not (h % 2 == 0 and qb == 0))
                if h % 2 == 1 and qb == 3:
                    P.dma("sp", C["oT"][h // 2, :, s * 2048:(s + 1) * 2048], OT[:], reads=[OT], writes=[C["oT_buf"]], partial=True)

        pend = [emit_qk(0), emit_qk(1)]
        for idx in range(len(steps)):
            if idx + 2 < len(steps):
                pend.append(emit_qk(idx + 2))
            emit_pv(idx, pend.pop(0))
    P.end_stage()


def stage_mix_b(P, C, S, D, li):
    P.begin_group()
    cos = P.sb([128, 32, 16], F32, "cos")
    sin = P.sb([128, 32, 16], F32, "sin")
    rk_all = P.sb([128, 32, 16], F32, "rk_all")
    stage_tables(P, C, "pos_pm", "b_inv", 32, 16, cos, sin)
    import os
    dbg = os.environ.get("BDBG", "123")
    if "1" in dbg:
        stage_b1(P, C, S, li, cos, sin, rk_all)
    if "2" in dbg:
        stage_b2(P, C, rk_all)
    if "3" in dbg:
        stage_outproj(P, C, S, D, C["b_w_out"][0], 8)
    P.end_group()


def stage_a1(P, C, S, li, cos, sin):
    TT = 512
    P.begin_stage()
    w_d = C["a_w_in"][0].rearrange("(k p) n -> p k n", p=128)
    wa = [P.sb([128, 8, 512], BF16, "wa") for _ in range(6)]
    N = NormUnit(P, TT, C["gains"][li, 1])
    ident = P.sb([128, 128], BF16, "ident")
    P.dma("pool", ident[:], C["ident"], writes=[ident])
    grow = P.sb([128, 128], F32, "grow")
    P.dma("sp", grow[:], C["a_grow"], writes=[grow])
    epsq = P.sb([128, 2], F32, "epsq")
    P.op("dve", lambda e: e.memset(epsq[:, 0:1], 1e-6), writes=[epsq])
    P.op("dve", lambda e: e.memset(epsq[:, 1:2], 64e-6), writes=[epsq], partial=True)
    xt = P.sb([128, 8, TT], F32, "xt")
    xn = P.sb([128, 8, SEQ], BF16, "xnseq")
    xn_parts = [P.sb([128, 8, TT], BF16, "xnp") for _ in range(2)]
    sq = [P.sb([128, 512], F32, "sq") for _ in range(2)]
    SSb = [P.sb([128, 32], F32, "ss") for _ in range(2)]
    Tb = [P.sb([128, 2048], F32, "T") for _ in range(2)]
    GTb = [P.sb([128, 8, 32], F32, "gt") for _ in range(2)]
    rt = [P.sb([128, 256], F32, "rt") for _ in range(8)]
    qkb = [P.sb([128, 2048], BF16, "qkb") for _ in range(2)]
    vb = [P.sb([128, 1024], BF16, "vb") for _ in range(2)]
    qTs = [P.sb([128, 16, 128], BF16, "qTs") for _ in range(2)]
    pp = [P.ps(name="pp") for _ in range(4)]
    ptr = [P.ps(name="ptr") for _ in range(2)]
    ipp = 0
    itr = 0
    irt = 0
    jt = 0
    deferred = []
    for s in range(2):
        for t4 in range(4):
            load_x(P, S, s * SEQ + t4 * TT, TT, xt)
            XP = xn_parts[t4 % 2]
            N.run(xt, XP)
            P.op("pool", lambda e, t4=t4, XP=XP: e.tensor_copy(out=xn[:, :, t4 * TT:(t4 + 1) * TT], in_=XP[:]), reads=[XP], writes=[xn], partial=(t4 > 0))
        for g in range(3):
            for c6 in range(6):
                col = (c6 // 2) * 3072 + g * 1024 + (c6 % 2) * 512
                P.dma("pool", wa[c6][:], w_d[:, :, col:col + 512], writes=[wa[c6]])
            qkT_v = C["a_qkT"][s * 3 + g].rearrange("h d t -> d h t")
            for ti in range(16):
                b = jt % 2
                jt += 1
                SS, T, GT, QKB, VB, QT = SSb[b], Tb[b], GTb[b], qkb[b], vb[b], qTs[b]
                tix = (s * 3 + g) * 16 + ti
                st, dil, nb = a_tile_start(g, ti)
                tok = slice(st, st + 127 * dil + 1, dil)
                cs, sn = cos[:, tix, :], sin[:, tix, :]
                for o, g0 in ((0, 0), (4, 64)):
                    ga, gb = grow[:, g0:g0 + 32], grow[:, g0 + 32:g0 + 64]
                    for idx, (gg, tt) in enumerate(((ga, cs), (gb, sn), (gb, cs), (ga, sn))):
                        P.op("pool", lambda e, o=o, idx=idx, gg=gg, tt=tt: e.tensor_tensor(out=GT[:, o + idx, :], in0=gg, in1=tt, op=ALU.mult),
                             reads=[grow, cos, sin], writes=[GT], partial=not (o == 0 and idx == 0))
                for c6 in range(6):
                    ps = pp[ipp % 4]
                    ipp += 1
                    mm_group(P, ps[:, :], ps, [(xn[:, k, tok], wa[c6][:, k, :], [xn, wa[c6]]) for k in range(8)])
                    if c6 >= 4:
                        P.op("act", lambda e, c6=c6: e.copy(out=VB[:, (c6 - 4) * 512:(c6 - 3) * 512], in_=ps[:, :]), reads=[ps], writes=[VB], partial=(c6 == 5))
                        continue
                    SQ = sq[c6 % 2]
                    P.op("act", lambda e: e.activation(out=SQ[:], in_=ps[:, :], func=AF.Square), reads=[ps], writes=[SQ])
                    P.op("dve", lambda e, c6=c6: e.reduce_sum(out=SS[:, c6 * 8:(c6 + 1) * 8], in_=SQ[:, :].rearrange("p (h d) -> p h d", d=64), axis=AX.X),
                         reads=[SQ], writes=[SS], partial=(c6 > 0))
                    o = 0 if c6 < 2 else 4
                    ps3 = ps[:, :].rearrange("p (h d) -> p h d", d=64)
                    x1, x2 = ps3[:, :, 0:32], ps3[:, :, 32:64]
                    bc = lambda i: GT[:, o + i, :].unsqueeze(1).to_broadcast([128, 8, 32])
                    r = [rt[(irt + i) % 8] for i in range(4)]
                    irt += 4
                    r3 = [r_[:, :].rearrange("p (h d) -> p h d", d=32) for r_ in r]
                    P.op("dve", lambda e: e.tensor_tensor(out=r3[0], in0=x1, in1=bc(0), op=ALU.mult), reads=[ps, GT], writes=[r[0]])
                    P.op("dve", lambda e: e.tensor_tensor(out=r3[1], in0=x2, in1=bc(1), op=ALU.mult), reads=[ps, GT], writes=[r[1]])
                    P.op("dve", lambda e: e.tensor_tensor(out=r3[2], in0=x2, in1=bc(2), op=ALU.mult), reads=[ps, GT], writes=[r[2]])
                    P.op("dve", lambda e: e.tensor_tensor(out=r3[3], in0=x1, in1=bc(3), op=ALU.mult), reads=[ps, GT], writes=[r[3]])
                    T3 = T[:, c6 * 512:(c6 + 1) * 512].rearrange("p (h d) -> p h d", d=64)
                    P.op("pool", lambda e: e.tensor_tensor(out=T3[:, :, 0:32], in0=r3[0], in1=r3[1], op=ALU.subtract), reads=[r[0], r[1]], writes=[T], partial=(c6 > 0))
                    P.op("pool", lambda e: e.tensor_tensor(out=T3[:, :, 32:64], in0=r3[2], in1=r3[3], op=ALU.add), reads=[r[2], r[3]], writes=[T], partial=True)
                while deferred:
                    deferred.pop(0)()
                P.op("act", lambda e: e.activation(out=SS[:, 0:16], in_=SS[:, 0:16], func=AF.Sqrt, scale=1.0 / 64, bias=epsq[:, 0:1]), reads=[SS, epsq], writes=[SS], partial=True)
                P.op("act", lambda e: e.activation(out=SS[:, 16:32], in_=SS[:, 16:32], func=AF.Sqrt, bias=epsq[:, 1:2]), reads=[SS, epsq], writes=[SS], partial=True)
                P.op("dve", lambda e: e.reciprocal(out=SS[:, :], in_=SS[:, :]), reads=[SS], writes=[SS], partial=True)
                v64 = lambda ap: ap.rearrange("p (h d) -> p h d", d=64)
                P.op("dve", lambda e: e.tensor_tensor(out=v64(QKB[:, 0:1024]), in0=v64(T[:, 0:1024]), in1=SS[:, 0:16].unsqueeze(2).to_broadcast([128, 16, 64]), op=ALU.mult),
                     reads=[T, SS], writes=[QKB])
                P.op("pool", lambda e: e.tensor_tensor(out=v64(QKB[:, 1024:2048]), in0=v64(T[:, 1024:2048]), in1=SS[:, 16:32].unsqueeze(2).to_broadcast([128, 16, 64]), op=ALU.mult),
                     reads=[T, SS], writes=[QKB], partial=True)
                P.dma("sp", C["a_v"][s * 3 + g, ti * 128:(ti + 1) * 128, :], VB[:], reads=[VB], writes=[C["a_v_buf"]], partial=True)
                def tr_fn(QKB=QKB, QT=QT, qkT_v=qkT_v, ti=ti):
                    nonlocal itr
                    for b4 in range(4):
                        pt = ptr[itr % 2]
                        itr += 1
                        for i in range(4):
                            hh = b4 * 4 + i
                            P.op("pe", lambda e, hh=hh, i=i: e.matmul(pt[:, i * 128:(i + 1) * 128], lhsT=QKB[:, hh * 128:(hh + 1) * 128], rhs=ident[:], start=True, stop=True),
                                 reads=[QKB, ident], writes=[pt], signal=(i == 3))
                        P.op("act", lambda e, b4=b4: e.copy(out=QT[:, b4 * 4:(b4 + 1) * 4, :], in_=pt[:, :].rearrange("p (h t) -> p h t", t=128)),
                             reads=[pt], writes=[QT], partial=(b4 > 0))
                    for q2 in range(2):
                        P.dma("sp", qkT_v[:, q2 * 8:(q2 + 1) * 8, ti * 128:(ti + 1) * 128], QT[:, q2 * 8:(q2 + 1) * 8, :], reads=[QT], writes=[C["a_qkT_buf"]], partial=True)
                deferred.append(tr_fn)
    while deferred:
        deferred.pop(0)()
    P.end_stage()


def stage_a2(P, C):
    P.begin_stage()
    mask = P.sb([128, 256], BF16, "mask")
    P.dma("pool", mask[:], C["a_mask"], writes=[mask])
    ones = P.sb([128, 64], BF16, "ones64")
    P.op("dve", lambda e: e.memset(ones[:], 1.0), writes=[ones])
    qT = [[P.sb([64, 2048], BF16, "qT") for _ in range(2)] for _ in range(2)]
    kT = [[P.sb([64, 2048], BF16, "kT") for _ in range(2)] for _ in range(2)]
    Vp = [P.sb([128, 16, 128], BF16, "Vp") for _ in range(2)]
    PT = [P.sb([128, 256], BF16, "PT") for _ in range(4)]
    accn = [P.sb([128, 2048], F32, "accn") for _ in range(2)]
    accd = [P.sb([128, 2048], F32, "accd") for _ in range(2)]
    oTs = [P.sb([128, 2048], BF16, "oTs") for _ in range(2)]
    ps_s = [P.ps(name="ps_s") for _ in range(3)]
    psn = [P.ps(name="psn") for _ in range(2)]
    psd = [P.ps(name="psd") for _ in range(2)]
    ist = 0
    iq = 0
    units = [(s, hp, g) for s in range(2) for hp in range(8) for g in range(3)]

    def load_unit(u, i):
        s, hp, g = u
        sg = s * 3 + g
        for e_ in range(2):
            P.dma("sp", qT[i][e_][:], C["a_qkT"][sg, hp, e_ * 64:(e_ + 1) * 64, :], reads=[C["a_qkT_buf"]], writes=[qT[i][e_]])
            P.dma("sp", kT[i][e_][:], C["a_qkT"][sg, 8 + hp, e_ * 64:(e_ + 1) * 64, :], reads=[C["a_qkT_buf"]], writes=[kT[i][e_]])
        vsrc = C["a_v"][sg, :, hp * 128:(hp + 1) * 128].rearrange("(j p) n -> p j n", p=128)
        for h2 in range(2):
            P.dma("sp", Vp[i][:, h2 * 8:(h2 + 1) * 8, :], vsrc[:, h2 * 8:(h2 + 1) * 8, :], reads=[C["a_v_buf"]], writes=[Vp[i]], partial=(h2 > 0))

    load_unit(units[0], 0)
    steps = [(ui, e_, qb, blk) for ui in range(len(units)) for e_ in range(2) for qb in range(4) for blk in range(4)]
    banks = {}

    def emit_qk(idx):
        nonlocal ist
        ui, e_, qb, blk = steps[idx]
        s, hp, g = units[ui]
        i = ui % 2
        nb = 16 // A_DIL[g]
        QT, KT = qT[i][e_], kT[i][e_]
        tq = 4 * qb + blk
        has_prev = (tq % nb) != 0
        pss = ps_s[ist % 3]
        pt_ = PT[ist % 4]
        ist += 1
        c0 = 0 if has_prev else 128
        if has_prev:
            P.op("pe", lambda e: e.matmul(pss[:, 0:128], lhsT=KT[:, (tq - 1) * 128:tq * 128], rhs=QT[:, tq * 128:(tq + 1) * 128], start=True, stop=True),
                 reads=[KT, QT], writes=[pss], signal=False)
        P.op("pe", lambda e: e.matmul(pss[:, 128:256], lhsT=KT[:, tq * 128:(tq + 1) * 128], rhs=QT[:, tq * 128:(tq + 1) * 128], start=True, stop=True),
             reads=[KT, QT], writes=[pss], partial=has_prev)
        P.op("act", lambda e: e.activation(out=pt_[:, c0:256], in_=pss[:, c0:256], func=AF.Exp), reads=[pss], writes=[pt_])
        P.op("dve", lambda e: e.tensor_tensor(out=pt_[:, c0:256], in0=pt_[:, c0:256], in1=mask[:, c0:256], op=ALU.mult), reads=[pt_, mask], writes=[pt_])
        return pt_

    def emit_pv(idx, pt_):
        nonlocal iq
        ui, e_, qb, blk = steps[idx]
        s, hp, g = units[ui]
        i = ui % 2
        if e_ == 0 and qb == 0 and blk == 0 and ui + 1 < len(units):
            load_unit(units[ui + 1], (ui + 1) % 2)
        nb = 16 // A_DIL[g]
        V = Vp[i]
        e0 = e_ * 64
        tq = 4 * qb + blk
        has_prev = (tq % nb) != 0
        if blk == 0:
            banks[(ui, e_, qb)] = (psn[iq % 2], psd[iq % 2])
            iq += 1
        pn, pd = banks[(ui, e_, qb)]
        AN, AD = accn[(ui // 3) % 2], accd[(ui // 3) % 2]
        osl = slice(blk * 128, (blk + 1) * 128)
        if has_prev:
            P.op("pe", lambda e: e.matmul(pn[e0:e0 + 64, osl], lhsT=V[:, tq - 1, e0:e0 + 64], rhs=pt_[:, 0:128], start=True, stop=False),
                 reads=[V, pt_], writes=[pn], signal=False, partial=True)
        P.op("pe", lambda e: e.matmul(pn[e0:e0 + 64, osl], lhsT=V[:, tq, e0:e0 + 64], rhs=pt_[:, 128:256], start=(not has_prev), stop=True),
             reads=[V, pt_], writes=[pn], signal=False, partial=True)
        if has_prev:
            P.op("pe", lambda e: e.matmul(pd[e0:e0 + 64, osl], lhsT=ones[:], rhs=pt_[:, 0:128], start=True, stop=False),
                 reads=[ones, pt_], writes=[pd], signal=False, partial=True)
        P.op("pe", lambda e: e.matmul(pd[e0:e0 + 64, osl], lhsT=ones[:], rhs=pt_[:, 128:256], start=(not has_prev), stop=True),
             reads=[ones, pt_], writes=[pd], signal=True, partial=True)
        if blk == 3:
            for acc, src in ((AN, pn), (AD, pd)):
                if g == 0:
                    dst = acc[e0:e0 + 64, qb * 512:(qb + 1) * 512]
                    sv = src[e0:e0 + 64, :]
                    P.op("act", lambda e, dst=dst, sv=sv: e.copy(out=dst, in_=sv), reads=[src], writes=[acc], partial=True)
                else:
                    if g == 1:
                        dst = acc[e0:e0 + 64, :].rearrange("p (l r) -> p r l", r=4)[:, qb, :]
                        sv = src[e0:e0 + 64, :]
                    else:
                        dst = acc[e0:e0 + 64, :].rearrange("p (u r) -> p r u", r=16)[:, 4 * qb:4 * qb + 4, :]
                        sv = src[e0:e0 + 64, :].rearrange("p (b u) -> p b u", u=128)
                    P.op("dve", lambda e, dst=dst, sv=sv: e.tensor_tensor(out=dst, in0=dst, in1=sv, op=ALU.add), reads=[src, acc], writes=[acc], partial=True)
            if g == 2 and e_ == 1 and qb == 3:
                OT = oTs[(ui // 3) % 2]
                P.op("dve", lambda e: e.reciprocal(out=AD[:], in_=AD[:]), reads=[AD], writes=[AD])
                P.op("dve", lambda e: e.tensor_tensor(out=OT[:], in0=AN[:], in1=AD[:], op=ALU.mult), reads=[AN, AD], writes=[OT])
                P.dma("sp", C["oT"][hp, :, s * 2048:(s + 1) * 2048], OT[:], reads=[OT, AN, AD], writes=[C["oT_buf"]], partial=True)

    pend = [emit_qk(0), emit_qk(1)]
    for idx in range(len(steps)):
        if idx + 2 < len(steps):
            pend.append(emit_qk(idx + 2))
        emit_pv(idx, pend.pop(0))
    P.end_stage()


def stage_mix_a(P, C, S, D, li):
    import os
    dbg = os.environ.get("ADBG", "123")
    P.begin_group()
    cos = P.sb([128, 96, 32], F32, "cos")
    sin = P.sb([128, 96, 32], F32, "sin")
    stage_tables(P, C, "pos_a", "a_inv", 96, 32, cos, sin)
    if "1" in dbg:
        stage_a1(P, C, S, li, cos, sin)
    if "2" in dbg:
        stage_a2(P, C)
    if "3" in dbg:
        stage_outproj(P, C, S, D, C["a_w_out"][0], 8)
    P.end_group()


MIXERS[3] = stage_mix_d
MIXERS[2] = stage_mix_c
MIXERS[1] = stage_mix_b
MIXERS[0] = stage_mix_a


FULL_STAGES = []
for _li in range(4):
    FULL_STAGES += [("ffn", _li, 0), ("mix", _li), ("ffn", _li, 1)]

WEIGHT_SPECS = [
    ("ffn_w_in", [4, 2, 1024, 5632]), ("ffn_w_out", [4, 2, 2816, 1024]),
    ("a_w_in", [1, 1024, 9216]), ("a_w_out", [1, 1024, 1024]),
    ("b_w_in", [1, 1024, 672]), ("b_w_q_up", [1, 384, 1536]), ("b_w_kv_up", [1, 256, 2048]), ("b_w_out", [1, 1024, 1024]),
    ("c_w_in", [1, 1024, 6144]), ("c_w_out", [1, 2048, 1024]),
    ("d_w_in", [1, 1024, 2048]), ("d_w_rg", [1, 4, 256, 256]), ("d_w_ig", [1, 4, 256, 256]), ("d_w_out", [1, 1024, 1024]),
]


def build_program(stages):
    P = Prog()
    nc = P.nc
    C = {}
    x_in = nc.dram_tensor("x", [DM, NTOK], F32, kind="ExternalInput").ap()
    out = nc.dram_tensor("out", [DM, NTOK], F32, kind="ExternalOutput").ap()
    xs = nc.dram_tensor("xs", [DM, NTOK], F32, kind="Internal").ap()
    C["positions"] = nc.dram_tensor("positions", [2, SEQ], I32, kind="ExternalInput").ap()
    C["gains"] = nc.dram_tensor("gains", [4, 3, 128, 8], F32, kind="ExternalInput").ap()
    for name, shape in WEIGHT_SPECS:
        C[name] = nc.dram_tensor(name, shape, F32, kind="ExternalInput").ap()
    for name, shape in small_specs():
        C[name] = nc.dram_tensor(name, shape, F32, kind="ExternalInput").ap()
    for name, shape in int_specs():
        C[name] = nc.dram_tensor(name, shape, I32, kind="ExternalInput").ap()
    for name, shape in scratch_specs():
        C[name] = nc.dram_tensor(name, shape, BF16, kind="Internal").ap()
        C[name + "_buf"] = P.dbuf(C[name], name)
    Sx, So, Ss = Stream(P, x_in), Stream(P, out), Stream(P, xs)
    n = len(stages)
    for i, st in enumerate(stages):
        S = Sx if i == 0 else Ss
        D = So if i == n - 1 else Ss
        if st[0] == "ffn":
            stage_ffn(P, C, S, D, st[1], st[2])
        else:
            MIXERS[st[1] % 4](P, C, S, D, st[1])
    P.finish(So.bufs)
    return P


def host_inputs(inputs, core):
    m = {}
    xc = np.asarray(inputs["x"][2 * core:2 * core + 2], dtype=np.float32).reshape(NTOK, DM)
    m["x"] = np.ascontiguousarray(xc.T)
    m["positions"] = np.ascontiguousarray(np.asarray(inputs["positions"][2 * core:2 * core + 2], dtype=np.int32))
    g = np.asarray(inputs["norm_gains"], dtype=np.float32).reshape(4, 3, 8, 128)
    m["gains"] = np.ascontiguousarray(g.transpose(0, 1, 3, 2))
    for name, shape in WEIGHT_SPECS:
        m[name] = np.ascontiguousarray(np.asarray(inputs[name], dtype=np.float32))
    m.update(host_small(inputs))
    m.update(host_pos(m["positions"]))
    return m


_PROG_CACHE = {}


def run_stages(inputs, stages, cores):
    key = tuple(stages)
    if key not in _PROG_CACHE:
        _PROG_CACHE[key] = build_program(stages)
    P = _PROG_CACHE[key]
    shared = None
    in_maps = []
    for c in cores:
        m = host_inputs(inputs, c) if shared is None else dict(shared)
        if shared is None:
            shared = m
        else:
            xc = np.asarray(inputs["x"][2 * c:2 * c + 2], dtype=np.float32).reshape(NTOK, DM)
            m["x"] = np.ascontiguousarray(xc.T)
            m["positions"] = np.ascontiguousarray(np.asarray(inputs["positions"][2 * c:2 * c + 2], dtype=np.int32))
            m.update(host_pos(m["positions"]))
        in_maps.append(m)
    res = run_bass_kernel_spmd(P.nc, in_maps, core_ids=list(range(len(cores))))
    outs = [np.asarray(r["out"]).T.reshape(2, SEQ, DM) for r in res.results]
    return outs


def kernel(**inputs):
    outs = run_stages(inputs, FULL_STAGES, list(range(8)))
    return np.concatenate(outs, axis=0).astype(np.float32)
```
